# Optimizing a Trainium2 kernel written in Bass

```python
import jax, jax.numpy as jnp
from jax import lax
import numpy as np

D_MODEL = 1024
BATCH = 8
SEQ = 4096
DEPTH = 1

CHUNK = 64
Q_BLOCK = 128
MLA_HEADS = 8
MLA_NOPE = 64
MLA_ROPE = 32
MLA_QK = MLA_NOPE + MLA_ROPE
MLA_V = 64
Q_LORA = 256
KV_LORA = 128
ROPE_BASE = 10000.0
CA_HEADS = 8
CA_HEAD_DIM = 64
LEFT_CHUNKS = 8
BAND = (LEFT_CHUNKS + 1) * CHUNK
MAX_REL = 256
MLA_WIDTH = MLA_HEADS * MLA_V
CA_WIDTH = CA_HEADS * CA_HEAD_DIM
IN_WIDTHS = (Q_LORA, KV_LORA, MLA_ROPE, CA_WIDTH, CA_WIDTH, CA_WIDTH, D_MODEL, D_MODEL)
IN_WIDTH = Q_LORA + KV_LORA + MLA_ROPE + 3 * CA_WIDTH + 2 * D_MODEL
N_GROUPS = 4
EXPERTS_PER_GROUP = 8
N_EXPERTS = N_GROUPS * EXPERTS_PER_GROUP
TOP_K = 2
EXPERT_FF = 256
NORM_EPS = 1e-6
NEG_INF = -1e30
N_MOD = 6

kernel_name = 'hybrid_mla_chunkattn_hmoe_adaln'


def rms_norm(x, g):
    xf = x.astype(jnp.float32)
    y = xf * lax.rsqrt(jnp.mean(xf * xf, axis=-1, keepdims=True) + NORM_EPS)
    return (y * g.astype(jnp.float32)).astype(x.dtype)


def modulate(x, shift, scale):
    return x * (1 + scale[:, None, :]) + shift[:, None, :]


def rope(x, positions):
    half = x.shape[-1] // 2
    inv = ROPE_BASE ** (-jnp.arange(half, dtype=jnp.float32) / half)
    ang = positions.astype(jnp.float32)[..., None] * inv
    ang = ang.reshape(ang.shape[:2] + (1,) * (x.ndim - 3) + (half,))
    cos = jnp.cos(ang).astype(x.dtype)
    sin = jnp.sin(ang).astype(x.dtype)
    x1, x2 = x[..., :half], x[..., half:]
    return jnp.concatenate([x1 * cos - x2 * sin, x2 * cos + x1 * sin], axis=-1)


def mla_attend(q, k, v):
    b, s, h, _ = q.shape
    n_blocks = s // Q_BLOCK
    scale = MLA_QK ** -0.5
    key_chunk = jnp.arange(s) // CHUNK

    def one_block(i):
        qb = lax.dynamic_slice_in_dim(q, i * Q_BLOCK, Q_BLOCK, axis=1)
        sc = jnp.einsum('bqhd,bkhd->bhqk', qb, k).astype(jnp.float32) * scale
        q_chunk = (i * Q_BLOCK + jnp.arange(Q_BLOCK)) // CHUNK
        mask = key_chunk[None, :] <= q_chunk[:, None]
        sc = jnp.where(mask[None, None], sc, NEG_INF)
        p = jax.nn.softmax(sc, axis=-1).astype(v.dtype)
        return jnp.einsum('bhqk,bkhd->bqhd', p, v)

    out = lax.map(one_block, jnp.arange(n_blocks))
    return out.transpose(1, 0, 2, 3, 4).reshape(b, s, h * v.shape[-1])


def chunk_band_attend(q, k, v, rel_table):
    b, s, h, d = q.shape
    n_chunks = s // CHUNK
    scale = d ** -0.5
    pad = ((0, 0), (LEFT_CHUNKS * CHUNK, 0), (0, 0), (0, 0))
    kp = jnp.pad(k, pad)
    vp = jnp.pad(v, pad)
    rel = (jnp.arange(CHUNK)[:, None] + LEFT_CHUNKS * CHUNK) - jnp.arange(BAND)[None, :]
    idx = jnp.clip(rel, -MAX_REL, MAX_REL) + MAX_REL
    bias = rel_table.astype(jnp.float32)[:, idx]

    def one_chunk(n):
        qc = lax.dynamic_slice_in_dim(q, n * CHUNK, CHUNK, axis=1)
        kb = lax.dynamic_slice_in_dim(kp, n * CHUNK, BAND, axis=1)
        vb = lax.dynamic_slice_in_dim(vp, n * CHUNK, BAND, axis=1)
        sc = jnp.einsum('bqhd,bkhd->bhqk', qc, kb).astype(jnp.float32) * scale + bias[None]
        valid = (n * CHUNK - LEFT_CHUNKS * CHUNK + jnp.arange(BAND)) >= 0
        sc = jnp.where(valid[None, None, None, :], sc, NEG_INF)
        p = jax.nn.softmax(sc, axis=-1).astype(vb.dtype)
        return jnp.einsum('bhqk,bkhd->bqhd', p, vb)

    out = lax.map(one_chunk, jnp.arange(n_chunks))
    return out.transpose(1, 0, 2, 3, 4).reshape(b, s, h * d)


def hybrid_mixer(h, positions, w_in, g_q, w_uq, g_kv, w_uk, w_uv, rel_table, w_oa, w_ob, w_out):
    b, s, _ = h.shape
    proj = h @ w_in
    cuts = [int(v) for v in np.cumsum(IN_WIDTHS)[:-1]]
    q_lat, kv_lat, k_rope, q_b, k_b, v_b, gate_a, gate_b = jnp.split(proj, cuts, axis=-1)
    q_a = (rms_norm(q_lat, g_q) @ w_uq).reshape(b, s, MLA_HEADS, MLA_QK)
    q_a = jnp.concatenate([q_a[..., :MLA_NOPE], rope(q_a[..., MLA_NOPE:], positions)], axis=-1)
    kv_n = rms_norm(kv_lat, g_kv)
    k_nope = (kv_n @ w_uk).reshape(b, s, MLA_HEADS, MLA_NOPE)
    v_a = (kv_n @ w_uv).reshape(b, s, MLA_HEADS, MLA_V)
    k_r = rope(k_rope, positions)
    k_a = jnp.concatenate(
        [k_nope, jnp.broadcast_to(k_r[:, :, None, :], (b, s, MLA_HEADS, MLA_ROPE))], axis=-1)
    o_a = mla_attend(q_a, k_a, v_a)
    o_b = chunk_band_attend(
        q_b.reshape(b, s, CA_HEADS, CA_HEAD_DIM),
        k_b.reshape(b, s, CA_HEADS, CA_HEAD_DIM),
        v_b.reshape(b, s, CA_HEADS, CA_HEAD_DIM), rel_table)
    merged = jax.nn.sigmoid(gate_a) * (o_a @ w_oa) + jax.nn.sigmoid(gate_b) * (o_b @ w_ob)
    return merged @ w_out


def hierarchical_moe(h, w_rg, b_rg, w_re, b_re, w_gate, w_up, w_down):
    b, s, d = h.shape
    t = h.reshape(b * s, d)
    n_tok = t.shape[0]
    g_prob = jax.nn.softmax((t @ w_rg + b_rg).astype(jnp.float32), axis=-1)
    g_w, g_idx = lax.top_k(g_prob, 1)
    g_w, g_idx = g_w[:, 0], g_idx[:, 0]
    e_logits = (t @ w_re + b_re).astype(jnp.float32).reshape(n_tok, N_GROUPS, EXPERTS_PER_GROUP)
    e_sel = jnp.take_along_axis(e_logits, g_idx[:, None, None], axis=1)[:, 0]
    e_prob = jax.nn.softmax(e_sel, axis=-1)
    top_w, top_i = lax.top_k(e_prob, TOP_K)
    top_w = top_w / jnp.sum(top_w, axis=-1, keepdims=True)
    in_group = jnp.sum(jax.nn.one_hot(top_i, EXPERTS_PER_GROUP, dtype=jnp.float32) * top_w[..., None], axis=1)
    combine = (jax.nn.one_hot(g_idx, N_GROUPS, dtype=jnp.float32)[:, :, None]
               * in_group[:, None, :] * g_w[:, None, None]).astype(h.dtype)
    y = jnp.zeros_like(t)
    for g in range(N_GROUPS):
        sl = slice(g * EXPERTS_PER_GROUP, (g + 1) * EXPERTS_PER_GROUP)
        a = jnp.einsum('td,edf->tef', t, w_gate[sl])
        u = jnp.einsum('td,edf->tef', t, w_up[sl])
        hid = jax.nn.silu(a) * u * combine[:, g, :, None]
        y = y + jnp.einsum('tef,efd->td', hid, w_down[sl])
    return y.reshape(b, s, d)


def setup_inputs(seed: int = 0) -> dict:
    key = jax.random.key(seed)
    ks = iter(jax.random.split(key, 32))
    L, D = DEPTH, D_MODEL

    def w(shape, fan_in):
        return jax.random.normal(next(ks), shape, jnp.float32) * (fan_in ** -0.5)

    def gain(shape):
        return 1.0 + 0.02 * jax.random.normal(next(ks), shape, jnp.float32)

    def small(shape, s=0.01):
        return s * jax.random.normal(next(ks), shape, jnp.float32)

    x = jax.random.normal(next(ks), (BATCH, SEQ, D), jnp.float32)
    c = jax.random.normal(next(ks), (BATCH, D), jnp.float32)
    offsets = jax.random.randint(next(ks), (BATCH, 1), 0, 10000, dtype=jnp.int32)
    positions = offsets + jnp.arange(SEQ, dtype=jnp.int32)[None, :]
    return {
        'x': x,
        'c': c,
        'positions': positions,
        'w_ada': w((L, D, N_MOD * D), D),
        'b_ada': small((L, N_MOD * D), 0.02),
        'g_mix': gain((L, D)),
        'w_in': w((L, D, IN_WIDTH), D),
        'g_q': gain((L, Q_LORA)),
        'w_uq': w((L, Q_LORA, MLA_HEADS * MLA_QK), Q_LORA),
        'g_kv': gain((L, KV_LORA)),
        'w_uk': w((L, KV_LORA, MLA_HEADS * MLA_NOPE), KV_LORA),
        'w_uv': w((L, KV_LORA, MLA_WIDTH), KV_LORA),
        'rel_bias': small((L, CA_HEADS, 2 * MAX_REL + 1), 0.5),
        'w_oa': w((L, MLA_WIDTH, D), MLA_WIDTH),
        'w_ob': w((L, CA_WIDTH, D), CA_WIDTH),
        'w_out': w((L, D, D), D),
        'g_ffn': gain((L, D)),
        'w_rg': w((L, D, N_GROUPS), D),
        'b_rg': small((L, N_GROUPS)),
        'w_re': w((L, D, N_EXPERTS), D),
        'b_re': small((L, N_EXPERTS)),
        'w_gate': w((L, N_EXPERTS, D, EXPERT_FF), D),
        'w_up': w((L, N_EXPERTS, D, EXPERT_FF), D),
        'w_down': w((L, N_EXPERTS, EXPERT_FF, D), EXPERT_FF),
        'g_final': gain((D,)),
    }


def reference(x, c, positions, w_ada, b_ada, g_mix, w_in, g_q, w_uq, g_kv, w_uk, w_uv,
              rel_bias, w_oa, w_ob, w_out, g_ffn, w_rg, b_rg, w_re, b_re,
              w_gate, w_up, w_down, g_final):
    c_act = jax.nn.silu(c)
    for l in range(DEPTH):
        mod = c_act @ w_ada[l] + b_ada[l]
        sh1, sc1, gt1, sh2, sc2, gt2 = jnp.split(mod, N_MOD, axis=-1)
        h = modulate(rms_norm(x, g_mix[l]), sh1, sc1)
        mix = hybrid_mixer(h, positions, w_in[l], g_q[l], w_uq[l], g_kv[l], w_uk[l], w_uv[l],
                           rel_bias[l], w_oa[l], w_ob[l], w_out[l])
        x = x + gt1[:, None, :] * mix
        h = modulate(rms_norm(x, g_ffn[l]), sh2, sc2)
        ffn = hierarchical_moe(h, w_rg[l], b_rg[l], w_re[l], b_re[l], w_gate[l], w_up[l], w_down[l])
        x = x + gt2[:, None, :] * ffn
    return rms_norm(x, g_final)
```

```python
import os
import math
from contextlib import ExitStack

import numpy as np
import concourse.bass as bass
import concourse.mybir as mybir
from concourse.bass_utils import run_bass_kernel_spmd

F32 = mybir.dt.float32
BF16 = mybir.dt.bfloat16
I32 = mybir.dt.int32
U32 = mybir.dt.uint32
U8 = mybir.dt.uint8
AF = mybir.ActivationFunctionType
ALU = mybir.AluOpType
AX = mybir.AxisListType

S = 4096
D = 1024
TT = 512
NT = S // TT
NB = S // 128
EPS = 1e-6
WC = 4192
C_QB, C_KB, C_VB, C_GA, C_GB, C_KRP, C_KRS = 416, 928, 1440, 1952, 2976, 4000, 4096
PI = math.pi
SLOT_T = 256
SH = 8
NTILE = 64
NSLOT = NTILE * SLOT_T


class Sched:
    COMPUTE = ("pe", "act", "dve", "pool")

    def __init__(self, nc, es, n_dma_sems=8):
        self.nc = nc
        self.ops = []
        self.n_dma_sems = n_dma_sems
        self.eng_sem = {e: es.enter_context(nc.semaphore("c_" + e)) for e in self.COMPUTE}
        self.dma_sems = {q: [es.enter_context(nc.semaphore("d_%s%d" % (q, i))) for i in range(n_dma_sems)]
                         for q in ("sp", "pool", "act")}
        self.state = {}
        self.last_on = {}
        self.dma_since_bar = []

    def add(self, eng, fn, reads=(), writes=(), dma=False, extra_deps=()):
        op = dict(eng=eng, fn=fn, dma=dma, idx=len(self.ops), signal=False)
        deps = set(extra_deps)
        st = self.state
        for k in reads:
            w, rd = st.setdefault(k, [None, {}])
            if w is not None:
                deps.add(w)
        for k in writes:
            w, rd = st.setdefault(k, [None, {}])
            if w is not None:
                deps.add(w)
            deps.update(rd.values())
        me = (eng, "dma", op["idx"]) if dma else eng
        for k in reads:
            st[k][1][me] = op["idx"]
        for k in writes:
            st[k][0] = op["idx"]
            st[k][1] = {}
        real = set()
        for d in deps:
            dop = self.ops[d]
            if (not dop["dma"]) and (not dma) and dop["eng"] == "pe" and eng == "pe":
                continue
            real.add(d)
            dop["signal"] = True
        op["deps"] = real
        self.ops.append(op)
        if dma:
            self.dma_since_bar.append(op["idx"])
        elif fn is not None:
            self.last_on[eng] = op["idx"]
        return op

    def barrier(self):
        lasts = dict(self.last_on)
        dmas = list(self.dma_since_bar)
        self.dma_since_bar = []
        for eng in ("pe", "act", "dve", "pool", "sp"):
            deps = [v for e, v in lasts.items() if e != eng] + dmas
            self.add(eng, None, extra_deps=deps)
        self.state = {}

    def emit(self, block):
        cnt = {e: 0 for e in self.COMPUTE}
        dcnt = {q: 0 for q in self.dma_sems}
        for op in self.ops:
            if not op["signal"]:
                continue
            if op["dma"]:
                q = op["eng"]
                i = dcnt[q]
                dcnt[q] += 1
                op["sem"] = self.dma_sems[q][i % self.n_dma_sems]
                op["val"] = 16 * (i // self.n_dma_sems + 1)
            else:
                e = op["eng"]
                assert op["fn"] is not None
                cnt[e] += 1
                op["sem"] = self.eng_sem[e]
                op["val"] = cnt[e]
        self.stats = dict(cnt=cnt, dcnt=dcnt, nops=len(self.ops))
        per_eng = {e: [] for e in ("pe", "act", "dve", "pool", "sp")}
        for op in self.ops:
            per_eng[op["eng"]].append(op)
        ops = self.ops

        def run(eng_name, engine):
            seen = {}
            for op in per_eng[eng_name]:
                need = {}
                for d in op["deps"]:
                    dop = ops[d]
                    s, v = dop["sem"], dop["val"]
                    if need.get(s.num, (None, 0))[1] < v:
                        need[s.num] = (s, v)
                for num, (s, v) in need.items():
                    if seen.get(num, 0) >= v:
                        continue
                    engine.wait_ge(s, v)
                    seen[num] = v
                if op["fn"] is None:
                    continue
                ins = op["fn"](engine)
                if op["signal"]:
                    ins.then_inc(op["sem"], 16 if op["dma"] else 1)

        block.tensor(lambda e: run("pe", e))
        block.scalar(lambda e: run("act", e))
        block.vector(lambda e: run("dve", e))
        block.gpsimd(lambda e: run("pool", e))
        block.sync(lambda e: run("sp", e))


class Arena:
    def __init__(self, big, size):
        self.big = big
        self.size = size
        self.top = 0

    def alloc(self, free_shape, dtype):
        esz = {F32: 4, BF16: 2, I32: 4, U32: 4, U8: 1}[dtype]
        n = int(np.prod(free_shape))
        nbytes = (n * esz + 63) // 64 * 64
        off = self.top
        self.top += nbytes
        assert self.top <= self.size, "SBUF arena overflow %d > %d" % (self.top, self.size)
        v = self.big[:, off:off + n * esz]
        if dtype != U8:
            v = v.bitcast(dtype)
        if len(free_shape) == 2:
            v = v.rearrange("p (a b) -> p a b", b=free_shape[1])
        elif len(free_shape) == 3:
            v = v.rearrange("p (a b c) -> p a b c", b=free_shape[1], c=free_shape[2])
        return v

    def mark(self):
        return self.top

    def release(self, m):
        self.top = m


def build_nc(stage=99, dbg=False):
    sub = int(os.environ.get('KSUB', '99'))
    nc = bass.Bass("TRN2", target_bir_lowering=False)
    es = ExitStack()

    def din(name, shape, dt=F32):
        return nc.dram_tensor(name, list(shape), dt, kind="ExternalInput").ap()

    def dscr(name, shape, dt):
        kind = "ExternalOutput" if dbg else "Internal"
        return nc.dram_tensor(name, list(shape), dt, kind=kind).ap()

    x_d = din("x", [S, D])
    cfm_d = din("cfm", [128, 8])
    pos_d = din("pos", [1, S], I32)
    wada_d = din("wada", [128, 8 * 6144])
    badar_d = din("bada_row", [1, 6144])
    gmixr_d = din("gmix_row", [1, D])
    gffnr_d = din("gffn_row", [1, D])
    gfin_d = din("gfinal_row", [1, D])
    win_d = din("win", [128, 8 * WC])
    gq_d = din("gq_fm", [128, 2])
    gkv_d = din("gkv_fm", [128, 1])
    wuq_d = din("wuq", [128, 2 * 768])
    wuqs_d = din("wuq_sw", [128, 2 * 768])
    wuk_d = din("wuk", [128, 512])
    wuv_d = din("wuv", [128, 512])
    rconst_d = din("rconst", [128, 2])
    ident_d = din("ident", [128, 128])
    sel_d = din("sel", [128, 96])
    biasT_d = din("biasT", [128, 8 * 640])
    woa_d = din("woa", [128, 4 * D])
    wob_d = din("wob", [128, 4 * D])
    wout_d = din("wout", [128, 8 * D])
    wr_d = din("wr", [128, 8 * 36])
    rb_d = din("rb", [1, 36])
    iota_d = din("iota_e", [128, 32])
    lst_d = din("lst", [128, 128])
    jv_d = din("jv", [128, NTILE])
    pidx_d = din("pidx", [128, 1])
    wg_d = din("wgl", [32 * 128, 2048])
    wu_d = din("wul", [32 * 128, 2048])
    wdn_d = din("wdl", [32 * 128, 2048])
    out_d = nc.dram_tensor("out", [S, D], F32, kind="ExternalOutput").ap()

    tabc_d = dscr("tabc", [128, 512], F32)
    tabsp_d = dscr("tabsp", [128, 512], F32)
    tabsn_d = dscr("tabsn", [128, 512], F32)
    qT_d = dscr("qT", [96, 8 * S], BF16)
    kT_d = dscr("kT", [96, 8 * S], BF16)
    va_d = dscr("va", [S, 520], BF16)
    qbT_d = dscr("qbT", [128, 4 * S], BF16)
    kbT_d = dscr("kbT", [128, 4 * S], BF16)
    vb_d = dscr("vb", [S, 520], BF16)
    ga_d = dscr("gaT", [128, 8 * S], F32)
    gb_d = dscr("gbT", [128, 8 * S], F32)
    moddbg_d = dscr("moddbg", [128, 48], F32) if dbg else None
    x1_d = dscr("x1s", [S, D], F32)
    h2_d = dscr("h2s", [S, D], BF16)
    xs_d = dscr("xs", [NSLOT, D], BF16)
    ys_d = dscr("ys", [NSLOT, D], F32)
    wx_d = nc.dram_tensor("wx", [32 * 128, 6144], BF16, kind="Internal").ap()

    SB_BYTES = 212480
    big = nc.alloc_sbuf_tensor("big", [128, SB_BYTES], U8)
    A = Arena(big, SB_BYTES)
    banks = [nc.alloc_psum_tensor("psb%d" % i, [128, 512], F32).ap() for i in range(8)]
    sc = Sched(nc, es)

    psn = [0]

    def ps_next():
        i = psn[0] % 8
        psn[0] += 1
        return banks[i], ("ps", i)

    def dma(q, out, in_, reads, writes, **kw):
        sc.add(q, lambda e: e.dma_start(out=out, in_=in_, **kw), reads, writes, dma=True)

    def mm(out, lhsT, rhs, start, stop, reads, writes):
        sc.add("pe", lambda e: e.matmul(out, lhsT, rhs, start=start, stop=stop), reads, writes)

    def tr(out, in_, ident, reads, writes):
        sc.add("pe", lambda e: e.transpose(out, in_, ident), reads, writes)

    def act(out, in_, func, reads, writes, **kw):
        sc.add("act", lambda e: e.activation(out=out, in_=in_, func=func, **kw), reads, writes)

    def tcopy(eng, out, in_, reads, writes):
        if eng == "act":
            sc.add(eng, lambda e: e.activation(out=out, in_=in_, func=AF.Copy), reads, writes)
        else:
            sc.add(eng, lambda e: e.tensor_copy(out=out, in_=in_), reads, writes)

    def tt(eng, out, in0, in1, op, reads, writes):
        sc.add(eng, lambda e: e.tensor_tensor(out=out, in0=in0, in1=in1, op=op), reads, writes)

    def ts(eng, out, in0, s1, s2, op0, op1, reads, writes):
        if s2 is None:
            sc.add(eng, lambda e: e.tensor_scalar(out=out, in0=in0, scalar1=s1, scalar2=None, op0=op0),
                   reads, writes)
        else:
            sc.add(eng, lambda e: e.tensor_scalar(out=out, in0=in0, scalar1=s1, scalar2=s2, op0=op0, op1=op1),
                   reads, writes)

    def stt(out, in0, scalar, in1, op0, op1, reads, writes):
        sc.add("dve", lambda e: e.scalar_tensor_tensor(out=out, in0=in0, scalar=scalar, in1=in1, op0=op0, op1=op1),
               reads, writes)

    def memset(eng, ap, val, writes):
        sc.add(eng, lambda e: e.memset(ap, val), (), writes)

    def recip(out, in_, reads, writes):
        sc.add("dve", lambda e: e.reciprocal(out=out, in_=in_), reads, writes)

    ident_f = A.alloc([128], F32)
    ident_b = A.alloc([128], BF16)
    ones_f = A.alloc([128], F32)
    ones_b = A.alloc([128], BF16)
    eps_c = A.alloc([1], F32)
    modfm = A.alloc([48], F32)
    s1_fm = A.alloc([8], F32)
    s2_fm = A.alloc([8], F32)
    gt1_bc = A.alloc([D], F32)
    gt2_bc = A.alloc([D], F32)
    s2_bc = A.alloc([D], F32)
    b2_bc = A.alloc([D], F32)

    stg = A.alloc([2048], BF16)
    pre_steps = []
    for mi, wsrc in enumerate((wg_d, wu_d, wdn_d)):
        for e_ in range(32):
            pre_steps.append(("ld", mi, wsrc, e_))
            pre_steps.append(("st", mi, wsrc, e_))
    pre_pos = [0]

    def precast_step():
        if pre_pos[0] >= len(pre_steps):
            return
        kind, mi, wsrc, e_ = pre_steps[pre_pos[0]]
        pre_pos[0] += 1
        if kind == "ld":
            dma("pool", stg, wsrc[e_ * 128:(e_ + 1) * 128, :], (), ["stg"])
        else:
            dma("sp", wx_d[e_ * 128:(e_ + 1) * 128, mi * 2048:(mi + 1) * 2048], stg, ["stg"], [("wx", mi, e_)])

    dma("sp", ident_f, ident_d, (), ["ident_f"])
    tcopy("dve", ident_b, ident_f, ["ident_f"], ["ident_b"])
    memset("dve", ones_f, 1.0, ["ones_f"])
    memset("dve", ones_b, 1.0, ["ones_b"])
    memset("dve", eps_c, EPS, ["eps_c"])

    pP = A.mark()
    wuq_b = A.alloc([2, 768], BF16)
    wuqs_b = A.alloc([2, 768], BF16)
    wukp_b = A.alloc([8, 96], BF16)
    wuv_b = A.alloc([512], BF16)
    sel_b = A.alloc([96], BF16)
    gq = A.alloc([2], F32)
    gkv = A.alloc([1], F32)
    win_b = A.alloc([8, WC], BF16)
    winv = win_d.rearrange("p (k n) -> p k n", k=8)
    for kc in range(8):
        for c0 in range(0, WC, 1048):
            dma("pool", win_b[:, kc, c0:c0 + 1048], winv[:, kc, c0:c0 + 1048], (), [("win", kc, c0)])
    p0 = A.mark()
    cfm = A.alloc([8], F32)
    cact = A.alloc([8], F32)
    c_rep = A.alloc([8, 128], F32)
    bada_bc = A.alloc([6144], F32)
    gmix_bc = A.alloc([D], F32)
    gffn_bc = A.alloc([D], F32)
    sh1_r = A.alloc([D], F32)
    sc1_r = A.alloc([D], F32)
    sc2_r = A.alloc([D], F32)
    dtmp = A.alloc([128], F32)
    wbuf = [A.alloc([3072], F32) for _ in range(2)]
    dma("sp", cfm, cfm_d, (), ["cfm"])
    dma("sp", bada_bc, badar_d[0:1, :].to_broadcast([128, 6144]), (), ["bada_bc"])
    dma("sp", gmix_bc, gmixr_d[0:1, :].to_broadcast([128, D]), (), ["gmix_bc"])
    dma("sp", gffn_bc, gffnr_d[0:1, :].to_broadcast([128, D]), (), ["gffn_bc"])
    act(cact, cfm, AF.Silu, ["cfm"], ["cact"])
    tcopy("dve", c_rep, cact[:, :, None].broadcast_to([128, 8, 128]), ["cact"], ["c_rep"])
    wv = wada_d.rearrange("p (k n) -> p k n", k=8)
    dests = [sh1_r, sc1_r, gt1_bc, b2_bc, sc2_r, gt2_bc]
    dkeys = ["sh1_r", "sc1_r", "gt1_bc", "b2_bc", "sc2_r", "gt2_bc"]
    wcn = 0
    for hf_ in range(2):
        for kc in range(8):
            wb = wbuf[wcn % 2]
            wk = ("wbuf", wcn % 2)
            wcn += 1
            dma("sp", wb, wv[:, kc, hf_ * 3072:(hf_ + 1) * 3072], (), [wk])
            for nt in range(6):
                mm(banks[nt], c_rep[:, kc, :], wb[:, nt * 512:(nt + 1) * 512], kc == 0, kc == 7,
                   [wk, "c_rep"], [("ps", nt)])
        for nt in range(6):
            n0 = hf_ * 3072 + nt * 512
            di = n0 // 1024
            tt("dve", dests[di][:, n0 % 1024:n0 % 1024 + 512], banks[nt], bada_bc[:, n0:n0 + 512], ALU.add,
               [("ps", nt), "bada_bc"], [(dkeys[di], (n0 % 1024) // 512)])
    K2 = lambda nm: [(nm, 0), (nm, 1)]
    stt(sc1_r, sc1_r, 1.0, gmix_bc, ALU.add, ALU.mult, K2("sc1_r") + ["gmix_bc"], ["s1_r"])
    stt(s2_bc, sc2_r, 1.0, gffn_bc, ALU.add, ALU.mult, K2("sc2_r") + ["gffn_bc"], ["s2_bc"])
    for (row, rkeys, dst_fm, dk) in ((sc1_r, ["s1_r"], s1_fm, "s1"), (sh1_r, K2("sh1_r"), modfm, "modfm")):
        for kc in range(8):
            tt("dve", dtmp, row[:, kc * 128:(kc + 1) * 128], ident_f, ALU.mult, rkeys + ["ident_f"], ["dtmp"])
            sc.add("dve", lambda e, dst_fm=dst_fm, kc=kc: e.tensor_reduce(out=dst_fm[:, kc:kc + 1], in_=dtmp, axis=AX.X,
                                                                         op=ALU.add), ["dtmp"], [dk])

    rconst = A.alloc([2], F32)
    dma("sp", rconst, rconst_d, (), ["rconst"])
    HALF = 512
    posi = A.alloc([HALF], I32)
    ang = A.alloc([HALF], F32)
    halfpi = A.alloc([1], F32)
    memset("dve", halfpi, PI / 2, ["halfpi"])
    tmpS = [A.alloc([HALF], F32) for _ in range(4)]
    kiS = A.alloc([HALF], I32)
    for cb in range(8):
        dma("sp", posi[cb * 16:(cb + 1) * 16, :], pos_d[0:1, cb * 512:(cb + 1) * 512].to_broadcast([16, 512]), (),
            [("posi", cb)])
    POSI = [("posi", cb) for cb in range(8)]
    tcopy("dve", ang, posi, POSI, ["ang"])
    ts("dve", ang, ang, rconst[:, 0:1], None, ALU.mult, None, ["ang", "rconst"], ["ang"])
    t1, r0, mk, t2 = tmpS
    ts("dve", t1, ang, 1.0 / (2 * PI), None, ALU.mult, None, ["ang"], ["s_t1"])
    tcopy("dve", kiS, t1, ["s_t1"], ["s_ki"])
    stt(r0, kiS, -2 * PI, ang, ALU.mult, ALU.add, ["s_ki", "ang"], ["s_r0"])
    ts("dve", mk, r0, PI, -2 * PI, ALU.is_gt, ALU.mult, ["s_r0"], ["s_mk"])
    tt("dve", r0, r0, mk, ALU.add, ["s_r0", "s_mk"], ["s_r0"])
    ts("dve", mk, r0, -PI, 2 * PI, ALU.is_lt, ALU.mult, ["s_r0"], ["s_mk"])
    tt("dve", r0, r0, mk, ALU.add, ["s_r0", "s_mk"], ["s_r0"])
    ts("dve", r0, r0, PI, -PI, ALU.min, ALU.max, ["s_r0"], ["s_r0"])
    act(t1, r0, AF.Sin, ["s_r0"], ["s_t1"])
    dma("sp", tabsp_d, t1, ["s_t1"], ["tabsp"])
    act(t2, r0, AF.Sin, ["s_r0"], ["s_t2"], scale=-1.0)
    dma("sp", tabsn_d, t2, ["s_t2"], ["tabsn"])
    ts("dve", t1, ang, PI / 2, 1.0 / (2 * PI), ALU.add, ALU.mult, ["ang", "s_t1"], ["s_t1"])
    tcopy("dve", kiS, t1, ["s_t1"], ["s_ki"])
    stt(r0, kiS, -2 * PI, ang, ALU.mult, ALU.add, ["s_ki", "ang", "s_r0"], ["s_r0"])
    ts("dve", mk, r0, PI / 2, -2 * PI, ALU.is_gt, ALU.mult, ["s_r0"], ["s_mk"])
    tt("dve", r0, r0, mk, ALU.add, ["s_r0", "s_mk"], ["s_r0"])
    ts("dve", mk, r0, -1.5 * PI, 2 * PI, ALU.is_lt, ALU.mult, ["s_r0"], ["s_mk"])
    tt("dve", r0, r0, mk, ALU.add, ["s_r0", "s_mk"], ["s_r0"])
    ts("dve", r0, r0, PI / 2, -1.5 * PI, ALU.min, ALU.max, ["s_r0"], ["s_r0"])
    act(t1, r0, AF.Sin, ["s_r0", "halfpi"], ["s_t1"], bias=halfpi[:, 0:1])
    dma("sp", tabc_d, t1, ["s_t1"], ["tabc"])

    wtmp = A.alloc([2, 768], F32)
    wtmp2 = A.alloc([2, 768], F32)
    wtmp3 = A.alloc([512], F32)
    wtmp4 = A.alloc([512], F32)
    seltmp = A.alloc([96], F32)
    dma("sp", gq, gq_d, (), ["gq"])
    dma("sp", gkv, gkv_d, (), ["gkv"])
    dma("sp", wtmp, wuq_d.rearrange("p (k n) -> p k n", k=2), (), ["wtmp"])
    dma("sp", wtmp2, wuqs_d.rearrange("p (k n) -> p k n", k=2), (), ["wtmp2"])
    dma("sp", wtmp3, wuk_d, (), ["wtmp3"])
    dma("sp", wtmp4, wuv_d, (), ["wtmp4"])
    dma("sp", seltmp, sel_d, (), ["seltmp"])
    for kc in range(2):
        ts("dve", wuq_b[:, kc, :], wtmp[:, kc, :], gq[:, kc:kc + 1], None, ALU.mult, None, ["wtmp", "gq"], ["wuq_b"])
        ts("dve", wuqs_b[:, kc, :], wtmp2[:, kc, :], gq[:, kc:kc + 1], None, ALU.mult, None, ["wtmp2", "gq"], ["wuqs_b"])
    memset("dve", wukp_b, 0.0, ["wukp_b"])
    ts("dve", wukp_b[:, :, 0:64], wtmp3.rearrange("p (h d) -> p h d", d=64), gkv[:, 0:1], None, ALU.mult, None,
       ["wtmp3", "gkv", "wukp_b"], ["wukp_b"])
    ts("dve", wuv_b, wtmp4, gkv[:, 0:1], None, ALU.mult, None, ["wtmp4", "gkv"], ["wuv_b"])
    tcopy("dve", sel_b[0:96], seltmp[0:96], ["seltmp"], ["sel_b"])

    sc.barrier()
    A.release(p0)
    if stage <= 0:
        return finish(nc, es, sc, None)

    WIN = []

    xb = [A.alloc([D], F32) for _ in range(2)]
    junk = A.alloc([D], BF16)
    ssq = A.alloc([4], F32)
    rstd = A.alloc([4], F32)
    xn = [A.alloc([D], F32) for _ in range(2)]
    hT = [A.alloc([8, TT], BF16) for _ in range(2)]
    ctab = A.alloc([TT], F32)
    stab = A.alloc([TT], F32)
    qlat = A.alloc([2, TT], F32)
    qsq = A.alloc([2, TT], F32)
    kvlat = A.alloc([TT], F32)
    kvsq = A.alloc([TT], F32)
    rbc = [A.alloc([TT], F32) for _ in range(2)]
    qln = A.alloc([2, TT], BF16)
    kvn = A.alloc([TT], BF16)
    krr = A.alloc([TT], BF16)
    rt1 = [A.alloc([TT], F32) for _ in range(2)]
    rt2 = [A.alloc([TT], F32) for _ in range(2)]
    qT_s = A.alloc([8, TT], BF16)
    kT_s = A.alloc([8, TT], BF16)
    qbT_s = A.alloc([4, TT], BF16)
    kbT_s = A.alloc([4, TT], BF16)
    va_s = A.alloc([4, 520], BF16)
    vb_s = A.alloc([4, 520], BF16)
    gst = [A.alloc([TT], F32) for _ in range(4)]
    memset("dve", ctab[0:64], 1.0, ["ctab0"])
    memset("dve", stab[0:64], 0.0, ["stab0"])
    memset("dve", va_s, 1.0, ["va_s"])
    memset("dve", vb_s, 1.0, ["vb_s"])

    qT_v = qT_d.rearrange("p (h t) -> p h t", h=8)
    kT_v = kT_d.rearrange("p (h t) -> p h t", h=8)
    qbT_v = qbT_d.rearrange("p (h t) -> p h t", h=4)
    kbT_v = kbT_d.rearrange("p (h t) -> p h t", h=4)
    ga_v = ga_d.rearrange("p (c t) -> p c t", c=8)
    gb_v = gb_d.rearrange("p (c t) -> p c t", c=8)
    gcnt = [0]
    ecnt = [0]

    def evac_copy(out, in_, reads, writes):
        eng = "act" if ecnt[0] % 2 == 0 else "dve"
        ecnt[0] += 1
        tcopy(eng, out, in_, reads, writes)

    NT1 = NT if stage > 1 else 1

    def prep_stats(ti, bi):
        t0 = ti * TT
        g = ti * 4 + bi
        xt = xb[g % 2]
        xk = ("xb", g % 2)
        dma("sp", xt, x_d[t0 + bi * 128:t0 + (bi + 1) * 128, :], (), [xk])
        act(junk, xt, AF.Square, [xk], [("ssq", bi)], accum_out=ssq[:, bi:bi + 1])
        act(rstd[:, bi:bi + 1], ssq[:, bi:bi + 1], AF.Sqrt, [("ssq", bi), "eps_c"], [("rstd", bi)],
            scale=1.0 / D, bias=eps_c[:, 0:1])
        recip(rstd[:, bi:bi + 1], rstd[:, bi:bi + 1], [("rstd", bi)], [("rstd", bi)])
        ts("dve", xn[g % 2], xt, rstd[:, bi:bi + 1], None, ALU.mult, None, [xk, ("rstd", bi)], [("xn", g % 2)])

    def prep_tr(ti, bi):
        g = ti * 4 + bi
        xnt = xn[g % 2]
        nk = ("xn", g % 2)
        h_t = hT[ti % 2]
        for half in range(2):
            pb, pk = ps_next()
            for q in range(4):
                kc = half * 4 + q
                tr(pb[:, q * 128:(q + 1) * 128], xnt[:, kc * 128:(kc + 1) * 128], ident_f, [nk, "ident_f"], [pk])
            for q in range(4):
                kc = half * 4 + q
                dst = h_t[:, kc, bi * 128:(bi + 1) * 128]
                hk = ("hT", ti % 2, bi, q % 2)
                act(dst, pb[:, q * 128:(q + 1) * 128], AF.Identity, [pk, "s1", "modfm"], [hk],
                    scale=s1_fm[:, kc:kc + 1], bias=modfm[:, kc:kc + 1])

    def prep_all(ti):
        prep_stats(ti, 0)
        prep_stats(ti, 1)
        prep_tr(ti, 0)
        prep_stats(ti, 2)
        prep_tr(ti, 1)
        prep_stats(ti, 3)
        prep_tr(ti, 2)
        prep_tr(ti, 3)

    prep_all(0)
    for ti in range(NT1):
        t0 = ti * TT
        h_t = hT[ti % 2]
        HK = [("hT", ti % 2, bi_, q_) for bi_ in range(4) for q_ in range(2)]
        nxt = ti + 1 if ti + 1 < NT1 else None
        dma("sp", ctab[64:80], tabc_d[ti * 16:(ti + 1) * 16, :], (), ["ctab"])
        dma("sp", ctab[80:96], tabc_d[ti * 16:(ti + 1) * 16, :], (), ["ctab2"])
        dma("sp", stab[64:80], tabsn_d[ti * 16:(ti + 1) * 16, :], (), ["stab"])
        dma("sp", stab[80:96], tabsp_d[ti * 16:(ti + 1) * 16, :], (), ["stab2"])

        def proj(c0, m):
            pb, pk = ps_next()
            for kc in range(8):
                mm(pb[0:m, :], win_b[:, kc, c0:c0 + m], h_t[:, kc, :], kc == 0, kc == 7, HK + WIN, [pk])
            return pb, pk

        for c in range(2):
            pb, pk = proj(c * 128, 128)
            act(qlat[:, c, :], pb, AF.Copy, [pk], [("qlat", c)])
            act(qsq[:, c, :], pb, AF.Square, [pk], [("qsq", c)])
        pb, pk = proj(256, 128)
        act(kvlat, pb, AF.Copy, [pk], ["kvlat"])
        act(kvsq, pb, AF.Square, [pk], ["kvsq"])
        pa, pka = proj(C_KRP, 96)
        pbb, pkb = proj(C_KRS, 96)
        tt("dve", rt1[0][0:96], pa[0:96, :], ctab[0:96], ALU.mult, [pka, "ctab", "ctab2", "ctab0"], [("rt1", 0)])
        tt("dve", rt2[0][0:96], pbb[0:96, :], stab[0:96], ALU.mult, [pkb, "stab", "stab2", "stab0"], [("rt2", 0)])
        tt("pool", krr[0:96], rt1[0][0:96], rt2[0][0:96], ALU.add, [("rt1", 0), ("rt2", 0)], ["krr"])
        pq, pkq = ps_next()
        for c in range(2):
            mm(pq, ones_f, qsq[:, c, :], c == 0, c == 1, ["ones_f", ("qsq", c)], [pkq])
        act(rbc[0], pq, AF.Sqrt, [pkq, "eps_c"], [("rbc", 0)], scale=1.0 / 256, bias=eps_c[:, 0:1])
        recip(rbc[0], rbc[0], [("rbc", 0)], [("rbc", 0)])
        for c in range(2):
            tt("dve", qln[:, c, :], qlat[:, c, :], rbc[0], ALU.mult, [("qlat", c), ("rbc", 0)], [("qln", c)])
        pkv, pkkv = ps_next()
        mm(pkv, ones_f, kvsq, True, True, ["ones_f", "kvsq"], [pkkv])
        act(rbc[1], pkv, AF.Sqrt, [pkkv, "eps_c"], [("rbc", 1)], scale=1.0 / 128, bias=eps_c[:, 0:1])
        recip(rbc[1], rbc[1], [("rbc", 1)], [("rbc", 1)])
        tt("dve", kvn, kvlat, rbc[1], ALU.mult, ["kvlat", ("rbc", 1)], ["kvn"])
        if nxt is not None:
            prep_stats(nxt, 0)
            prep_stats(nxt, 1)
        for i in range(4):
            pb, pk = proj(C_QB + i * 128, 128)
            evac_copy(qbT_s[:, i, :], pb, [pk], ["qbT_s"])
        dma("sp", qbT_v[:, :, t0:t0 + TT], qbT_s, ["qbT_s"], [("qbT_d", ti)])
        for h in range(8):
            pa, pka = ps_next()
            for c in range(2):
                mm(pa[0:96, :], wuq_b[:, c, h * 96:(h + 1) * 96], qln[:, c, :], c == 0, c == 1,
                   ["wuq_b", ("qln", c)], [pka])
            pbb, pkb = ps_next()
            for c in range(2):
                mm(pbb[0:96, :], wuqs_b[:, c, h * 96:(h + 1) * 96], qln[:, c, :], c == 0, c == 1,
                   ["wuqs_b", ("qln", c)], [pkb])
            i2 = h % 2
            tt("dve", rt1[i2][0:96], pa[0:96, :], ctab[0:96], ALU.mult, [pka, "ctab", "ctab2", "ctab0"], [("rt1", i2)])
            tt("dve", rt2[i2][0:96], pbb[0:96, :], stab[0:96], ALU.mult, [pkb, "stab", "stab2", "stab0"], [("rt2", i2)])
            tt("pool", qT_s[0:96, h, :], rt1[i2][0:96], rt2[i2][0:96], ALU.add, [("rt1", i2), ("rt2", i2)], ["qT_s"])
            pk_, pkk = ps_next()
            mm(pk_[0:96, :], wukp_b[:, h, :], kvn, True, False, ["wukp_b", "kvn"], [pkk])
            mm(pk_[0:96, :], sel_b[0:96, :], krr[0:96, :], False, True, ["sel_b", "krr"], [pkk])
            evac_copy(kT_s[0:96, h, :], pk_[0:96, :], [pkk], ["kT_s"])
        for bi in range(4):
            pv, pkv_ = ps_next()
            mm(pv, kvn[:, bi * 128:(bi + 1) * 128], wuv_b, True, True, ["kvn", "wuv_b"], [pkv_])
            evac_copy(va_s[:, bi, :].rearrange("p (h d) -> p h d", d=65)[:, :, 0:64],
                      pv.rearrange("p (h d) -> p h d", d=64), [pkv_], ["va_s"])
        dma("sp", qT_v[:, :, t0:t0 + TT], qT_s[0:96], ["qT_s"], [("qT_d", ti)])
        dma("sp", kT_v[:, :, t0:t0 + TT], kT_s[0:96], ["kT_s"], [("kT_d", ti)])
        dma("sp", va_d[t0:t0 + TT, :].rearrange("(b p) f -> p b f", p=128), va_s, ["va_s"], [("va_d", ti)])
        if nxt is not None:
            prep_tr(nxt, 0)
            prep_stats(nxt, 2)
        for i in range(4):
            pb, pk = proj(C_KB + i * 128, 128)
            evac_copy(kbT_s[:, i, :], pb, [pk], ["kbT_s"])
        dma("sp", kbT_v[:, :, t0:t0 + TT], kbT_s, ["kbT_s"], [("kbT_d", ti)])
        if nxt is not None:
            prep_tr(nxt, 1)
            prep_stats(nxt, 3)
        for bi in range(4):
            pv, pkv_ = ps_next()
            for kc in range(8):
                mm(pv, h_t[:, kc, bi * 128:(bi + 1) * 128], win_b[:, kc, C_VB:C_VB + 512], kc == 0, kc == 7,
                   HK + WIN, [pkv_])
            evac_copy(vb_s[:, bi, :].rearrange("p (h d) -> p h d", d=65)[:, :, 0:64],
                      pv.rearrange("p (h d) -> p h d", d=64), [pkv_], ["vb_s"])
        dma("sp", vb_d[t0:t0 + TT, :].rearrange("(b p) f -> p b f", p=128), vb_s, ["vb_s"], [("vb_d", ti)])
        if nxt is not None:
            prep_tr(nxt, 2)
        for gix, (cbase, gv, nm) in enumerate(((C_GA, ga_v, "ga"), (C_GB, gb_v, "gb"))):
            for c in range(8):
                pb, pk = proj(cbase + c * 128, 128)
                gi = gcnt[0] % 4
                gcnt[0] += 1
                act(gst[gi], pb, AF.Sigmoid, [pk], [("gst", gi)])
                dma("sp", gv[:, c, t0:t0 + TT], gst[gi], [("gst", gi)], [(nm, ti, c)])
            if gix == 0 and nxt is not None:
                prep_tr(nxt, 3)

    sc.barrier()
    if stage <= 2:
        return finish(nc, es, sc, None)

    A.release(pP)
    o_a = A.alloc([NB, 512], BF16)
    o_b = A.alloc([NB, 512], BF16)
    p2 = A.mark()
    kT_r = A.alloc([8, S], BF16)
    va_r = A.alloc([NB, 520], BF16)
    qT_t = [A.alloc([8, TT], BF16) for _ in range(2)]
    E_t = [A.alloc([TT], BF16) for _ in range(6)]
    rden = [A.alloc([4], F32) for _ in range(2)]
    for h in range(8):
        dma("sp", kT_r[0:96, h, :], kT_v[:, h, :], (), [("kT_r", h)])
    va_v = va_d.rearrange("(b p) f -> p b f", p=128)
    for q4 in range(4):
        dma("sp", va_r[:, q4 * 8:(q4 + 1) * 8, :], va_v[:, q4 * 8:(q4 + 1) * 8, :], (), [("va_r", q4)])
    SCALE_A = 96 ** -0.5
    sbank = [0]
    ecnt2 = [0]
    accn = [0]
    NQT = NT if stage > 3 else 2
    LOOK = 3
    stageA, stageB = [], []
    for qt in range(NQT):
        for h in range(8):
            nkt = 4 * qt + 4
            for kt in range(nkt):
                stageA.append((qt, h, kt))
    grp = {}

    def emitA(rec):
        qt, h, kt = rec
        qtt = qT_t[qt % 2]
        qk = ("qT_t", qt % 2)
        if h == 0 and kt == 0:
            dma("sp", qtt[0:96], qT_v[:, :, qt * TT:(qt + 1) * TT], (), [qk])
        r = kt - 4 * qt
        c0 = 128 * r if r > 0 else 0
        sb = sbank[0] % 5
        sbank[0] += 1
        ps_, psk = banks[sb], ("ps", sb)
        mm(ps_[:, c0:TT], kT_r[0:96, h, kt * 128:(kt + 1) * 128], qtt[0:96, h, c0:TT], True, True,
           [("kT_r", h), qk], [psk])
        ei = ecnt2[0] % len(E_t)
        ecnt2[0] += 1
        Et, Ek = E_t[ei], ("E", ei)
        act(Et[:, c0:TT], ps_[:, c0:TT], AF.Exp, [psk], [Ek], scale=SCALE_A)
        if r >= 0:
            memset("dve", Et[64:128, c0:c0 + 64], 0.0, [Ek])
        grp[rec] = (Et, Ek)

    def emitB(rec):
        qt, h, kt = rec
        nkt = 4 * qt + 4
        r = kt - 4 * qt
        if kt == 0:
            ab = 5 + accn[0] % 2
            accn[0] += 1
            grp["acc"] = (banks[ab], ("ps", ab))
        acc, acck = grp["acc"]
        Et, Ek = grp.pop(rec)
        for qb in range(max(r, 0), 4):
            first = (kt == 0) and (qb == 0)
            last = (kt == nkt - 1) and (qb == 3)
            mm(acc[:, qb * 65:(qb + 1) * 65], Et[:, qb * 128:(qb + 1) * 128],
               va_r[:, kt, h * 65:(h + 1) * 65], first, last, [Ek, ("va_r", kt // 8)], [acck])
        if kt == nkt - 1:
            rd = rden[h % 2]
            rk = ("rden", h % 2)
            accv = acc[:, 0:260].rearrange("p (b d) -> p b d", d=65)
            recip(rd, accv[:, :, 64], [acck], [rk])
            tt("dve", o_a[:, qt * 4:(qt + 1) * 4, h * 64:(h + 1) * 64], accv[:, :, 0:64],
               rd[:, :, None].broadcast_to([128, 4, 64]), ALU.mult, [acck, rk], [("o_a", qt)])

    for i in range(len(stageA) + LOOK):
        if i % 8 == 3:
            precast_step()
        if i < len(stageA):
            emitA(stageA[i])
        if i >= LOOK:
            emitB(stageA[i - LOOK])
    if dbg:
        oa_dbg = dscr("oa_dbg", [S, 512], BF16)
        dma("sp", oa_dbg.rearrange("(b p) f -> p b f", p=128), o_a, [("o_a", q) for q in range(NQT)], ["oa_dbg"])
    sc.barrier()
    if stage <= 4:
        return finish(nc, es, sc, None)

    A.release(p2)
    qb_p = [A.alloc([S], BF16) for _ in range(2)]
    kb_p = [A.alloc([S], BF16) for _ in range(2)]
    vb_r = A.alloc([NB, 520], BF16)
    bias_r = A.alloc([8, 640], F32)
    Eb = [A.alloc([640], BF16) for _ in range(10)]
    stmp = [A.alloc([640], F32) for _ in range(4)]
    rden3 = [A.alloc([4], F32) for _ in range(2)]
    vb_v = vb_d.rearrange("(b p) f -> p b f", p=128)
    for q4 in range(4):
        dma("sp", vb_r[:, q4 * 8:(q4 + 1) * 8, :], vb_v[:, q4 * 8:(q4 + 1) * 8, :], (), [("vb_r", q4)])
    dma("sp", bias_r, biasT_d.rearrange("p (h q) -> p h q", h=8), (), ["bias_r"])
    for h_ in range(8):
        act(bias_r[:, h_, :], bias_r[:, h_, :], AF.Exp, ["bias_r"], ["bias_r"])
    sbank[0] = 0
    ecnt3 = 0
    stc = 0
    NJ = NB if stage > 5 else 8
    recs3 = [(h, j) for h in range(8) for j in range(NJ)]
    ering = {}
    st3 = dict(ecnt=0, stc=0, bs=0)

    def emitA3(rec):
        h, j = rec
        pr, po = h // 2, (h % 2) * 64
        qb_r, kb_r = qb_p[pr % 2], kb_p[pr % 2]
        if h % 2 == 0 and j == 0:
            dma("sp", qb_r, qbT_v[:, pr, :], (), [("qb_r", pr % 2)])
            dma("sp", kb_r, kbT_v[:, pr, :], (), [("kb_r", pr % 2)])
        nq = min(640, S - 128 * j)
        n1 = min(nq, 512)
        sb = sbank[0] % 4
        sbank[0] += 1
        psA, pkA = banks[sb], ("ps", sb)
        st_ = stmp[st3["stc"] % 4]
        stk = ("stmp", st3["stc"] % 4)
        st3["stc"] += 1
        mm(psA[:, 0:n1], kb_r[po:po + 64, 128 * j:128 * j + 128], qb_r[po:po + 64, 128 * j:128 * j + n1],
           True, True, [("kb_r", pr % 2), ("qb_r", pr % 2)], [pkA])
        act(st_[:, 0:n1], psA[:, 0:n1], AF.Exp, [pkA], [(stk, 0)], scale=0.125)
        if nq > 512:
            bslot = (4, 7)[st3["bs"] % 2]
            st3["bs"] += 1
            psB, pkB = banks[bslot], ("ps", bslot)
            mm(psB[:, 0:nq - 512], kb_r[po:po + 64, 128 * j:128 * j + 128],
               qb_r[po:po + 64, 128 * j + 512:128 * j + nq], True, True, [("kb_r", pr % 2), ("qb_r", pr % 2)], [pkB])
            act(st_[:, 512:nq], psB[:, 0:nq - 512], AF.Exp, [pkB], [(stk, 1)], scale=0.125)
        ei = st3["ecnt"] % len(Eb)
        st3["ecnt"] += 1
        Et, Ek = Eb[ei], ("Eb", ei)
        tt("dve" if st3["ecnt"] % 2 == 0 else "pool", Et[:, 0:nq], st_[:, 0:nq], bias_r[:, h, 0:nq], ALU.mult,
           [(stk, 0), (stk, 1), "bias_r"], [Ek])
        ering[(h, j)] = (Et, Ek)

    def emitB3(rec):
        h, j = rec
        if j % 4 == 0:
            ab = 5 + accn[0] % 2
            accn[0] += 1
            ering["acc"] = (banks[ab], ("ps", ab))
        acc, acck = ering["acc"]
        jj0 = max(0, j - 4)
        for jj in range(jj0, j + 1):
            Ej, Ejk = ering[(h, jj)]
            off = (j - jj) * 128
            mm(acc[:, (j % 4) * 65:(j % 4 + 1) * 65], Ej[:, off:off + 128], vb_r[:, jj, h * 65:(h + 1) * 65],
               (j % 4 == 0) and (jj == jj0), (j % 4 == 3) and (jj == j), [Ejk, ("vb_r", jj // 8)], [acck])
        if j % 4 == 3:
            rd = rden3[(j // 4) % 2]
            rk = ("rden3", (j // 4) % 2)
            accv = acc[:, 0:260].rearrange("p (b d) -> p b d", d=65)
            recip(rd, accv[:, :, 64], [acck], [rk])
            tt("dve", o_b[:, j - 3:j + 1, h * 64:(h + 1) * 64], accv[:, :, 0:64],
               rd[:, :, None].broadcast_to([128, 4, 64]), ALU.mult, [acck, rk], [("o_b", j // 4)])

    LOOK3 = 3
    for i in range(len(recs3) + LOOK3):
        if i % 5 == 2:
            precast_step()
        if i < len(recs3):
            emitA3(recs3[i])
        if i >= LOOK3:
            emitB3(recs3[i - LOOK3])
    if dbg:
        ob_dbg = dscr("ob_dbg", [S, 512], BF16)
        dma("sp", ob_dbg.rearrange("(b p) f -> p b f", p=128), o_b, [("o_b", q) for q in range(NJ // 4)], ["ob_dbg"])
    sc.barrier()
    if stage <= 6:
        return finish(nc, es, sc, None)

    A.release(p2)
    cw_all = A.alloc([NB, 2], F32)
    e_all = A.alloc([NB, 2], F32)
    r_all = A.alloc([NB, 2], F32)
    A1all = A.alloc([NB, 32], F32)
    A2all = A.alloc([NB, 32], F32)
    carry = A.alloc([32], F32)
    iota_e = A.alloc([32], F32)
    rbias = A.alloc([36], F32)
    Lst = A.alloc([128], BF16)
    p4 = A.mark()
    woa_b = A.alloc([4, D], BF16)
    wob_b = A.alloc([4, D], BF16)
    wout_b = A.alloc([8, D], BF16)
    wr_f = A.alloc([8, 36], F32)
    for c in range(4):
        dma("pool", woa_b[:, c, :], woa_d.rearrange("p (k n) -> p k n", k=4)[:, c, :], (), [("woa", c)])
        dma("pool", wob_b[:, c, :], wob_d.rearrange("p (k n) -> p k n", k=4)[:, c, :], (), [("wob", c)])
    WOA = [("woa", c) for c in range(4)]
    WOB = [("wob", c) for c in range(4)]
    WOUT = [("wout", c) for c in range(8)]
    dma("sp", wr_f, wr_d.rearrange("p (k n) -> p k n", k=8), (), ["wr_f"])
    dma("sp", rbias, rb_d[0:1, :].to_broadcast([128, 36]), (), ["rbias"])
    dma("sp", iota_e, iota_d, (), ["iota_e"])
    ltmp = A.alloc([128], F32)
    dma("sp", ltmp, lst_d, (), ["ltmp"])
    tcopy("dve", Lst, ltmp, ["ltmp"], ["Lst"])
    memset("dve", carry, 0.0, ["carry"])

    oaT2 = [A.alloc([4, TT], BF16) for _ in range(2)]
    obT2 = [A.alloc([4, TT], BF16) for _ in range(2)]
    mT = A.alloc([8, TT], BF16)
    gat = [A.alloc([TT], F32) for _ in range(2)]
    gbt = [A.alloc([TT], F32) for _ in range(2)]
    mt1 = [A.alloc([TT], F32) for _ in range(2)]
    mt2 = [A.alloc([TT], F32) for _ in range(2)]
    R4 = 4
    RX = 3
    xr = [A.alloc([D], F32) for _ in range(RX)]
    uu = [A.alloc([D], F32) for _ in range(R4)]
    h2b = [A.alloc([D], BF16)] * 2
    h2T = [A.alloc([8, 128], F32)] * 2
    sm = A.alloc([16], F32)
    lg = A.alloc([4, 36], F32)
    dg = A.alloc([4, 4], F32)
    ge = A.alloc([4, 4], F32)
    pen = A.alloc([4, 4], F32)
    msk = A.alloc([4, 32], F32)
    top8a = A.alloc([4, 8], F32)
    idx8a = A.alloc([4, 8], U32)
    r4s = A.alloc([8, 4], F32)
    Ab = A.alloc([4, 32], BF16)
    Pt = A.alloc([4, 32], F32)
    ptm = A.alloc([4, 32], F32)
    woutv = wout_d.rearrange("p (k n) -> p k n", k=8)
    for c in range(8):
        stb = xr[c % RX]
        stk_ = [("x1", c % RX, 0), ("x1", c % RX, 1)]
        dma("sp", stb, woutv[:, c, :], (), stk_)
        tt("dve", wout_b[:, c, :], stb, gt1_bc, ALU.mult, stk_, [("wout", c)])
    RB = 6
    rot = [0]

    def ps_rot():
        i = rot[0] % RB
        rot[0] += 1
        return banks[i], ("ps", i)

    pp, pkp = banks[6], ("ps", 6)
    rl, pkr = banks[7], ("ps", 7)
    NT4 = NT if stage > 7 else 1

    def S1(ti, bi):
        blk = ti * 4 + bi
        t0 = ti * TT
        r3 = blk % R4
        rx = blk % RX
        g2 = blk % 2
        xk = [("x1", rx, 0), ("x1", rx, 1)]
        dma("sp", xr[rx], x_d[t0 + bi * 128:t0 + (bi + 1) * 128, :], (), xk)
        for half in range(2):
            pm, pkm = ps_rot()
            for m in range(8):
                mm(pm, mT[:, m, bi * 128:(bi + 1) * 128], wout_b[:, m, half * 512:(half + 1) * 512],
                   m == 0, m == 7, [("mT", m_) for m_ in range(8)] + WOUT, [pkm])
            hs = slice(half * 512, (half + 1) * 512)
            tt("dve", xr[rx][:, hs], pm, xr[rx][:, hs], ALU.add, [pkm, ("x1", rx, half)], [("x1", rx, half)])
        dma("sp", x1_d[t0 + bi * 128:t0 + (bi + 1) * 128, :], xr[rx], xk, [("x1_d", blk)])
        act(h2b[g2], xr[rx], AF.Square, xk, [("ssq4", g2), ("h2b", 0)], accum_out=sm[:, g2:g2 + 1])
        act(sm[:, 2 + g2:3 + g2], sm[:, g2:g2 + 1], AF.Ln, [("ssq4", g2), "eps_c"], [("rs4", g2)],
            scale=1.0 / D, bias=eps_c[:, 0:1])
        act(sm[:, 4 + g2:5 + g2], sm[:, 2 + g2:3 + g2], AF.Exp, [("rs4", g2)], [("rs4b", g2)], scale=-0.5)
        tt("pool", uu[r3], xr[rx], s2_bc, ALU.mult, xk, [("uu", r3)])
        stt(uu[r3], uu[r3], sm[:, 4 + g2:5 + g2], b2_bc, ALU.mult, ALU.add, [("uu", r3), ("rs4b", g2)], [("uu", r3)])
        tcopy("act", h2b[g2], uu[r3], [("uu", r3), ("ssq4", g2)], [("h2b", 0)])
        dma("sp", h2_d[t0 + bi * 128:t0 + (bi + 1) * 128, :], h2b[g2], [("h2b", 0)], [("h2_d", blk)])

    def S2a(ti, bi):
        blk = ti * 4 + bi
        r3 = blk % R4
        hb = blk % 2
        for half in range(2):
            pb, pk = ps_rot()
            for q in range(4):
                kc = half * 4 + q
                tr(pb[:, q * 128:(q + 1) * 128], uu[r3][:, kc * 128:(kc + 1) * 128], ident_f, [("uu", r3)], [pk])
            evac_copy(h2T[hb][:, half * 4:(half + 1) * 4, :], pb.rearrange("p (q t) -> p q t", q=4), [pk],
                      [("h2T", 0, half)])
        for kc in range(8):
            mm(rl[:, bi * 36:(bi + 1) * 36], h2T[hb][:, kc, :], wr_f[:, kc, :], (bi == 0) and (kc == 0),
               (bi == 3) and (kc == 7), [("h2T", 0, kc // 4), "wr_f"], [pkr])

    def dve(fn, reads, writes):
        sc.add("dve", fn, reads, writes)

    def S2b1(ti):
        b0 = ti * 4
        bs = slice(b0, b0 + 4)
        tt("dve", lg, rl[:, 0:144].rearrange("p (b n) -> p b n", n=36), rbias[:, None, :].broadcast_to([128, 4, 36]),
           ALU.add, [pkr, "rbias"], ["lg"])
        gmax = r4s[:, 0, :]
        dve(lambda e: e.tensor_reduce(out=gmax, in_=lg[:, :, 0:4], axis=AX.X, op=ALU.max), ["lg"], ["gmax"])
        tt("dve", dg, lg[:, :, 0:4], gmax[:, :, None].broadcast_to([128, 4, 4]), ALU.subtract, ["lg", "gmax"], ["dg"])
        act(ge, dg, AF.Exp, ["dg"], ["ge"])
        gsum = r4s[:, 1, :]
        dve(lambda e: e.tensor_reduce(out=gsum, in_=ge, axis=AX.X, op=ALU.add), ["ge"], ["gsum"])
        gw = r4s[:, 2, :]
        recip(gw, gsum, ["gsum"], ["gw"])
        ts("dve", pen, dg, 0.0, None, ALU.is_equal, None, ["dg"], ["pen"])
        ts("dve", pen, pen, -1.0, 1e30, ALU.add, ALU.mult, ["pen"], ["pen"])
        tt("dve", msk.rearrange("p b (g e) -> p b g e", e=8), lg[:, :, 4:36].rearrange("p b (g e) -> p b g e", e=8),
           pen[:, :, :, None].broadcast_to([128, 4, 4, 8]), ALU.add, ["lg", "pen"], ["msk"])
        for b in range(4):
            dve(lambda e, b=b: e.max(out=top8a[:, b, :], in_=msk[:, b, :]), ["msk"], [("top8", b)])
            dve(lambda e, b=b: e.max_index(out=idx8a[:, b, :], in_max=top8a[:, b, :], in_values=msk[:, b, :]),
                ["msk", ("top8", b)], [("idx8", b)])
        T8 = [("top8", b) for b in range(4)]
        I8 = [("idx8", b) for b in range(4)]
        tcopy("dve", e_all[:, bs, :], idx8a[:, :, 0:2], I8, [("e_all", ti)])
        dlt = r4s[:, 3, :]
        tt("dve", dlt, top8a[:, :, 1], top8a[:, :, 0], ALU.subtract, T8, ["dlt"])
        ex_ = r4s[:, 4, :]
        act(ex_, dlt, AF.Exp, ["dlt"], ["ex_"])
        den = r4s[:, 5, :]
        ts("dve", den, ex_, 1.0, None, ALU.add, None, ["ex_"], ["den"])
        recip(den, den, ["den"], ["den"])
        tt("dve", cw_all[:, bs, 0], den, gw, ALU.mult, ["den", "gw"], [("cw", ti, 0)])
        tt("dve", ex_, ex_, den, ALU.mult, ["ex_", "den"], ["ex_"])
        tt("dve", cw_all[:, bs, 1], ex_, gw, ALU.mult, ["ex_", "gw"], [("cw", ti, 1)])
        iob = iota_e[:, None, :].broadcast_to([128, 4, 32])
        tt("dve", A1all[:, bs, :], iob, e_all[:, bs, 0:1].broadcast_to([128, 4, 32]), ALU.is_equal,
           ["iota_e", ("e_all", ti)], [("A1", ti)])
        tt("dve", A2all[:, bs, :], iob, e_all[:, bs, 1:2].broadcast_to([128, 4, 32]), ALU.is_equal,
           ["iota_e", ("e_all", ti)], [("A2", ti)])
        tt("dve", Ab, A1all[:, bs, :], A2all[:, bs, :], ALU.add, [("A1", ti), ("A2", ti)], ["Ab"])

    def S2b2(ti):
        b0 = ti * 4
        bs = slice(b0, b0 + 4)
        n_mm = 0
        tot_mm = 4 + 6 + 4
        for b in range(4):
            mm(pp[:, b * 32:(b + 1) * 32], Lst, Ab[:, b, :], n_mm == 0, False, ["Lst", "Ab"], [pkp])
            n_mm += 1
            for b_ in range(b):
                mm(pp[:, b * 32:(b + 1) * 32], ones_b, Ab[:, b_, :], False, False, ["ones_b", "Ab"], [pkp])
                n_mm += 1
        for b in range(4):
            n_mm += 1
            mm(pp[:, 128:160], ones_b, Ab[:, b, :], False, n_mm == tot_mm, ["ones_b", "Ab"], [pkp])
        tt("dve", Pt, pp[:, 0:128].rearrange("p (b n) -> p b n", n=32), carry[:, None, :].broadcast_to([128, 4, 32]),
           ALU.add, [pkp, "carry"], ["Pt"])
        tt("dve", carry, pp[:, 128:160], carry, ALU.add, [pkp, "carry", "Pt"], ["carry"])
        tt("dve", ptm, Pt, A1all[:, bs, :], ALU.mult, ["Pt", ("A1", ti)], ["ptm"])
        dve(lambda e: e.tensor_reduce(out=r_all[:, bs, 0], in_=ptm, axis=AX.X, op=ALU.add), ["ptm"], [("r_all", ti, 0)])
        tt("dve", ptm, Pt, A2all[:, bs, :], ALU.mult, ["Pt", ("A2", ti), ("r_all", ti, 0)], ["ptm"])
        dve(lambda e: e.tensor_reduce(out=r_all[:, bs, 1], in_=ptm, axis=AX.X, op=ALU.add), ["ptm"], [("r_all", ti, 1)])

    gcn4 = [0]

    def otrans(ti):
        ob2 = ti % 2
        for (osrc, odst, onm) in ((o_a, oaT2[ob2], "oaT"), (o_b, obT2[ob2], "obT")):
            for c in range(4):
                pb, pk = ps_rot()
                pbb = pb.bitcast(BF16)
                for bi in range(4):
                    tr(pbb[:, bi * 128:(bi + 1) * 128], osrc[:, ti * 4 + bi, c * 128:(c + 1) * 128], ident_b,
                       [("o_x",), "ident_b"], [pk])
                evac_copy(odst[:, c, :], pbb[:, 0:TT], [pk], [(onm, ob2, c)])

    def projmerge(ti):
        t0 = ti * TT
        ob2 = ti % 2
        oaT, obT = oaT2[ob2], obT2[ob2]
        for m in range(8):
            gi = gcn4[0] % 2
            gcn4[0] += 1
            dma("sp", gat[gi], ga_v[:, m, t0:t0 + TT], (), [("gat", gi)])
            dma("sp", gbt[gi], gb_v[:, m, t0:t0 + TT], (), [("gbt", gi)])
            pa, pka = ps_rot()
            for c in range(4):
                mm(pa, woa_b[:, c, m * 128:(m + 1) * 128], oaT[:, c, :], c == 0, c == 3, WOA + [("oaT", ob2, c)], [pka])
            pbk, pkb = ps_rot()
            for c in range(4):
                mm(pbk, wob_b[:, c, m * 128:(m + 1) * 128], obT[:, c, :], c == 0, c == 3, WOB + [("obT", ob2, c)], [pkb])
            i2 = m % 2
            tt("dve", mt1[i2], pa, gat[gi], ALU.mult, [pka, ("gat", gi)], [("mt1", i2)])
            tt("dve", mt2[i2], pbk, gbt[gi], ALU.mult, [pkb, ("gbt", gi)], [("mt2", i2)])
            tt("pool", mT[:, m, :], mt1[i2], mt2[i2], ALU.add, [("mt1", i2), ("mt2", i2)], [("mT", m)])

    otrans(0)
    for ti in range(NT4):
        projmerge(ti)
        if ti + 1 < NT4:
            otrans(ti + 1)
        if ti >= 1:
            S2b2(ti - 1)
        for bi in range(4):
            S1(ti, bi)
        for bi in range(4):
            S2a(ti, bi)
        S2b1(ti)
    S2b2(NT4 - 1)
    if dbg:
        rt_dbg = dscr("rt_dbg", [128, NB * 6], F32)
        rv = rt_dbg.rearrange("p (b s) -> p b s", s=6)
        ALLK = [("cw", t_, k_) for t_ in range(NT4) for k_ in range(2)] + [("e_all", t_) for t_ in range(NT4)] + [("r_all", t_, k_) for t_ in range(NT4) for k_ in range(2)]
        dma("sp", rv[:, :, 0:2], cw_all, ALLK, ["rt1"])
        dma("sp", rv[:, :, 2:4], e_all, ALLK, ["rt2"])
        dma("sp", rv[:, :, 4:6], r_all, ALLK, ["rt3"])
    sc.barrier()
    if stage <= 8:
        return finish(nc, es, sc, None)

    A.release(p4)
    padf = A.alloc([32], F32)
    padi = A.alloc([32], I32)
    incl = A.alloc([32], F32)
    offs = A.alloc([32], F32)
    ones32 = A.alloc([32], F32)
    big3 = A.alloc([NTILE, 32], F32)
    slotf = A.alloc([NB, 2], F32)
    slot_i = A.alloc([NB * 2], I32)
    jv = A.alloc([NTILE], F32)
    pidx = A.alloc([1], F32)
    tef = A.alloc([NTILE], F32)
    tesh = A.alloc([NTILE], F32)
    widx_i = A.alloc([NTILE], I32)
    p5 = A.mark()
    dma("sp", jv, jv_d, (), ["jv"])
    dma("sp", pidx, pidx_d, (), ["pidx"])
    memset("dve", ones32, 1.0, ["ones32"])
    ts("dve", padf, carry, float(SLOT_T - 1), None, ALU.add, None, ["carry"], ["padf"])
    tcopy("dve", padi, padf, ["padf"], ["padi"])
    ts("dve", padi, padi, SH, None, ALU.arith_shift_right, None, ["padi"], ["padi"])
    ts("dve", padi, padi, SH, None, ALU.logical_shift_left, None, ["padi"], ["padi"])
    tcopy("dve", padf, padi, ["padi"], ["padf"])
    sc.add("dve", lambda e: e.tensor_tensor_scan(out=incl, data0=ones32, data1=padf, initial=0.0,
                                                  op0=ALU.mult, op1=ALU.add), ["ones32", "padf"], ["incl"])
    tt("dve", offs, incl, padf, ALU.subtract, ["incl", "padf"], ["offs"])
    for k, Aall in ((0, A1all), (1, A2all)):
        tt("dve", big3[:, 0:NB, :], Aall, offs[:, None, :].broadcast_to([128, NB, 32]), ALU.mult, ["offs"], ["big3"])
        sc.add("dve", lambda e, k=k: e.tensor_reduce(out=slotf[:, :, k], in_=big3[:, 0:NB, :], axis=AX.X, op=ALU.add),
               ["big3"], [("slotf", k)])
    tt("dve", slotf, slotf, r_all, ALU.add, [("slotf", 0), ("slotf", 1)], ["slotf"])
    tcopy("dve", slot_i, slotf.rearrange("p b k -> p (b k)"), ["slotf"], ["slot_i"])
    tt("dve", big3, incl[:, None, :].broadcast_to([128, NTILE, 32]), jv[:, :, None].broadcast_to([128, NTILE, 32]),
       ALU.is_le, ["incl", "jv", ("slotf", 0), ("slotf", 1)], ["big3"])
    sc.add("dve", lambda e: e.tensor_reduce(out=tef, in_=big3, axis=AX.X, op=ALU.add), ["big3"], ["tef"])
    ts("dve", tef, tef, 31.0, None, ALU.min, None, ["tef"], ["tef"])
    memset("dve", tesh, -1.0, ["tesh"])
    tcopy("dve", tesh[:, 3:NTILE], tef[:, 0:NTILE - 3], ["tef", "tesh"], ["tesh"])
    tt("dve", tesh, tesh, tef, ALU.is_equal, ["tesh", "tef"], ["tesh"])
    ts("dve", tef, tef, 128.0, pidx[:, 0:1], ALU.mult, ALU.add, ["tef", "pidx"], ["tef"])
    stt(tef, tesh, 100000.0, tef, ALU.mult, ALU.add, ["tesh", "tef"], ["tef"])
    tcopy("dve", widx_i, tef, ["tef"], ["widx_i"])
    if dbg:
        sl_dbg = dscr("sl_dbg", [128, NB * 2 + NTILE], I32)
        dma("sp", sl_dbg[:, 0:NB * 2], slot_i, ["slot_i"], ["sl1"])
        dma("sp", sl_dbg[:, NB * 2:], widx_i, ["widx_i"], ["sl2"])
    hrow = [A.alloc([D], BF16) for _ in range(3)]
    for blk in range(NB):
        hr = hrow[blk % 3]
        dma("sp", hr, h2_d[blk * 128:(blk + 1) * 128, :], (), [("hrow", blk % 3)])
        for k in range(2):
            off_ap = slot_i[:, blk * 2 + k:blk * 2 + k + 1]
            sc.add("pool", lambda e, hr=hr, off_ap=off_ap: e.indirect_dma_start(
                out=xs_d[:, :], out_offset=bass.IndirectOffsetOnAxis(ap=off_ap, axis=0), in_=hr, in_offset=None),
                [("hrow", blk % 3), "slot_i"], [("xs_d", blk, k)], dma=True)
    sc.barrier()
    if stage <= 9:
        return finish(nc, es, sc, None)

    A.release(p5)
    while pre_pos[0] < len(pre_steps):
        precast_step()
    NW = 3
    wall_t = [A.alloc([6144], BF16) for _ in range(NW)]
    wg_t = [w[:, 0:2048] for w in wall_t]
    wu_t = [w[:, 2048:4096] for w in wall_t]
    wd_t = [w[:, 4096:6144] for w in wall_t]
    xs_t = [A.alloc([2, D], BF16) for _ in range(NW)]
    xsT = [A.alloc([8, SLOT_T], BF16) for _ in range(2)]
    sil = [A.alloc([2, SLOT_T], F32) for _ in range(2)]
    hidT = [A.alloc([2, SLOT_T], BF16) for _ in range(2)]
    yt = [A.alloc([D], F32) for _ in range(3)]
    ycn = 0
    breg = {}
    NTL = NTILE if stage > 10 else 4
    WXALL = [("wx", mi, e_) for mi in range(3) for e_ in range(32)]

    def prefetch6(j):
        b3 = j % NW
        wi = widx_i[:, j:j + 1]

        def wgather(e, wi=wi, b3=b3):
            if "r" not in breg:
                breg["r"] = e.alloc_register("wbound")
                e.reg_mov(breg["r"], 32 * 128 - 1)
            return e.indirect_dma_start(
                out=wall_t[b3], out_offset=None, in_=wx_d[:, :], in_offset=bass.IndirectOffsetOnAxis(ap=wi, axis=0),
                bounds_check=breg["r"], oob_is_err=False)
        sc.add("pool", wgather, (), [("wg", b3), ("wu", b3), ("wd", b3)], dma=True)
        dma("sp", xs_t[b3], xs_d[j * SLOT_T:(j + 1) * SLOT_T, :].rearrange("(s p) f -> p s f", p=128), (), [("xs_t", b3)])

    for j in range(min(2, NTL)):
        prefetch6(j)
    for j in range(NTL):
        if j + 2 < NTL:
            prefetch6(j + 2)
        b2 = j % 2
        b3 = j % NW
        for sbk in range(2):
            for half in range(2):
                pb, pk = ps_next()
                pbb = pb.bitcast(BF16)
                for q in range(4):
                    kc = half * 4 + q
                    tr(pbb[:, q * 128:(q + 1) * 128], xs_t[b3][:, sbk, kc * 128:(kc + 1) * 128], ident_b,
                       [("xs_t", b3), "ident_b"], [pk])
                evac_copy(xsT[b2][:, half * 4:(half + 1) * 4, sbk * 128:(sbk + 1) * 128],
                          pbb[:, 0:512].rearrange("p (q t) -> p q t", q=4), [pk], [("xsT", b2, sbk, half)])
        XST = [("xsT", b2, a, b) for a in range(2) for b in range(2)]
        pg, pkg = ps_next()
        for f in range(2):
            for kc in range(8):
                mm(pg[:, f * SLOT_T:(f + 1) * SLOT_T], wg_t[b3][:, kc * 256 + f * 128:kc * 256 + (f + 1) * 128],
                   xsT[b2][:, kc, :], kc == 0, kc == 7, [("wg", b3)] + XST, [pkg])
        pu, pku = ps_next()
        for f in range(2):
            for kc in range(8):
                mm(pu[:, f * SLOT_T:(f + 1) * SLOT_T], wu_t[b3][:, kc * 256 + f * 128:kc * 256 + (f + 1) * 128],
                   xsT[b2][:, kc, :], kc == 0, kc == 7, [("wu", b3)] + XST, [pku])
        act(sil[b2].rearrange("p f s -> p (f s)"), pg, AF.Silu, [pkg], [("sil", b2)])
        tt("dve", hidT[b2].rearrange("p f s -> p (f s)"), pu, sil[b2].rearrange("p f s -> p (f s)"), ALU.mult,
           [pku, ("sil", b2)], [("hidT", b2)])
        for sbk in range(2):
            y_ = yt[ycn % 3]
            yk = ("yt", ycn % 3)
            ycn += 1
            for half in range(2):
                py, pky = ps_next()
                for f in range(2):
                    mm(py, hidT[b2][:, f, sbk * 128:(sbk + 1) * 128], wd_t[b3][:, f * 1024 + half * 512:f * 1024 + (half + 1) * 512],
                       f == 0, f == 1, [("hidT", b2), ("wd", b3)], [pky])
                tt("dve", y_[:, half * 512:(half + 1) * 512], py, gt2_bc[:, half * 512:(half + 1) * 512], ALU.mult,
                   [pky], [(yk, half)])
            dma("sp", ys_d[j * SLOT_T + sbk * 128:j * SLOT_T + (sbk + 1) * 128, :], y_, [(yk, 0), (yk, 1)], [("ys_d", j, sbk)])
    sc.barrier()
    if stage <= 11:
        return finish(nc, es, sc, None)

    A.release(p5)
    gfin = A.alloc([D], F32)
    dma("sp", gfin, gfin_d[0:1, :].to_broadcast([128, D]), (), ["gfin"])
    R7 = 4
    g1 = [A.alloc([D], F32) for _ in range(R7)]
    g2_ = [A.alloc([D], F32) for _ in range(R7)]
    x1r = [A.alloc([D], F32) for _ in range(R7)]
    ot = [A.alloc([D], F32) for _ in range(2)]
    junk7 = A.alloc([D], BF16)
    sm7 = A.alloc([8], F32)

    def load7(blk):
        i4 = blk % R7
        for k, gt_ in ((0, g1), (1, g2_)):
            off_ap = slot_i[:, blk * 2 + k:blk * 2 + k + 1]
            sc.add("pool", lambda e, gt_=gt_, off_ap=off_ap, i4=i4: e.indirect_dma_start(
                out=gt_[i4], out_offset=None, in_=ys_d[:, :], in_offset=bass.IndirectOffsetOnAxis(ap=off_ap, axis=0)),
                (), [("g", k, i4)], dma=True)
        dma("sp", x1r[i4], x1_d[blk * 128:(blk + 1) * 128, :], (), [("x1r", i4)])

    for blk in range(2):
        load7(blk)
    for blk in range(NB):
        if blk + 2 < NB:
            load7(blk + 2)
        i4 = blk % R7
        i2 = blk % 2
        stt(x1r[i4], g1[i4], cw_all[:, blk, 0:1], x1r[i4], ALU.mult, ALU.add, [("g", 0, i4), ("x1r", i4)], [("x1r", i4)])
        stt(x1r[i4], g2_[i4], cw_all[:, blk, 1:2], x1r[i4], ALU.mult, ALU.add, [("g", 1, i4), ("x1r", i4)], [("x1r", i4)])
        act(junk7, x1r[i4], AF.Square, [("x1r", i4)], [("ss7", i2)], accum_out=sm7[:, i2:i2 + 1])
        act(sm7[:, 2 + i2:3 + i2], sm7[:, i2:i2 + 1], AF.Sqrt, [("ss7", i2)], [("rs7", i2)], scale=1.0 / D, bias=eps_c[:, 0:1])
        recip(sm7[:, 2 + i2:3 + i2], sm7[:, 2 + i2:3 + i2], [("rs7", i2)], [("rs7", i2)])
        stt(ot[i2], x1r[i4], sm7[:, 2 + i2:3 + i2], gfin, ALU.mult, ALU.mult, [("x1r", i4), ("rs7", i2), "gfin"], [("ot", i2)])
        dma("sp", out_d[blk * 128:(blk + 1) * 128, :], ot[i2], [("ot", i2)], [("out", blk)])
    sc.barrier()
    return finish(nc, es, sc, None)


def finish(nc, es, sc, _):
    block = es.enter_context(nc.Block())
    sc.emit(block)
    es.close()
    return nc


def fm(v, k):
    return np.ascontiguousarray(np.asarray(v, np.float32).reshape(k, 128).T)


def kmajor(w):
    K, N = w.shape
    return np.ascontiguousarray(w.reshape(K // 128, 128, N).transpose(1, 0, 2).reshape(128, (K // 128) * N))


def host_inputs(I):
    shared = {}
    shared["wada"] = kmajor(I["w_ada"][0])
    shared["bada_row"] = np.ascontiguousarray(I["b_ada"][0].reshape(1, 6144).astype(np.float32))
    shared["gmix_row"] = np.ascontiguousarray(I["g_mix"][0].reshape(1, D).astype(np.float32))
    shared["gffn_row"] = np.ascontiguousarray(I["g_ffn"][0].reshape(1, D).astype(np.float32))
    shared["gfinal_row"] = np.ascontiguousarray(I["g_final"].reshape(1, D).astype(np.float32))
    w_in = I["w_in"][0]
    kr = w_in[:, 384:416]
    z64 = np.zeros((D, 64), np.float32)
    kr_sw = np.concatenate([kr[:, 16:32], kr[:, 0:16]], axis=1)
    win_ext = np.concatenate([w_in, z64, kr, z64, kr_sw], axis=1)
    assert win_ext.shape[1] == WC
    shared["win"] = kmajor(win_ext)
    shared["gq_fm"] = fm(I["g_q"][0], 2)
    shared["gkv_fm"] = fm(I["g_kv"][0], 1)
    wuq = I["w_uq"][0]
    wuq_sw = wuq.reshape(256, 8, 96).copy()
    wuq_sw[:, :, 64:80] = wuq.reshape(256, 8, 96)[:, :, 80:96]
    wuq_sw[:, :, 80:96] = wuq.reshape(256, 8, 96)[:, :, 64:80]
    shared["wuq"] = kmajor(wuq)
    shared["wuq_sw"] = kmajor(wuq_sw.reshape(256, 768))
    shared["wuk"] = np.ascontiguousarray(I["w_uk"][0])
    shared["wuv"] = np.ascontiguousarray(I["w_uv"][0])
    rc = np.zeros((128, 2), np.float32)
    inv = (10000.0 ** (-np.arange(16, dtype=np.float32) / 16)).astype(np.float32)
    for p in range(128):
        rc[p, 0] = inv[p % 16]
    shared["rconst"] = rc
    shared["ident"] = np.eye(128, dtype=np.float32)
    sel = np.zeros((128, 96), np.float32)
    for p in range(64, 96):
        sel[p, p] = 1.0
    shared["sel"] = sel
    kk = np.arange(128)[:, None]
    qq = np.arange(640)[None, :]
    idx = np.clip(qq - kk, -256, 256) + 256
    dq = qq // 64 - kk // 64
    valid = (dq >= 0) & (dq <= 8)
    rb = I["rel_bias"][0]
    bt = np.where(valid[None], rb[:, idx], np.float32(-1e30)).astype(np.float32)
    shared["biasT"] = np.ascontiguousarray(bt.transpose(1, 0, 2).reshape(128, 8 * 640))
    shared["woa"] = kmajor(I["w_oa"][0])
    shared["wob"] = kmajor(I["w_ob"][0])
    shared["wout"] = kmajor(I["w_out"][0])
    shared["wr"] = kmajor(np.concatenate([I["w_rg"][0], I["w_re"][0]], axis=1))
    shared["rb"] = np.ascontiguousarray(np.concatenate([I["b_rg"][0], I["b_re"][0]]).reshape(1, 36).astype(np.float32))
    shared["iota_e"] = np.ascontiguousarray(np.broadcast_to(np.arange(32, dtype=np.float32), (128, 32)))
    shared["lst"] = np.triu(np.ones((128, 128), np.float32), 1)
    shared["jv"] = np.ascontiguousarray(np.broadcast_to((np.arange(NTILE, dtype=np.float32) * SLOT_T), (128, NTILE)))
    shared["pidx"] = np.arange(128, dtype=np.float32).reshape(128, 1)
    shared["wgl"] = np.ascontiguousarray(I["w_gate"][0].reshape(32, 8, 128, 256).transpose(0, 2, 1, 3).reshape(32 * 128, 2048))
    shared["wul"] = np.ascontiguousarray(I["w_up"][0].reshape(32, 8, 128, 256).transpose(0, 2, 1, 3).reshape(32 * 128, 2048))
    shared["wdl"] = np.ascontiguousarray(I["w_down"][0].reshape(32, 2, 128, 1024).transpose(0, 2, 1, 3).reshape(32 * 128, 2048))
    per_core = []
    for b in range(8):
        d = dict(shared)
        d["x"] = np.ascontiguousarray(I["x"][b])
        d["cfm"] = fm(I["c"][b], 8)
        d["pos"] = np.ascontiguousarray(I["positions"][b].reshape(1, S).astype(np.int32))
        per_core.append(d)
    return per_core


_NC_CACHE = {}


def kernel(**inputs):
    I = {k: np.asarray(v) for k, v in inputs.items()}
    in_maps = host_inputs(I)
    if "nc" not in _NC_CACHE:
        _NC_CACHE["nc"] = build_nc()
    nc = _NC_CACHE["nc"]
    res = run_bass_kernel_spmd(nc, in_maps, core_ids=list(range(8)))
    return np.stack([r["out"] for r in res.results], axis=0).astype(np.float32)
```

```python
import os
import math
from contextlib import ExitStack

import numpy as np
import concourse.bass as bass
import concourse.mybir as mybir
from concourse.bass_utils import run_bass_kernel_spmd

F32 = mybir.dt.float32
BF16 = mybir.dt.bfloat16
I32 = mybir.dt.int32
U32 = mybir.dt.uint32
U8 = mybir.dt.uint8
AF = mybir.ActivationFunctionType
ALU = mybir.AluOpType
AX = mybir.AxisListType

S = 4096
D = 1024
TT = 512
NT = S // TT
NB = S // 128
EPS = 1e-6
WC = 4192
C_QB, C_KB, C_VB, C_GA, C_GB, C_KRP, C_KRS = 416, 928, 1440, 1952, 2976, 4000, 4096
PI = math.pi
SLOT_T = 256
SH = 8
NTILE = 64
NSLOT = NTILE * SLOT_T


class Sched:
    COMPUTE = ("pe", "act", "dve", "pool")

    def __init__(self, nc, es, n_dma_sems=8):
        self.nc = nc
        self.ops = []
        self.n_dma_sems = n_dma_sems
        self.eng_sem = {e: es.enter_context(nc.semaphore("c_" + e)) for e in self.COMPUTE}
        self.dma_sems = {q: [es.enter_context(nc.semaphore("d_%s%d" % (q, i))) for i in range(n_dma_sems)]
                         for q in ("sp", "pool", "act")}
        self.state = {}
        self.last_on = {}
        self.dma_since_bar = []

    def add(self, eng, fn, reads=(), writes=(), dma=False, extra_deps=()):
        op = dict(eng=eng, fn=fn, dma=dma, idx=len(self.ops), signal=False)
        deps = set(extra_deps)
        st = self.state
        for k in reads:
            w, rd = st.setdefault(k, [None, {}])
            if w is not None:
                deps.add(w)
        for k in writes:
            w, rd = st.setdefault(k, [None, {}])
            if w is not None:
                deps.add(w)
            deps.update(rd.values())
        me = (eng, "dma", op["idx"]) if dma else eng
        for k in reads:
            st[k][1][me] = op["idx"]
        for k in writes:
            st[k][0] = op["idx"]
            st[k][1] = {}
        real = set()
        for d in deps:
            dop = self.ops[d]
            if (not dop["dma"]) and (not dma) and dop["eng"] == "pe" and eng == "pe":
                continue
            real.add(d)
            dop["signal"] = True
        op["deps"] = real
        self.ops.append(op)
        if dma:
            self.dma_since_bar.append(op["idx"])
        elif fn is not None:
            self.last_on[eng] = op["idx"]
        return op

    def barrier(self):
        lasts = dict(self.last_on)
        dmas = list(self.dma_since_bar)
        self.dma_since_bar = []
        for eng in ("pe", "act", "dve", "pool", "sp"):
            deps = [v for e, v in lasts.items() if e != eng] + dmas
            self.add(eng, None, extra_deps=deps)
        self.state = {}

    def emit(self, block):
        cnt = {e: 0 for e in self.COMPUTE}
        dcnt = {q: 0 for q in self.dma_sems}
        for op in self.ops:
            if not op["signal"]:
                continue
            if op["dma"]:
                q = op["eng"]
                i = dcnt[q]
                dcnt[q] += 1
                op["sem"] = self.dma_sems[q][i % self.n_dma_sems]
                op["val"] = 16 * (i // self.n_dma_sems + 1)
            else:
                e = op["eng"]
                assert op["fn"] is not None
                cnt[e] += 1
                op["sem"] = self.eng_sem[e]
                op["val"] = cnt[e]
        self.stats = dict(cnt=cnt, dcnt=dcnt, nops=len(self.ops))
        per_eng = {e: [] for e in ("pe", "act", "dve", "pool", "sp")}
        for op in self.ops:
            per_eng[op["eng"]].append(op)
        ops = self.ops

        def run(eng_name, engine):
            seen = {}
            for op in per_eng[eng_name]:
                need = {}
                for d in op["deps"]:
                    dop = ops[d]
                    s, v = dop["sem"], dop["val"]
                    if need.get(s.num, (None, 0))[1] < v:
                        need[s.num] = (s, v)
                for num, (s, v) in need.items():
                    if seen.get(num, 0) >= v:
                        continue
                    engine.wait_ge(s, v)
                    seen[num] = v
                if op["fn"] is None:
                    continue
                ins = op["fn"](engine)
                if op["signal"]:
                    ins.then_inc(op["sem"], 16 if op["dma"] else 1)

        block.tensor(lambda e: run("pe", e))
        block.scalar(lambda e: run("act", e))
        block.vector(lambda e: run("dve", e))
        block.gpsimd(lambda e: run("pool", e))
        block.sync(lambda e: run("sp", e))


class Arena:
    def __init__(self, big, size):
        self.big = big
        self.size = size
        self.top = 0

    def alloc(self, free_shape, dtype):
        esz = {F32: 4, BF16: 2, I32: 4, U32: 4, U8: 1}[dtype]
        n = int(np.prod(free_shape))
        nbytes = (n * esz + 63) // 64 * 64
        off = self.top
        self.top += nbytes
        assert self.top <= self.size, "SBUF arena overflow %d > %d" % (self.top, self.size)
        v = self.big[:, off:off + n * esz]
        if dtype != U8:
            v = v.bitcast(dtype)
        if len(free_shape) == 2:
            v = v.rearrange("p (a b) -> p a b", b=free_shape[1])
        elif len(free_shape) == 3:
            v = v.rearrange("p (a b c) -> p a b c", b=free_shape[1], c=free_shape[2])
        return v

    def mark(self):
        return self.top

    def release(self, m):
        self.top = m


def build_nc(stage=99, dbg=False):
    sub = int(os.environ.get('KSUB', '99'))
    nc = bass.Bass("TRN2", target_bir_lowering=False)
    es = ExitStack()

    def din(name, shape, dt=F32):
        return nc.dram_tensor(name, list(shape), dt, kind="ExternalInput").ap()

    def dscr(name, shape, dt):
        kind = "ExternalOutput" if dbg else "Internal"
        return nc.dram_tensor(name, list(shape), dt, kind=kind).ap()

    x_d = din("x", [S, D])
    cfm_d = din("cfm", [128, 8])
    pos_d = din("pos", [1, S], I32)
    wada_d = din("wada", [128, 8 * 6144])
    badar_d = din("bada_row", [1, 6144])
    gmixr_d = din("gmix_row", [1, D])
    gffnr_d = din("gffn_row", [1, D])
    gfin_d = din("gfinal_row", [1, D])
    win_d = din("win", [128, 8 * WC])
    gq_d = din("gq_fm", [128, 2])
    gkv_d = din("gkv_fm", [128, 1])
    wuq_d = din("wuq", [128, 2 * 768])
    wuqs_d = din("wuq_sw", [128, 2 * 768])
    wuk_d = din("wuk", [128, 512])
    wuv_d = din("wuv", [128, 512])
    rconst_d = din("rconst", [128, 2])
    ident_d = din("ident", [128, 128])
    sel_d = din("sel", [128, 96])
    biasT_d = din("biasT", [128, 8 * 640])
    woa_d = din("woa", [128, 4 * D])
    wob_d = din("wob", [128, 4 * D])
    wout_d = din("wout", [128, 8 * D])
    wr_d = din("wr", [128, 8 * 36])
    rb_d = din("rb", [1, 36])
    iota_d = din("iota_e", [128, 32])
    lst_d = din("lst", [128, 128])
    jv_d = din("jv", [128, NTILE])
    pidx_d = din("pidx", [128, 1])
    wg_d = din("wgl", [32 * 128, 2048])
    wu_d = din("wul", [32 * 128, 2048])
    wdn_d = din("wdl", [32 * 128, 2048])
    out_d = nc.dram_tensor("out", [S, D], F32, kind="ExternalOutput").ap()

    tabc_d = dscr("tabc", [128, 512], F32)
    tabsp_d = dscr("tabsp", [128, 512], F32)
    tabsn_d = dscr("tabsn", [128, 512], F32)
    qT_d = dscr("qT", [96, 8 * S], BF16)
    kT_d = dscr("kT", [96, 8 * S], BF16)
    va_d = dscr("va", [S, 520], BF16)
    qbT_d = dscr("qbT", [128, 4 * S], BF16)
    kbT_d = dscr("kbT", [128, 4 * S], BF16)
    vb_d = dscr("vb", [S, 520], BF16)
    ga_d = dscr("gaT", [128, 8 * S], F32)
    gb_d = dscr("gbT", [128, 8 * S], F32)
    moddbg_d = dscr("moddbg", [128, 48], F32) if dbg else None
    x1_d = dscr("x1s", [S, D], F32)
    h2_d = dscr("h2s", [S, D], BF16)
    xs_d = dscr("xs", [NSLOT, D], BF16)
    ys_d = dscr("ys", [NSLOT, D], F32)
    wx_d = nc.dram_tensor("wx", [32 * 128, 6144], BF16, kind="Internal").ap()

    SB_BYTES = 212480
    big = nc.alloc_sbuf_tensor("big", [128, SB_BYTES], U8)
    A = Arena(big, SB_BYTES)
    banks = [nc.alloc_psum_tensor("psb%d" % i, [128, 512], F32).ap() for i in range(8)]
    sc = Sched(nc, es)

    psn = [0]

    def ps_next():
        i = psn[0] % 8
        psn[0] += 1
        return banks[i], ("ps", i)

    def dma(q, out, in_, reads, writes, **kw):
        sc.add(q, lambda e: e.dma_start(out=out, in_=in_, **kw), reads, writes, dma=True)

    def mm(out, lhsT, rhs, start, stop, reads, writes):
        sc.add("pe", lambda e: e.matmul(out, lhsT, rhs, start=start, stop=stop), reads, writes)

    def tr(out, in_, ident, reads, writes):
        sc.add("pe", lambda e: e.transpose(out, in_, ident), reads, writes)

    def act(out, in_, func, reads, writes, **kw):
        sc.add("act", lambda e: e.activation(out=out, in_=in_, func=func, **kw), reads, writes)

    def tcopy(eng, out, in_, reads, writes):
        if eng == "act":
            sc.add(eng, lambda e: e.activation(out=out, in_=in_, func=AF.Copy), reads, writes)
        else:
            sc.add(eng, lambda e: e.tensor_copy(out=out, in_=in_), reads, writes)

    def tt(eng, out, in0, in1, op, reads, writes):
        sc.add(eng, lambda e: e.tensor_tensor(out=out, in0=in0, in1=in1, op=op), reads, writes)

    def ts(eng, out, in0, s1, s2, op0, op1, reads, writes):
        if s2 is None:
            sc.add(eng, lambda e: e.tensor_scalar(out=out, in0=in0, scalar1=s1, scalar2=None, op0=op0),
                   reads, writes)
        else:
            sc.add(eng, lambda e: e.tensor_scalar(out=out, in0=in0, scalar1=s1, scalar2=s2, op0=op0, op1=op1),
                   reads, writes)

    def stt(out, in0, scalar, in1, op0, op1, reads, writes):
        sc.add("dve", lambda e: e.scalar_tensor_tensor(out=out, in0=in0, scalar=scalar, in1=in1, op0=op0, op1=op1),
               reads, writes)

    def memset(eng, ap, val, writes):
        sc.add(eng, lambda e: e.memset(ap, val), (), writes)

    def recip(out, in_, reads, writes):
        sc.add("dve", lambda e: e.reciprocal(out=out, in_=in_), reads, writes)

    ident_f = A.alloc([128], F32)
    ident_b = A.alloc([128], BF16)
    ones_f = A.alloc([128], F32)
    ones_b = A.alloc([128], BF16)
    eps_c = A.alloc([1], F32)
    modfm = A.alloc([48], F32)
    s1_fm = A.alloc([8], F32)
    s2_fm = A.alloc([8], F32)
    gt1_bc = A.alloc([D], F32)
    gt2_bc = A.alloc([D], F32)
    s2_bc = A.alloc([D], F32)
    b2_bc = A.alloc([D], F32)

    stg = A.alloc([2048], BF16)
    pre_steps = []
    for mi, wsrc in enumerate((wg_d, wu_d, wdn_d)):
        for e_ in range(32):
            pre_steps.append(("ld", mi, wsrc, e_))
            pre_steps.append(("st", mi, wsrc, e_))
    pre_pos = [0]

    def precast_step():
        if pre_pos[0] >= len(pre_steps):
            return
        kind, mi, wsrc, e_ = pre_steps[pre_pos[0]]
        pre_pos[0] += 1
        if kind == "ld":
            dma("pool", stg, wsrc[e_ * 128:(e_ + 1) * 128, :], (), ["stg"])
        else:
            dma("sp", wx_d[e_ * 128:(e_ + 1) * 128, mi * 2048:(mi + 1) * 2048], stg, ["stg"], [("wx", mi, e_)])

    dma("sp", ident_f, ident_d, (), ["ident_f"])
    tcopy("dve", ident_b, ident_f, ["ident_f"], ["ident_b"])
    memset("dve", ones_f, 1.0, ["ones_f"])
    memset("dve", ones_b, 1.0, ["ones_b"])
    memset("dve", eps_c, EPS, ["eps_c"])

    pP = A.mark()
    wuq_b = A.alloc([2, 768], BF16)
    wuqs_b = A.alloc([2, 768], BF16)
    wukp_b = A.alloc([8, 96], BF16)
    wuv_b = A.alloc([512], BF16)
    sel_b = A.alloc([96], BF16)
    gq = A.alloc([2], F32)
    gkv = A.alloc([1], F32)
    win_b = A.alloc([8, WC], BF16)
    winv = win_d.rearrange("p (k n) -> p k n", k=8)
    for kc in range(8):
        for c0 in range(0, WC, 1048):
            dma("pool", win_b[:, kc, c0:c0 + 1048], winv[:, kc, c0:c0 + 1048], (), [("win", kc, c0)])
    p0 = A.mark()
    cfm = A.alloc([8], F32)
    cact = A.alloc([8], F32)
    c_rep = A.alloc([8, 128], F32)
    bada_bc = A.alloc([6144], F32)
    gmix_bc = A.alloc([D], F32)
    gffn_bc = A.alloc([D], F32)
    sh1_r = A.alloc([D], F32)
    sc1_r = A.alloc([D], F32)
    sc2_r = A.alloc([D], F32)
    dtmp = A.alloc([128], F32)
    wbuf = [A.alloc([3072], F32) for _ in range(2)]
    dma("sp", cfm, cfm_d, (), ["cfm"])
    dma("sp", bada_bc, badar_d[0:1, :].to_broadcast([128, 6144]), (), ["bada_bc"])
    dma("sp", gmix_bc, gmixr_d[0:1, :].to_broadcast([128, D]), (), ["gmix_bc"])
    dma("sp", gffn_bc, gffnr_d[0:1, :].to_broadcast([128, D]), (), ["gffn_bc"])
    act(cact, cfm, AF.Silu, ["cfm"], ["cact"])
    tcopy("dve", c_rep, cact[:, :, None].broadcast_to([128, 8, 128]), ["cact"], ["c_rep"])
    wv = wada_d.rearrange("p (k n) -> p k n", k=8)
    dests = [sh1_r, sc1_r, gt1_bc, b2_bc, sc2_r, gt2_bc]
    dkeys = ["sh1_r", "sc1_r", "gt1_bc", "b2_bc", "sc2_r", "gt2_bc"]
    wcn = 0
    for hf_ in range(2):
        for kc in range(8):
            wb = wbuf[wcn % 2]
            wk = ("wbuf", wcn % 2)
            wcn += 1
            dma("sp", wb, wv[:, kc, hf_ * 3072:(hf_ + 1) * 3072], (), [wk])
            for nt in range(6):
                mm(banks[nt], c_rep[:, kc, :], wb[:, nt * 512:(nt + 1) * 512], kc == 0, kc == 7,
                   [wk, "c_rep"], [("ps", nt)])
        for nt in range(6):
            n0 = hf_ * 3072 + nt * 512
            di = n0 // 1024
            tt("dve", dests[di][:, n0 % 1024:n0 % 1024 + 512], banks[nt], bada_bc[:, n0:n0 + 512], ALU.add,
               [("ps", nt), "bada_bc"], [(dkeys[di], (n0 % 1024) // 512)])
    K2 = lambda nm: [(nm, 0), (nm, 1)]
    stt(sc1_r, sc1_r, 1.0, gmix_bc, ALU.add, ALU.mult, K2("sc1_r") + ["gmix_bc"], ["s1_r"])
    stt(s2_bc, sc2_r, 1.0, gffn_bc, ALU.add, ALU.mult, K2("sc2_r") + ["gffn_bc"], ["s2_bc"])
    for (row, rkeys, dst_fm, dk) in ((sc1_r, ["s1_r"], s1_fm, "s1"), (sh1_r, K2("sh1_r"), modfm, "modfm")):
        for kc in range(8):
            tt("dve", dtmp, row[:, kc * 128:(kc + 1) * 128], ident_f, ALU.mult, rkeys + ["ident_f"], ["dtmp"])
            sc.add("dve", lambda e, dst_fm=dst_fm, kc=kc: e.tensor_reduce(out=dst_fm[:, kc:kc + 1], in_=dtmp, axis=AX.X,
                                                                         op=ALU.add), ["dtmp"], [dk])

    rconst = A.alloc([2], F32)
    dma("sp", rconst, rconst_d, (), ["rconst"])
    HALF = 512
    posi = A.alloc([HALF], I32)
    ang = A.alloc([HALF], F32)
    halfpi = A.alloc([1], F32)
    memset("dve", halfpi, PI / 2, ["halfpi"])
    tmpS = [A.alloc([HALF], F32) for _ in range(4)]
    kiS = A.alloc([HALF], I32)
    for cb in range(8):
        dma("sp", posi[cb * 16:(cb + 1) * 16, :], pos_d[0:1, cb * 512:(cb + 1) * 512].to_broadcast([16, 512]), (),
            [("posi", cb)])
    POSI = [("posi", cb) for cb in range(8)]
    tcopy("dve", ang, posi, POSI, ["ang"])
    ts("dve", ang, ang, rconst[:, 0:1], None, ALU.mult, None, ["ang", "rconst"], ["ang"])
    t1, r0, mk, t2 = tmpS
    ts("dve", t1, ang, 1.0 / (2 * PI), None, ALU.mult, None, ["ang"], ["s_t1"])
    tcopy("dve", kiS, t1, ["s_t1"], ["s_ki"])
    stt(r0, kiS, -2 * PI, ang, ALU.mult, ALU.add, ["s_ki", "ang"], ["s_r0"])
    ts("dve", mk, r0, PI, -2 * PI, ALU.is_gt, ALU.mult, ["s_r0"], ["s_mk"])
    tt("dve", r0, r0, mk, ALU.add, ["s_r0", "s_mk"], ["s_r0"])
    ts("dve", mk, r0, -PI, 2 * PI, ALU.is_lt, ALU.mult, ["s_r0"], ["s_mk"])
    tt("dve", r0, r0, mk, ALU.add, ["s_r0", "s_mk"], ["s_r0"])
    ts("dve", r0, r0, PI, -PI, ALU.min, ALU.max, ["s_r0"], ["s_r0"])
    act(t1, r0, AF.Sin, ["s_r0"], ["s_t1"])
    dma("sp", tabsp_d, t1, ["s_t1"], ["tabsp"])
    act(t2, r0, AF.Sin, ["s_r0"], ["s_t2"], scale=-1.0)
    dma("sp", tabsn_d, t2, ["s_t2"], ["tabsn"])
    ts("dve", t1, ang, PI / 2, 1.0 / (2 * PI), ALU.add, ALU.mult, ["ang", "s_t1"], ["s_t1"])
    tcopy("dve", kiS, t1, ["s_t1"], ["s_ki"])
    stt(r0, kiS, -2 * PI, ang, ALU.mult, ALU.add, ["s_ki", "ang", "s_r0"], ["s_r0"])
    ts("dve", mk, r0, PI / 2, -2 * PI, ALU.is_gt, ALU.mult, ["s_r0"], ["s_mk"])
    tt("dve", r0, r0, mk, ALU.add, ["s_r0", "s_mk"], ["s_r0"])
    ts("dve", mk, r0, -1.5 * PI, 2 * PI, ALU.is_lt, ALU.mult, ["s_r0"], ["s_mk"])
    tt("dve", r0, r0, mk, ALU.add, ["s_r0", "s_mk"], ["s_r0"])
    ts("dve", r0, r0, PI / 2, -1.5 * PI, ALU.min, ALU.max, ["s_r0"], ["s_r0"])
    act(t1, r0, AF.Sin, ["s_r0", "halfpi"], ["s_t1"], bias=halfpi[:, 0:1])
    dma("sp", tabc_d, t1, ["s_t1"], ["tabc"])

    wtmp = A.alloc([2, 768], F32)
    wtmp2 = A.alloc([2, 768], F32)
    wtmp3 = A.alloc([512], F32)
    wtmp4 = A.alloc([512], F32)
    seltmp = A.alloc([96], F32)
    dma("sp", gq, gq_d, (), ["gq"])
    dma("sp", gkv, gkv_d, (), ["gkv"])
    dma("sp", wtmp, wuq_d.rearrange("p (k n) -> p k n", k=2), (), ["wtmp"])
    dma("sp", wtmp2, wuqs_d.rearrange("p (k n) -> p k n", k=2), (), ["wtmp2"])
    dma("sp", wtmp3, wuk_d, (), ["wtmp3"])
    dma("sp", wtmp4, wuv_d, (), ["wtmp4"])
    dma("sp", seltmp, sel_d, (), ["seltmp"])
    for kc in range(2):
        ts("dve", wuq_b[:, kc, :], wtmp[:, kc, :], gq[:, kc:kc + 1], None, ALU.mult, None, ["wtmp", "gq"], ["wuq_b"])
        ts("dve", wuqs_b[:, kc, :], wtmp2[:, kc, :], gq[:, kc:kc + 1], None, ALU.mult, None, ["wtmp2", "gq"], ["wuqs_b"])
    memset("dve", wukp_b, 0.0, ["wukp_b"])
    ts("dve", wukp_b[:, :, 0:64], wtmp3.rearrange("p (h d) -> p h d", d=64), gkv[:, 0:1], None, ALU.mult, None,
       ["wtmp3", "gkv", "wukp_b"], ["wukp_b"])
    ts("dve", wuv_b, wtmp4, gkv[:, 0:1], None, ALU.mult, None, ["wtmp4", "gkv"], ["wuv_b"])
    tcopy("dve", sel_b[0:96], seltmp[0:96], ["seltmp"], ["sel_b"])

    sc.barrier()
    A.release(p0)
    if stage <= 0:
        return finish(nc, es, sc, None)

    WIN = []

    xb = [A.alloc([D], F32) for _ in range(2)]
    junk = A.alloc([D], BF16)
    ssq = A.alloc([4], F32)
    rstd = A.alloc([4], F32)
    xn = [A.alloc([D], F32) for _ in range(2)]
    hT = [A.alloc([8, TT], BF16) for _ in range(2)]
    ctab = A.alloc([TT], F32)
    stab = A.alloc([TT], F32)
    qlat = A.alloc([2, TT], F32)
    qsq = A.alloc([2, TT], F32)
    kvlat = A.alloc([TT], F32)
    kvsq = A.alloc([TT], F32)
    rbc = [A.alloc([TT], F32) for _ in range(2)]
    qln = A.alloc([2, TT], BF16)
    kvn = A.alloc([TT], BF16)
    krr = A.alloc([TT], BF16)
    rt1 = [A.alloc([TT], F32) for _ in range(2)]
    rt2 = [A.alloc([TT], F32) for _ in range(2)]
    qT_s = A.alloc([8, TT], BF16)
    kT_s = A.alloc([8, TT], BF16)
    qbT_s = A.alloc([4, TT], BF16)
    kbT_s = A.alloc([4, TT], BF16)
    va_s = A.alloc([4, 520], BF16)
    vb_s = A.alloc([4, 520], BF16)
    gst = [A.alloc([TT], F32) for _ in range(4)]
    memset("dve", ctab[0:64], 1.0, ["ctab0"])
    memset("dve", stab[0:64], 0.0, ["stab0"])
    memset("dve", va_s, 1.0, ["va_s"])
    memset("dve", vb_s, 1.0, ["vb_s"])

    qT_v = qT_d.rearrange("p (h t) -> p h t", h=8)
    kT_v = kT_d.rearrange("p (h t) -> p h t", h=8)
    qbT_v = qbT_d.rearrange("p (h t) -> p h t", h=4)
    kbT_v = kbT_d.rearrange("p (h t) -> p h t", h=4)
    ga_v = ga_d.rearrange("p (c t) -> p c t", c=8)
    gb_v = gb_d.rearrange("p (c t) -> p c t", c=8)
    gcnt = [0]
    ecnt = [0]

    def evac_copy(out, in_, reads, writes):
        eng = "act" if ecnt[0] % 2 == 0 else "dve"
        ecnt[0] += 1
        tcopy(eng, out, in_, reads, writes)

    NT1 = NT if stage > 1 else 1

    def prep_stats(ti, bi):
        t0 = ti * TT
        g = ti * 4 + bi
        xt = xb[g % 2]
        xk = ("xb", g % 2)
        dma("sp", xt, x_d[t0 + bi * 128:t0 + (bi + 1) * 128, :], (), [xk])
        act(junk, xt, AF.Square, [xk], [("ssq", bi)], accum_out=ssq[:, bi:bi + 1])
        act(rstd[:, bi:bi + 1], ssq[:, bi:bi + 1], AF.Sqrt, [("ssq", bi), "eps_c"], [("rstd", bi)],
            scale=1.0 / D, bias=eps_c[:, 0:1])
        recip(rstd[:, bi:bi + 1], rstd[:, bi:bi + 1], [("rstd", bi)], [("rstd", bi)])
        ts("dve", xn[g % 2], xt, rstd[:, bi:bi + 1], None, ALU.mult, None, [xk, ("rstd", bi)], [("xn", g % 2)])

    def prep_tr(ti, bi):
        g = ti * 4 + bi
        xnt = xn[g % 2]
        nk = ("xn", g % 2)
        h_t = hT[ti % 2]
        for half in range(2):
            pb, pk = ps_next()
            for q in range(4):
                kc = half * 4 + q
                tr(pb[:, q * 128:(q + 1) * 128], xnt[:, kc * 128:(kc + 1) * 128], ident_f, [nk, "ident_f"], [pk])
            for q in range(4):
                kc = half * 4 + q
                dst = h_t[:, kc, bi * 128:(bi + 1) * 128]
                hk = ("hT", ti % 2, bi, q % 2)
                act(dst, pb[:, q * 128:(q + 1) * 128], AF.Identity, [pk, "s1", "modfm"], [hk],
                    scale=s1_fm[:, kc:kc + 1], bias=modfm[:, kc:kc + 1])

    def prep_all(ti):
        prep_stats(ti, 0)
        prep_stats(ti, 1)
        prep_tr(ti, 0)
        prep_stats(ti, 2)
        prep_tr(ti, 1)
        prep_stats(ti, 3)
        prep_tr(ti, 2)
        prep_tr(ti, 3)

    prep_all(0)
    for ti in range(NT1):
        t0 = ti * TT
        h_t = hT[ti % 2]
        HK = [("hT", ti % 2, bi_, q_) for bi_ in range(4) for q_ in range(2)]
        nxt = ti + 1 if ti + 1 < NT1 else None
        dma("sp", ctab[64:80], tabc_d[ti * 16:(ti + 1) * 16, :], (), ["ctab"])
        dma("sp", ctab[80:96], tabc_d[ti * 16:(ti + 1) * 16, :], (), ["ctab2"])
        dma("sp", stab[64:80], tabsn_d[ti * 16:(ti + 1) * 16, :], (), ["stab"])
        dma("sp", stab[80:96], tabsp_d[ti * 16:(ti + 1) * 16, :], (), ["stab2"])

        def proj(c0, m):
            pb, pk = ps_next()
            for kc in range(8):
                mm(pb[0:m, :], win_b[:, kc, c0:c0 + m], h_t[:, kc, :], kc == 0, kc == 7, HK + WIN, [pk])
            return pb, pk

        for c in range(2):
            pb, pk = proj(c * 128, 128)
            act(qlat[:, c, :], pb, AF.Copy, [pk], [("qlat", c)])
            act(qsq[:, c, :], pb, AF.Square, [pk], [("qsq", c)])
        pb, pk = proj(256, 128)
        act(kvlat, pb, AF.Copy, [pk], ["kvlat"])
        act(kvsq, pb, AF.Square, [pk], ["kvsq"])
        pa, pka = proj(C_KRP, 96)
        pbb, pkb = proj(C_KRS, 96)
        tt("dve", rt1[0][0:96], pa[0:96, :], ctab[0:96], ALU.mult, [pka, "ctab", "ctab2", "ctab0"], [("rt1", 0)])
        tt("dve", rt2[0][0:96], pbb[0:96, :], stab[0:96], ALU.mult, [pkb, "stab", "stab2", "stab0"], [("rt2", 0)])
        tt("pool", krr[0:96], rt1[0][0:96], rt2[0][0:96], ALU.add, [("rt1", 0), ("rt2", 0)], ["krr"])
        pq, pkq = ps_next()
        for c in range(2):
            mm(pq, ones_f, qsq[:, c, :], c == 0, c == 1, ["ones_f", ("qsq", c)], [pkq])
        act(rbc[0], pq, AF.Sqrt, [pkq, "eps_c"], [("rbc", 0)], scale=1.0 / 256, bias=eps_c[:, 0:1])
        recip(rbc[0], rbc[0], [("rbc", 0)], [("rbc", 0)])
        for c in range(2):
            tt("dve", qln[:, c, :], qlat[:, c, :], rbc[0], ALU.mult, [("qlat", c), ("rbc", 0)], [("qln", c)])
        pkv, pkkv = ps_next()
        mm(pkv, ones_f, kvsq, True, True, ["ones_f", "kvsq"], [pkkv])
        act(rbc[1], pkv, AF.Sqrt, [pkkv, "eps_c"], [("rbc", 1)], scale=1.0 / 128, bias=eps_c[:, 0:1])
        recip(rbc[1], rbc[1], [("rbc", 1)], [("rbc", 1)])
        tt("dve", kvn, kvlat, rbc[1], ALU.mult, ["kvlat", ("rbc", 1)], ["kvn"])
        if nxt is not None:
            prep_stats(nxt, 0)
            prep_stats(nxt, 1)
        for i in range(4):
            pb, pk = proj(C_QB + i * 128, 128)
            evac_copy(qbT_s[:, i, :], pb, [pk], ["qbT_s"])
        dma("sp", qbT_v[:, :, t0:t0 + TT], qbT_s, ["qbT_s"], [("qbT_d", ti)])
        for h in range(8):
            pa, pka = ps_next()
            for c in range(2):
                mm(pa[0:96, :], wuq_b[:, c, h * 96:(h + 1) * 96], qln[:, c, :], c == 0, c == 1,
                   ["wuq_b", ("qln", c)], [pka])
            pbb, pkb = ps_next()
            for c in range(2):
                mm(pbb[0:96, :], wuqs_b[:, c, h * 96:(h + 1) * 96], qln[:, c, :], c == 0, c == 1,
                   ["wuqs_b", ("qln", c)], [pkb])
            i2 = h % 2
            tt("dve", rt1[i2][0:96], pa[0:96, :], ctab[0:96], ALU.mult, [pka, "ctab", "ctab2", "ctab0"], [("rt1", i2)])
            tt("dve", rt2[i2][0:96], pbb[0:96, :], stab[0:96], ALU.mult, [pkb, "stab", "stab2", "stab0"], [("rt2", i2)])
            tt("pool", qT_s[0:96, h, :], rt1[i2][0:96], rt2[i2][0:96], ALU.add, [("rt1", i2), ("rt2", i2)], ["qT_s"])
            pk_, pkk = ps_next()
            mm(pk_[0:96, :], wukp_b[:, h, :], kvn, True, False, ["wukp_b", "kvn"], [pkk])
            mm(pk_[0:96, :], sel_b[0:96, :], krr[0:96, :], False, True, ["sel_b", "krr"], [pkk])
            evac_copy(kT_s[0:96, h, :], pk_[0:96, :], [pkk], ["kT_s"])
        for bi in range(4):
            pv, pkv_ = ps_next()
            mm(pv, kvn[:, bi * 128:(bi + 1) * 128], wuv_b, True, True, ["kvn", "wuv_b"], [pkv_])
            evac_copy(va_s[:, bi, :].rearrange("p (h d) -> p h d", d=65)[:, :, 0:64],
                      pv.rearrange("p (h d) -> p h d", d=64), [pkv_], ["va_s"])
        dma("sp", qT_v[:, :, t0:t0 + TT], qT_s[0:96], ["qT_s"], [("qT_d", ti)])
        dma("sp", kT_v[:, :, t0:t0 + TT], kT_s[0:96], ["kT_s"], [("kT_d", ti)])
        dma("sp", va_d[t0:t0 + TT, :].rearrange("(b p) f -> p b f", p=128), va_s, ["va_s"], [("va_d", ti)])
        if nxt is not None:
            prep_tr(nxt, 0)
            prep_stats(nxt, 2)
        for i in range(4):
            pb, pk = proj(C_KB + i * 128, 128)
            evac_copy(kbT_s[:, i, :], pb, [pk], ["kbT_s"])
        dma("sp", kbT_v[:, :, t0:t0 + TT], kbT_s, ["kbT_s"], [("kbT_d", ti)])
        if nxt is not None:
            prep_tr(nxt, 1)
            prep_stats(nxt, 3)
        for bi in range(4):
            pv, pkv_ = ps_next()
            for kc in range(8):
                mm(pv, h_t[:, kc, bi * 128:(bi + 1) * 128], win_b[:, kc, C_VB:C_VB + 512], kc == 0, kc == 7,
                   HK + WIN, [pkv_])
            evac_copy(vb_s[:, bi, :].rearrange("p (h d) -> p h d", d=65)[:, :, 0:64],
                      pv.rearrange("p (h d) -> p h d", d=64), [pkv_], ["vb_s"])
        dma("sp", vb_d[t0:t0 + TT, :].rearrange("(b p) f -> p b f", p=128), vb_s, ["vb_s"], [("vb_d", ti)])
        if nxt is not None:
            prep_tr(nxt, 2)
        for gix, (cbase, gv, nm) in enumerate(((C_GA, ga_v, "ga"), (C_GB, gb_v, "gb"))):
            for c in range(8):
                pb, pk = proj(cbase + c * 128, 128)
                gi = gcnt[0] % 4
                gcnt[0] += 1
                act(gst[gi], pb, AF.Sigmoid, [pk], [("gst", gi)])
                dma("sp", gv[:, c, t0:t0 + TT], gst[gi], [("gst", gi)], [(nm, ti, c)])
            if gix == 0 and nxt is not None:
                prep_tr(nxt, 3)

    sc.barrier()
    if stage <= 2:
        return finish(nc, es, sc, None)

    A.release(pP)
    o_a = A.alloc([NB, 512], BF16)
    o_b = A.alloc([NB, 512], BF16)
    p2 = A.mark()
    kT_r = A.alloc([8, S], BF16)
    va_r = A.alloc([NB, 520], BF16)
    qT_t = [A.alloc([8, TT], BF16) for _ in range(2)]
    E_t = [A.alloc([TT], BF16) for _ in range(6)]
    rden = [A.alloc([4], F32) for _ in range(2)]
    for h in range(8):
        dma("sp", kT_r[0:96, h, :], kT_v[:, h, :], (), [("kT_r", h)])
    va_v = va_d.rearrange("(b p) f -> p b f", p=128)
    for q4 in range(4):
        dma("sp", va_r[:, q4 * 8:(q4 + 1) * 8, :], va_v[:, q4 * 8:(q4 + 1) * 8, :], (), [("va_r", q4)])
    SCALE_A = 96 ** -0.5
    sbank = [0]
    ecnt2 = [0]
    accn = [0]
    NQT = NT if stage > 3 else 2
    LOOK = 3
    stageA, stageB = [], []
    for qt in range(NQT):
        for h in range(8):
            nkt = 4 * qt + 4
            for kt in range(nkt):
                stageA.append((qt, h, kt))
    grp = {}

    def emitA(rec):
        qt, h, kt = rec
        qtt = qT_t[qt % 2]
        qk = ("qT_t", qt % 2)
        if h == 0 and kt == 0:
            dma("sp", qtt[0:96], qT_v[:, :, qt * TT:(qt + 1) * TT], (), [qk])
        r = kt - 4 * qt
        c0 = 128 * r if r > 0 else 0
        sb = sbank[0] % 5
        sbank[0] += 1
        ps_, psk = banks[sb], ("ps", sb)
        mm(ps_[:, c0:TT], kT_r[0:96, h, kt * 128:(kt + 1) * 128], qtt[0:96, h, c0:TT], True, True,
           [("kT_r", h), qk], [psk])
        ei = ecnt2[0] % len(E_t)
        ecnt2[0] += 1
        Et, Ek = E_t[ei], ("E", ei)
        act(Et[:, c0:TT], ps_[:, c0:TT], AF.Exp, [psk], [Ek], scale=SCALE_A)
        if r >= 0:
            memset("dve", Et[64:128, c0:c0 + 64], 0.0, [Ek])
        grp[rec] = (Et, Ek)

    def emitB(rec):
        qt, h, kt = rec
        nkt = 4 * qt + 4
        r = kt - 4 * qt
        if kt == 0:
            ab = 5 + accn[0] % 2
            accn[0] += 1
            grp["acc"] = (banks[ab], ("ps", ab))
        acc, acck = grp["acc"]
        Et, Ek = grp.pop(rec)
        for qb in range(max(r, 0), 4):
            first = (kt == 0) and (qb == 0)
            last = (kt == nkt - 1) and (qb == 3)
            mm(acc[:, qb * 65:(qb + 1) * 65], Et[:, qb * 128:(qb + 1) * 128],
               va_r[:, kt, h * 65:(h + 1) * 65], first, last, [Ek, ("va_r", kt // 8)], [acck])
        if kt == nkt - 1:
            rd = rden[h % 2]
            rk = ("rden", h % 2)
            accv = acc[:, 0:260].rearrange("p (b d) -> p b d", d=65)
            recip(rd, accv[:, :, 64], [acck], [rk])
            tt("dve", o_a[:, qt * 4:(qt + 1) * 4, h * 64:(h + 1) * 64], accv[:, :, 0:64],
               rd[:, :, None].broadcast_to([128, 4, 64]), ALU.mult, [acck, rk], [("o_a", qt)])

    for i in range(len(stageA) + LOOK):
        if i % 8 == 3:
            precast_step()
        if i < len(stageA):
            emitA(stageA[i])
        if i >= LOOK:
            emitB(stageA[i - LOOK])
    if dbg:
        oa_dbg = dscr("oa_dbg", [S, 512], BF16)
        dma("sp", oa_dbg.rearrange("(b p) f -> p b f", p=128), o_a, [("o_a", q) for q in range(NQT)], ["oa_dbg"])
    sc.barrier()
    if stage <= 4:
        return finish(nc, es, sc, None)

    A.release(p2)
    qb_p = [A.alloc([S], BF16) for _ in range(2)]
    kb_p = [A.alloc([S], BF16) for _ in range(2)]
    vb_r = A.alloc([NB, 520], BF16)
    bias_r = A.alloc([8, 640], F32)
    Eb = [A.alloc([640], BF16) for _ in range(10)]
    stmp = [A.alloc([640], F32) for _ in range(4)]
    rden3 = [A.alloc([4], F32) for _ in range(2)]
    vb_v = vb_d.rearrange("(b p) f -> p b f", p=128)
    for q4 in range(4):
        dma("sp", vb_r[:, q4 * 8:(q4 + 1) * 8, :], vb_v[:, q4 * 8:(q4 + 1) * 8, :], (), [("vb_r", q4)])
    dma("sp", bias_r, biasT_d.rearrange("p (h q) -> p h q", h=8), (), ["bias_r"])
    for h_ in range(8):
        act(bias_r[:, h_, :], bias_r[:, h_, :], AF.Exp, ["bias_r"], ["bias_r"])
    sbank[0] = 0
    ecnt3 = 0
    stc = 0
    NJ = NB if stage > 5 else 8
    recs3 = [(h, j) for h in range(8) for j in range(NJ)]
    ering = {}
    st3 = dict(ecnt=0, stc=0, bs=0)

    def emitA3(rec):
        h, j = rec
        pr, po = h // 2, (h % 2) * 64
        qb_r, kb_r = qb_p[pr % 2], kb_p[pr % 2]
        if h % 2 == 0 and j == 0:
            dma("sp", qb_r, qbT_v[:, pr, :], (), [("qb_r", pr % 2)])
            dma("sp", kb_r, kbT_v[:, pr, :], (), [("kb_r", pr % 2)])
        nq = min(640, S - 128 * j)
        n1 = min(nq, 512)
        sb = sbank[0] % 4
        sbank[0] += 1
        psA, pkA = banks[sb], ("ps", sb)
        st_ = stmp[st3["stc"] % 4]
        stk = ("stmp", st3["stc"] % 4)
        st3["stc"] += 1
        mm(psA[:, 0:n1], kb_r[po:po + 64, 128 * j:128 * j + 128], qb_r[po:po + 64, 128 * j:128 * j + n1],
           True, True, [("kb_r", pr % 2), ("qb_r", pr % 2)], [pkA])
        act(st_[:, 0:n1], psA[:, 0:n1], AF.Exp, [pkA], [(stk, 0)], scale=0.125)
        if nq > 512:
            bslot = (4, 7)[st3["bs"] % 2]
            st3["bs"] += 1
            psB, pkB = banks[bslot], ("ps", bslot)
            mm(psB[:, 0:nq - 512], kb_r[po:po + 64, 128 * j:128 * j + 128],
               qb_r[po:po + 64, 128 * j + 512:128 * j + nq], True, True, [("kb_r", pr % 2), ("qb_r", pr % 2)], [pkB])
            act(st_[:, 512:nq], psB[:, 0:nq - 512], AF.Exp, [pkB], [(stk, 1)], scale=0.125)
        ei = st3["ecnt"] % len(Eb)
        st3["ecnt"] += 1
        Et, Ek = Eb[ei], ("Eb", ei)
        tt("dve" if st3["ecnt"] % 2 == 0 else "pool", Et[:, 0:nq], st_[:, 0:nq], bias_r[:, h, 0:nq], ALU.mult,
           [(stk, 0), (stk, 1), "bias_r"], [Ek])
        ering[(h, j)] = (Et, Ek)

    def emitB3(rec):
        h, j = rec
        if j % 4 == 0:
            ab = 5 + accn[0] % 2
            accn[0] += 1
            ering["acc"] = (banks[ab], ("ps", ab))
        acc, acck = ering["acc"]
        jj0 = max(0, j - 4)
        for jj in range(jj0, j + 1):
            Ej, Ejk = ering[(h, jj)]
            off = (j - jj) * 128
            mm(acc[:, (j % 4) * 65:(j % 4 + 1) * 65], Ej[:, off:off + 128], vb_r[:, jj, h * 65:(h + 1) * 65],
               (j % 4 == 0) and (jj == jj0), (j % 4 == 3) and (jj == j), [Ejk, ("vb_r", jj // 8)], [acck])
        if j % 4 == 3:
            rd = rden3[(j // 4) % 2]
            rk = ("rden3", (j // 4) % 2)
            accv = acc[:, 0:260].rearrange("p (b d) -> p b d", d=65)
            recip(rd, accv[:, :, 64], [acck], [rk])
            tt("dve", o_b[:, j - 3:j + 1, h * 64:(h + 1) * 64], accv[:, :, 0:64],
               rd[:, :, None].broadcast_to([128, 4, 64]), ALU.mult, [acck, rk], [("o_b", j // 4)])

    LOOK3 = 3
    for i in range(len(recs3) + LOOK3):
        if i % 5 == 2:
            precast_step()
        if i < len(recs3):
            emitA3(recs3[i])
        if i >= LOOK3:
            emitB3(recs3[i - LOOK3])
    if dbg:
        ob_dbg = dscr("ob_dbg", [S, 512], BF16)
        dma("sp", ob_dbg.rearrange("(b p) f -> p b f", p=128), o_b, [("o_b", q) for q in range(NJ // 4)], ["ob_dbg"])
    sc.barrier()
    if stage <= 6:
        return finish(nc, es, sc, None)

    A.release(p2)
    cw_all = A.alloc([NB, 2], F32)
    e_all = A.alloc([NB, 2], F32)
    r_all = A.alloc([NB, 2], F32)
    A1all = A.alloc([NB, 32], F32)
    A2all = A.alloc([NB, 32], F32)
    carry = A.alloc([32], F32)
    iota_e = A.alloc([32], F32)
    rbias = A.alloc([36], F32)
    Lst = A.alloc([128], BF16)
    p4 = A.mark()
    woa_b = A.alloc([4, D], BF16)
    wob_b = A.alloc([4, D], BF16)
    wout_b = A.alloc([8, D], BF16)
    wr_f = A.alloc([8, 36], F32)
    for c in range(4):
        dma("pool", woa_b[:, c, :], woa_d.rearrange("p (k n) -> p k n", k=4)[:, c, :], (), [("woa", c)])
        dma("pool", wob_b[:, c, :], wob_d.rearrange("p (k n) -> p k n", k=4)[:, c, :], (), [("wob", c)])
    WOA = [("woa", c) for c in range(4)]
    WOB = [("wob", c) for c in range(4)]
    WOUT = [("wout", c) for c in range(8)]
    dma("sp", wr_f, wr_d.rearrange("p (k n) -> p k n", k=8), (), ["wr_f"])
    dma("sp", rbias, rb_d[0:1, :].to_broadcast([128, 36]), (), ["rbias"])
    dma("sp", iota_e, iota_d, (), ["iota_e"])
    ltmp = A.alloc([128], F32)
    dma("sp", ltmp, lst_d, (), ["ltmp"])
    tcopy("dve", Lst, ltmp, ["ltmp"], ["Lst"])
    memset("dve", carry, 0.0, ["carry"])

    oaT2 = [A.alloc([4, TT], BF16) for _ in range(2)]
    obT2 = [A.alloc([4, TT], BF16) for _ in range(2)]
    mT = A.alloc([8, TT], BF16)
    gat = [A.alloc([TT], F32) for _ in range(2)]
    gbt = [A.alloc([TT], F32) for _ in range(2)]
    mt1 = [A.alloc([TT], F32) for _ in range(2)]
    mt2 = [A.alloc([TT], F32) for _ in range(2)]
    R4 = 4
    RX = 3
    xr = [A.alloc([D], F32) for _ in range(RX)]
    uu = [A.alloc([D], F32) for _ in range(R4)]
    h2b = [A.alloc([D], BF16)] * 2
    h2T = [A.alloc([8, 128], F32)] * 2
    sm = A.alloc([16], F32)
    lg = A.alloc([4, 36], F32)
    dg = A.alloc([4, 4], F32)
    ge = A.alloc([4, 4], F32)
    pen = A.alloc([4, 4], F32)
    msk = A.alloc([4, 32], F32)
    top8a = A.alloc([4, 8], F32)
    idx8a = A.alloc([4, 8], U32)
    r4s = A.alloc([8, 4], F32)
    Ab = A.alloc([4, 32], BF16)
    Pt = A.alloc([4, 32], F32)
    ptm = A.alloc([4, 32], F32)
    woutv = wout_d.rearrange("p (k n) -> p k n", k=8)
    for c in range(8):
        stb = xr[c % RX]
        stk_ = [("x1", c % RX, 0), ("x1", c % RX, 1)]
        dma("sp", stb, woutv[:, c, :], (), stk_)
        tt("dve", wout_b[:, c, :], stb, gt1_bc, ALU.mult, stk_, [("wout", c)])
    RB = 6
    rot = [0]

    def ps_rot():
        i = rot[0] % RB
        rot[0] += 1
        return banks[i], ("ps", i)

    pp, pkp = banks[6], ("ps", 6)
    rl, pkr = banks[7], ("ps", 7)
    NT4 = NT if stage > 7 else 1

    def S1(ti, bi):
        blk = ti * 4 + bi
        t0 = ti * TT
        r3 = blk % R4
        rx = blk % RX
        g2 = blk % 2
        xk = [("x1", rx, 0), ("x1", rx, 1)]
        dma("sp", xr[rx], x_d[t0 + bi * 128:t0 + (bi + 1) * 128, :], (), xk)
        for half in range(2):
            pm, pkm = ps_rot()
            for m in range(8):
                mm(pm, mT[:, m, bi * 128:(bi + 1) * 128], wout_b[:, m, half * 512:(half + 1) * 512],
                   m == 0, m == 7, [("mT", m_) for m_ in range(8)] + WOUT, [pkm])
            hs = slice(half * 512, (half + 1) * 512)
            tt("dve", xr[rx][:, hs], pm, xr[rx][:, hs], ALU.add, [pkm, ("x1", rx, half)], [("x1", rx, half)])
        dma("sp", x1_d[t0 + bi * 128:t0 + (bi + 1) * 128, :], xr[rx], xk, [("x1_d", blk)])
        act(h2b[g2], xr[rx], AF.Square, xk, [("ssq4", g2), ("h2b", 0)], accum_out=sm[:, g2:g2 + 1])
        act(sm[:, 2 + g2:3 + g2], sm[:, g2:g2 + 1], AF.Ln, [("ssq4", g2), "eps_c"], [("rs4", g2)],
            scale=1.0 / D, bias=eps_c[:, 0:1])
        act(sm[:, 4 + g2:5 + g2], sm[:, 2 + g2:3 + g2], AF.Exp, [("rs4", g2)], [("rs4b", g2)], scale=-0.5)
        tt("pool", uu[r3], xr[rx], s2_bc, ALU.mult, xk, [("uu", r3)])
        stt(uu[r3], uu[r3], sm[:, 4 + g2:5 + g2], b2_bc, ALU.mult, ALU.add, [("uu", r3), ("rs4b", g2)], [("uu", r3)])
        tcopy("act", h2b[g2], uu[r3], [("uu", r3), ("ssq4", g2)], [("h2b", 0)])
        dma("sp", h2_d[t0 + bi * 128:t0 + (bi + 1) * 128, :], h2b[g2], [("h2b", 0)], [("h2_d", blk)])

    def S2a(ti, bi):
        blk = ti * 4 + bi
        r3 = blk % R4
        hb = blk % 2
        for half in range(2):
            pb, pk = ps_rot()
            for q in range(4):
                kc = half * 4 + q
                tr(pb[:, q * 128:(q + 1) * 128], uu[r3][:, kc * 128:(kc + 1) * 128], ident_f, [("uu", r3)], [pk])
            evac_copy(h2T[hb][:, half * 4:(half + 1) * 4, :], pb.rearrange("p (q t) -> p q t", q=4), [pk],
                      [("h2T", 0, half)])
        for kc in range(8):
            mm(rl[:, bi * 36:(bi + 1) * 36], h2T[hb][:, kc, :], wr_f[:, kc, :], (bi == 0) and (kc == 0),
               (bi == 3) and (kc == 7), [("h2T", 0, kc // 4), "wr_f"], [pkr])

    def dve(fn, reads, writes):
        sc.add("dve", fn, reads, writes)

    def S2b1(ti):
        b0 = ti * 4
        bs = slice(b0, b0 + 4)
        tt("dve", lg, rl[:, 0:144].rearrange("p (b n) -> p b n", n=36), rbias[:, None, :].broadcast_to([128, 4, 36]),
           ALU.add, [pkr, "rbias"], ["lg"])
        gmax = r4s[:, 0, :]
        dve(lambda e: e.tensor_reduce(out=gmax, in_=lg[:, :, 0:4], axis=AX.X, op=ALU.max), ["lg"], ["gmax"])
        tt("dve", dg, lg[:, :, 0:4], gmax[:, :, None].broadcast_to([128, 4, 4]), ALU.subtract, ["lg", "gmax"], ["dg"])
        act(ge, dg, AF.Exp, ["dg"], ["ge"])
        gsum = r4s[:, 1, :]
        dve(lambda e: e.tensor_reduce(out=gsum, in_=ge, axis=AX.X, op=ALU.add), ["ge"], ["gsum"])
        gw = r4s[:, 2, :]
        recip(gw, gsum, ["gsum"], ["gw"])
        ts("dve", pen, dg, 0.0, None, ALU.is_equal, None, ["dg"], ["pen"])
        ts("dve", pen, pen, -1.0, 1e30, ALU.add, ALU.mult, ["pen"], ["pen"])
        tt("dve", msk.rearrange("p b (g e) -> p b g e", e=8), lg[:, :, 4:36].rearrange("p b (g e) -> p b g e", e=8),
           pen[:, :, :, None].broadcast_to([128, 4, 4, 8]), ALU.add, ["lg", "pen"], ["msk"])
        for b in range(4):
            dve(lambda e, b=b: e.max(out=top8a[:, b, :], in_=msk[:, b, :]), ["msk"], [("top8", b)])
            dve(lambda e, b=b: e.max_index(out=idx8a[:, b, :], in_max=top8a[:, b, :], in_values=msk[:, b, :]),
                ["msk", ("top8", b)], [("idx8", b)])
        T8 = [("top8", b) for b in range(4)]
        I8 = [("idx8", b) for b in range(4)]
        tcopy("dve", e_all[:, bs, :], idx8a[:, :, 0:2], I8, [("e_all", ti)])
        dlt = r4s[:, 3, :]
        tt("dve", dlt, top8a[:, :, 1], top8a[:, :, 0], ALU.subtract, T8, ["dlt"])
        ex_ = r4s[:, 4, :]
        act(ex_, dlt, AF.Exp, ["dlt"], ["ex_"])
        den = r4s[:, 5, :]
        ts("dve", den, ex_, 1.0, None, ALU.add, None, ["ex_"], ["den"])
        recip(den, den, ["den"], ["den"])
        tt("dve", cw_all[:, bs, 0], den, gw, ALU.mult, ["den", "gw"], [("cw", ti, 0)])
        tt("dve", ex_, ex_, den, ALU.mult, ["ex_", "den"], ["ex_"])
        tt("dve", cw_all[:, bs, 1], ex_, gw, ALU.mult, ["ex_", "gw"], [("cw", ti, 1)])
        iob = iota_e[:, None, :].broadcast_to([128, 4, 32])
        tt("dve", A1all[:, bs, :], iob, e_all[:, bs, 0:1].broadcast_to([128, 4, 32]), ALU.is_equal,
           ["iota_e", ("e_all", ti)], [("A1", ti)])
        tt("dve", A2all[:, bs, :], iob, e_all[:, bs, 1:2].broadcast_to([128, 4, 32]), ALU.is_equal,
           ["iota_e", ("e_all", ti)], [("A2", ti)])
        tt("dve", Ab, A1all[:, bs, :], A2all[:, bs, :], ALU.add, [("A1", ti), ("A2", ti)], ["Ab"])

    def S2b2(ti):
        b0 = ti * 4
        bs = slice(b0, b0 + 4)
        n_mm = 0
        tot_mm = 4 + 6 + 4
        for b in range(4):
            mm(pp[:, b * 32:(b + 1) * 32], Lst, Ab[:, b, :], n_mm == 0, False, ["Lst", "Ab"], [pkp])
            n_mm += 1
            for b_ in range(b):
                mm(pp[:, b * 32:(b + 1) * 32], ones_b, Ab[:, b_, :], False, False, ["ones_b", "Ab"], [pkp])
                n_mm += 1
        for b in range(4):
            n_mm += 1
            mm(pp[:, 128:160], ones_b, Ab[:, b, :], False, n_mm == tot_mm, ["ones_b", "Ab"], [pkp])
        tt("dve", Pt, pp[:, 0:128].rearrange("p (b n) -> p b n", n=32), carry[:, None, :].broadcast_to([128, 4, 32]),
           ALU.add, [pkp, "carry"], ["Pt"])
        tt("dve", carry, pp[:, 128:160], carry, ALU.add, [pkp, "carry", "Pt"], ["carry"])
        tt("dve", ptm, Pt, A1all[:, bs, :], ALU.mult, ["Pt", ("A1", ti)], ["ptm"])
        dve(lambda e: e.tensor_reduce(out=r_all[:, bs, 0], in_=ptm, axis=AX.X, op=ALU.add), ["ptm"], [("r_all", ti, 0)])
        tt("dve", ptm, Pt, A2all[:, bs, :], ALU.mult, ["Pt", ("A2", ti), ("r_all", ti, 0)], ["ptm"])
        dve(lambda e: e.tensor_reduce(out=r_all[:, bs, 1], in_=ptm, axis=AX.X, op=ALU.add), ["ptm"], [("r_all", ti, 1)])

    gcn4 = [0]

    def otrans(ti):
        ob2 = ti % 2
        for (osrc, odst, onm) in ((o_a, oaT2[ob2], "oaT"), (o_b, obT2[ob2], "obT")):
            for c in range(4):
                pb, pk = ps_rot()
                pbb = pb.bitcast(BF16)
                for bi in range(4):
                    tr(pbb[:, bi * 128:(bi + 1) * 128], osrc[:, ti * 4 + bi, c * 128:(c + 1) * 128], ident_b,
                       [("o_x",), "ident_b"], [pk])
                evac_copy(odst[:, c, :], pbb[:, 0:TT], [pk], [(onm, ob2, c)])

    def projmerge(ti):
        t0 = ti * TT
        ob2 = ti % 2
        oaT, obT = oaT2[ob2], obT2[ob2]
        for m in range(8):
            gi = gcn4[0] % 2
            gcn4[0] += 1
            dma("sp", gat[gi], ga_v[:, m, t0:t0 + TT], (), [("gat", gi)])
            dma("sp", gbt[gi], gb_v[:, m, t0:t0 + TT], (), [("gbt", gi)])
            pa, pka = ps_rot()
            for c in range(4):
                mm(pa, woa_b[:, c, m * 128:(m + 1) * 128], oaT[:, c, :], c == 0, c == 3, WOA + [("oaT", ob2, c)], [pka])
            pbk, pkb = ps_rot()
            for c in range(4):
                mm(pbk, wob_b[:, c, m * 128:(m + 1) * 128], obT[:, c, :], c == 0, c == 3, WOB + [("obT", ob2, c)], [pkb])
            i2 = m % 2
            tt("dve", mt1[i2], pa, gat[gi], ALU.mult, [pka, ("gat", gi)], [("mt1", i2)])
            tt("dve", mt2[i2], pbk, gbt[gi], ALU.mult, [pkb, ("gbt", gi)], [("mt2", i2)])
            tt("pool", mT[:, m, :], mt1[i2], mt2[i2], ALU.add, [("mt1", i2), ("mt2", i2)], [("mT", m)])

    otrans(0)
    projmerge(0)
    if NT4 > 1:
        otrans(1)
    for ti in range(NT4):
        for bi in range(4):
            S1(ti, bi)
        if ti >= 1:
            S2b2(ti - 1)
        if ti + 1 < NT4:
            projmerge(ti + 1)
        for bi in range(4):
            S2a(ti, bi)
        if ti + 2 < NT4:
            otrans(ti + 2)
        S2b1(ti)
    S2b2(NT4 - 1)
    if dbg:
        rt_dbg = dscr("rt_dbg", [128, NB * 6], F32)
        rv = rt_dbg.rearrange("p (b s) -> p b s", s=6)
        ALLK = [("cw", t_, k_) for t_ in range(NT4) for k_ in range(2)] + [("e_all", t_) for t_ in range(NT4)] + [("r_all", t_, k_) for t_ in range(NT4) for k_ in range(2)]
        dma("sp", rv[:, :, 0:2], cw_all, ALLK, ["rt1"])
        dma("sp", rv[:, :, 2:4], e_all, ALLK, ["rt2"])
        dma("sp", rv[:, :, 4:6], r_all, ALLK, ["rt3"])
    sc.barrier()
    if stage <= 8:
        return finish(nc, es, sc, None)

    A.release(p4)
    padf = A.alloc([32], F32)
    padi = A.alloc([32], I32)
    incl = A.alloc([32], F32)
    offs = A.alloc([32], F32)
    ones32 = A.alloc([32], F32)
    big3 = A.alloc([NTILE, 32], F32)
    slotf = A.alloc([NB, 2], F32)
    slot_i = A.alloc([NB * 2], I32)
    jv = A.alloc([NTILE], F32)
    pidx = A.alloc([1], F32)
    tef = A.alloc([NTILE], F32)
    tesh = A.alloc([NTILE], F32)
    widx_i = A.alloc([NTILE], I32)
    p5 = A.mark()
    dma("sp", jv, jv_d, (), ["jv"])
    dma("sp", pidx, pidx_d, (), ["pidx"])
    memset("dve", ones32, 1.0, ["ones32"])
    ts("dve", padf, carry, float(SLOT_T - 1), None, ALU.add, None, ["carry"], ["padf"])
    tcopy("dve", padi, padf, ["padf"], ["padi"])
    ts("dve", padi, padi, SH, None, ALU.arith_shift_right, None, ["padi"], ["padi"])
    ts("dve", padi, padi, SH, None, ALU.logical_shift_left, None, ["padi"], ["padi"])
    tcopy("dve", padf, padi, ["padi"], ["padf"])
    sc.add("dve", lambda e: e.tensor_tensor_scan(out=incl, data0=ones32, data1=padf, initial=0.0,
                                                  op0=ALU.mult, op1=ALU.add), ["ones32", "padf"], ["incl"])
    tt("dve", offs, incl, padf, ALU.subtract, ["incl", "padf"], ["offs"])
    for k, Aall in ((0, A1all), (1, A2all)):
        tt("dve", big3[:, 0:NB, :], Aall, offs[:, None, :].broadcast_to([128, NB, 32]), ALU.mult, ["offs"], ["big3"])
        sc.add("dve", lambda e, k=k: e.tensor_reduce(out=slotf[:, :, k], in_=big3[:, 0:NB, :], axis=AX.X, op=ALU.add),
               ["big3"], [("slotf", k)])
    tt("dve", slotf, slotf, r_all, ALU.add, [("slotf", 0), ("slotf", 1)], ["slotf"])
    tcopy("dve", slot_i, slotf.rearrange("p b k -> p (b k)"), ["slotf"], ["slot_i"])
    tt("dve", big3, incl[:, None, :].broadcast_to([128, NTILE, 32]), jv[:, :, None].broadcast_to([128, NTILE, 32]),
       ALU.is_le, ["incl", "jv", ("slotf", 0), ("slotf", 1)], ["big3"])
    sc.add("dve", lambda e: e.tensor_reduce(out=tef, in_=big3, axis=AX.X, op=ALU.add), ["big3"], ["tef"])
    ts("dve", tef, tef, 31.0, None, ALU.min, None, ["tef"], ["tef"])
    memset("dve", tesh, -1.0, ["tesh"])
    tcopy("dve", tesh[:, 3:NTILE], tef[:, 0:NTILE - 3], ["tef", "tesh"], ["tesh"])
    tt("dve", tesh, tesh, tef, ALU.is_equal, ["tesh", "tef"], ["tesh"])
    ts("dve", tef, tef, 128.0, pidx[:, 0:1], ALU.mult, ALU.add, ["tef", "pidx"], ["tef"])
    stt(tef, tesh, 100000.0, tef, ALU.mult, ALU.add, ["tesh", "tef"], ["tef"])
    tcopy("dve", widx_i, tef, ["tef"], ["widx_i"])
    if dbg:
        sl_dbg = dscr("sl_dbg", [128, NB * 2 + NTILE], I32)
        dma("sp", sl_dbg[:, 0:NB * 2], slot_i, ["slot_i"], ["sl1"])
        dma("sp", sl_dbg[:, NB * 2:], widx_i, ["widx_i"], ["sl2"])
    hrow = [A.alloc([D], BF16) for _ in range(3)]
    for blk in range(NB):
        hr = hrow[blk % 3]
        dma("sp", hr, h2_d[blk * 128:(blk + 1) * 128, :], (), [("hrow", blk % 3)])
        for k in range(2):
            off_ap = slot_i[:, blk * 2 + k:blk * 2 + k + 1]
            sc.add("pool", lambda e, hr=hr, off_ap=off_ap: e.indirect_dma_start(
                out=xs_d[:, :], out_offset=bass.IndirectOffsetOnAxis(ap=off_ap, axis=0), in_=hr, in_offset=None),
                [("hrow", blk % 3), "slot_i"], [("xs_d", blk, k)], dma=True)
    sc.barrier()
    if stage <= 9:
        return finish(nc, es, sc, None)

    A.release(p5)
    while pre_pos[0] < len(pre_steps):
        precast_step()
    NW = 3
    wall_t = [A.alloc([6144], BF16) for _ in range(NW)]
    wg_t = [w[:, 0:2048] for w in wall_t]
    wu_t = [w[:, 2048:4096] for w in wall_t]
    wd_t = [w[:, 4096:6144] for w in wall_t]
    xs_t = [A.alloc([2, D], BF16) for _ in range(NW)]
    xsT = [A.alloc([8, SLOT_T], BF16) for _ in range(2)]
    sil = [A.alloc([2, SLOT_T], F32) for _ in range(2)]
    hidT = [A.alloc([2, SLOT_T], BF16) for _ in range(2)]
    yt = [A.alloc([D], F32) for _ in range(3)]
    ycn = 0
    breg = {}
    NTL = NTILE if stage > 10 else 4
    WXALL = [("wx", mi, e_) for mi in range(3) for e_ in range(32)]

    def prefetch6(j):
        b3 = j % NW
        wi = widx_i[:, j:j + 1]

        def wgather(e, wi=wi, b3=b3):
            if "r" not in breg:
                breg["r"] = e.alloc_register("wbound")
                e.reg_mov(breg["r"], 32 * 128 - 1)
            return e.indirect_dma_start(
                out=wall_t[b3], out_offset=None, in_=wx_d[:, :], in_offset=bass.IndirectOffsetOnAxis(ap=wi, axis=0),
                bounds_check=breg["r"], oob_is_err=False)
        sc.add("pool", wgather, (), [("wg", b3), ("wu", b3), ("wd", b3)], dma=True)
        dma("sp", xs_t[b3], xs_d[j * SLOT_T:(j + 1) * SLOT_T, :].rearrange("(s p) f -> p s f", p=128), (), [("xs_t", b3)])

    for j in range(min(2, NTL)):
        prefetch6(j)
    for j in range(NTL):
        if j + 2 < NTL:
            prefetch6(j + 2)
        b2 = j % 2
        b3 = j % NW
        for sbk in range(2):
            for half in range(2):
                pb, pk = ps_next()
                pbb = pb.bitcast(BF16)
                for q in range(4):
                    kc = half * 4 + q
                    tr(pbb[:, q * 128:(q + 1) * 128], xs_t[b3][:, sbk, kc * 128:(kc + 1) * 128], ident_b,
                       [("xs_t", b3), "ident_b"], [pk])
                evac_copy(xsT[b2][:, half * 4:(half + 1) * 4, sbk * 128:(sbk + 1) * 128],
                          pbb[:, 0:512].rearrange("p (q t) -> p q t", q=4), [pk], [("xsT", b2, sbk, half)])
        XST = [("xsT", b2, a, b) for a in range(2) for b in range(2)]
        pg, pkg = ps_next()
        for f in range(2):
            for kc in range(8):
                mm(pg[:, f * SLOT_T:(f + 1) * SLOT_T], wg_t[b3][:, kc * 256 + f * 128:kc * 256 + (f + 1) * 128],
                   xsT[b2][:, kc, :], kc == 0, kc == 7, [("wg", b3)] + XST, [pkg])
        pu, pku = ps_next()
        for f in range(2):
            for kc in range(8):
                mm(pu[:, f * SLOT_T:(f + 1) * SLOT_T], wu_t[b3][:, kc * 256 + f * 128:kc * 256 + (f + 1) * 128],
                   xsT[b2][:, kc, :], kc == 0, kc == 7, [("wu", b3)] + XST, [pku])
        act(sil[b2].rearrange("p f s -> p (f s)"), pg, AF.Silu, [pkg], [("sil", b2)])
        tt("dve", hidT[b2].rearrange("p f s -> p (f s)"), pu, sil[b2].rearrange("p f s -> p (f s)"), ALU.mult,
           [pku, ("sil", b2)], [("hidT", b2)])
        for sbk in range(2):
            y_ = yt[ycn % 3]
            yk = ("yt", ycn % 3)
            ycn += 1
            for half in range(2):
                py, pky = ps_next()
                for f in range(2):
                    mm(py, hidT[b2][:, f, sbk * 128:(sbk + 1) * 128], wd_t[b3][:, f * 1024 + half * 512:f * 1024 + (half + 1) * 512],
                       f == 0, f == 1, [("hidT", b2), ("wd", b3)], [pky])
                tt("dve", y_[:, half * 512:(half + 1) * 512], py, gt2_bc[:, half * 512:(half + 1) * 512], ALU.mult,
                   [pky], [(yk, half)])
            dma("sp", ys_d[j * SLOT_T + sbk * 128:j * SLOT_T + (sbk + 1) * 128, :], y_, [(yk, 0), (yk, 1)], [("ys_d", j, sbk)])
    sc.barrier()
    if stage <= 11:
        return finish(nc, es, sc, None)

    A.release(p5)
    gfin = A.alloc([D], F32)
    dma("sp", gfin, gfin_d[0:1, :].to_broadcast([128, D]), (), ["gfin"])
    R7 = 4
    g1 = [A.alloc([D], F32) for _ in range(R7)]
    g2_ = [A.alloc([D], F32) for _ in range(R7)]
    x1r = [A.alloc([D], F32) for _ in range(R7)]
    ot = [A.alloc([D], F32) for _ in range(2)]
    junk7 = A.alloc([D], BF16)
    sm7 = A.alloc([8], F32)

    def load7(blk):
        i4 = blk % R7
        for k, gt_ in ((0, g1), (1, g2_)):
            off_ap = slot_i[:, blk * 2 + k:blk * 2 + k + 1]
            sc.add("pool", lambda e, gt_=gt_, off_ap=off_ap, i4=i4: e.indirect_dma_start(
                out=gt_[i4], out_offset=None, in_=ys_d[:, :], in_offset=bass.IndirectOffsetOnAxis(ap=off_ap, axis=0)),
                (), [("g", k, i4)], dma=True)
        dma("sp", x1r[i4], x1_d[blk * 128:(blk + 1) * 128, :], (), [("x1r", i4)])

    for blk in range(2):
        load7(blk)
    for blk in range(NB):
        if blk + 2 < NB:
            load7(blk + 2)
        i4 = blk % R7
        i2 = blk % 2
        stt(x1r[i4], g1[i4], cw_all[:, blk, 0:1], x1r[i4], ALU.mult, ALU.add, [("g", 0, i4), ("x1r", i4)], [("x1r", i4)])
        stt(x1r[i4], g2_[i4], cw_all[:, blk, 1:2], x1r[i4], ALU.mult, ALU.add, [("g", 1, i4), ("x1r", i4)], [("x1r", i4)])
        act(junk7, x1r[i4], AF.Square, [("x1r", i4)], [("ss7", i2)], accum_out=sm7[:, i2:i2 + 1])
        act(sm7[:, 2 + i2:3 + i2], sm7[:, i2:i2 + 1], AF.Sqrt, [("ss7", i2)], [("rs7", i2)], scale=1.0 / D, bias=eps_c[:, 0:1])
        recip(sm7[:, 2 + i2:3 + i2], sm7[:, 2 + i2:3 + i2], [("rs7", i2)], [("rs7", i2)])
        stt(ot[i2], x1r[i4], sm7[:, 2 + i2:3 + i2], gfin, ALU.mult, ALU.mult, [("x1r", i4), ("rs7", i2), "gfin"], [("ot", i2)])
        dma("sp", out_d[blk * 128:(blk + 1) * 128, :], ot[i2], [("ot", i2)], [("out", blk)])
    sc.barrier()
    return finish(nc, es, sc, None)


def finish(nc, es, sc, _):
    block = es.enter_context(nc.Block())
    sc.emit(block)
    es.close()
    return nc


def fm(v, k):
    return np.ascontiguousarray(np.asarray(v, np.float32).reshape(k, 128).T)


def kmajor(w):
    K, N = w.shape
    return np.ascontiguousarray(w.reshape(K // 128, 128, N).transpose(1, 0, 2).reshape(128, (K // 128) * N))


def host_inputs(I):
    shared = {}
    shared["wada"] = kmajor(I["w_ada"][0])
    shared["bada_row"] = np.ascontiguousarray(I["b_ada"][0].reshape(1, 6144).astype(np.float32))
    shared["gmix_row"] = np.ascontiguousarray(I["g_mix"][0].reshape(1, D).astype(np.float32))
    shared["gffn_row"] = np.ascontiguousarray(I["g_ffn"][0].reshape(1, D).astype(np.float32))
    shared["gfinal_row"] = np.ascontiguousarray(I["g_final"].reshape(1, D).astype(np.float32))
    w_in = I["w_in"][0]
    kr = w_in[:, 384:416]
    z64 = np.zeros((D, 64), np.float32)
    kr_sw = np.concatenate([kr[:, 16:32], kr[:, 0:16]], axis=1)
    win_ext = np.concatenate([w_in, z64, kr, z64, kr_sw], axis=1)
    assert win_ext.shape[1] == WC
    shared["win"] = kmajor(win_ext)
    shared["gq_fm"] = fm(I["g_q"][0], 2)
    shared["gkv_fm"] = fm(I["g_kv"][0], 1)
    wuq = I["w_uq"][0]
    wuq_sw = wuq.reshape(256, 8, 96).copy()
    wuq_sw[:, :, 64:80] = wuq.reshape(256, 8, 96)[:, :, 80:96]
    wuq_sw[:, :, 80:96] = wuq.reshape(256, 8, 96)[:, :, 64:80]
    shared["wuq"] = kmajor(wuq)
    shared["wuq_sw"] = kmajor(wuq_sw.reshape(256, 768))
    shared["wuk"] = np.ascontiguousarray(I["w_uk"][0])
    shared["wuv"] = np.ascontiguousarray(I["w_uv"][0])
    rc = np.zeros((128, 2), np.float32)
    inv = (10000.0 ** (-np.arange(16, dtype=np.float32) / 16)).astype(np.float32)
    for p in range(128):
        rc[p, 0] = inv[p % 16]
    shared["rconst"] = rc
    shared["ident"] = np.eye(128, dtype=np.float32)
    sel = np.zeros((128, 96), np.float32)
    for p in range(64, 96):
        sel[p, p] = 1.0
    shared["sel"] = sel
    kk = np.arange(128)[:, None]
    qq = np.arange(640)[None, :]
    idx = np.clip(qq - kk, -256, 256) + 256
    dq = qq // 64 - kk // 64
    valid = (dq >= 0) & (dq <= 8)
    rb = I["rel_bias"][0]
    bt = np.where(valid[None], rb[:, idx], np.float32(-1e30)).astype(np.float32)
    shared["biasT"] = np.ascontiguousarray(bt.transpose(1, 0, 2).reshape(128, 8 * 640))
    shared["woa"] = kmajor(I["w_oa"][0])
    shared["wob"] = kmajor(I["w_ob"][0])
    shared["wout"] = kmajor(I["w_out"][0])
    shared["wr"] = kmajor(np.concatenate([I["w_rg"][0], I["w_re"][0]], axis=1))
    shared["rb"] = np.ascontiguousarray(np.concatenate([I["b_rg"][0], I["b_re"][0]]).reshape(1, 36).astype(np.float32))
    shared["iota_e"] = np.ascontiguousarray(np.broadcast_to(np.arange(32, dtype=np.float32), (128, 32)))
    shared["lst"] = np.triu(np.ones((128, 128), np.float32), 1)
    shared["jv"] = np.ascontiguousarray(np.broadcast_to((np.arange(NTILE, dtype=np.float32) * SLOT_T), (128, NTILE)))
    shared["pidx"] = np.arange(128, dtype=np.float32).reshape(128, 1)
    shared["wgl"] = np.ascontiguousarray(I["w_gate"][0].reshape(32, 8, 128, 256).transpose(0, 2, 1, 3).reshape(32 * 128, 2048))
    shared["wul"] = np.ascontiguousarray(I["w_up"][0].reshape(32, 8, 128, 256).transpose(0, 2, 1, 3).reshape(32 * 128, 2048))
    shared["wdl"] = np.ascontiguousarray(I["w_down"][0].reshape(32, 2, 128, 1024).transpose(0, 2, 1, 3).reshape(32 * 128, 2048))
    per_core = []
    for b in range(8):
        d = dict(shared)
        d["x"] = np.ascontiguousarray(I["x"][b])
        d["cfm"] = fm(I["c"][b], 8)
        d["pos"] = np.ascontiguousarray(I["positions"][b].reshape(1, S).astype(np.int32))
        per_core.append(d)
    return per_core


_NC_CACHE = {}


def kernel(**inputs):
    I = {k: np.asarray(v) for k, v in inputs.items()}
    in_maps = host_inputs(I)
    if "nc" not in _NC_CACHE:
        _NC_CACHE["nc"] = build_nc()
    nc = _NC_CACHE["nc"]
    res = run_bass_kernel_spmd(nc, in_maps, core_ids=list(range(8)))
    return np.stack([r["out"] for r in res.results], axis=0).astype(np.float32)
```

```python
import os
import math
from contextlib import ExitStack

import numpy as np
import concourse.bass as bass
import concourse.mybir as mybir
from concourse.bass_utils import run_bass_kernel_spmd

F32 = mybir.dt.float32
BF16 = mybir.dt.bfloat16
I32 = mybir.dt.int32
U32 = mybir.dt.uint32
U8 = mybir.dt.uint8
AF = mybir.ActivationFunctionType
ALU = mybir.AluOpType
AX = mybir.AxisListType

S = 4096
D = 1024
TT = 512
NT = S // TT
NB = S // 128
EPS = 1e-6
WC = 4192
C_QB, C_KB, C_VB, C_GA, C_GB, C_KRP, C_KRS = 416, 928, 1440, 1952, 2976, 4000, 4096
PI = math.pi
SLOT_T = 256
SH = 8
NTILE = 64
NSLOT = NTILE * SLOT_T


class Sched:
    COMPUTE = ("pe", "act", "dve", "pool")

    def __init__(self, nc, es, n_dma_sems=8):
        self.nc = nc
        self.ops = []
        self.n_dma_sems = n_dma_sems
        self.eng_sem = {e: es.enter_context(nc.semaphore("c_" + e)) for e in self.COMPUTE}
        self.dma_sems = {q: [es.enter_context(nc.semaphore("d_%s%d" % (q, i))) for i in range(n_dma_sems)]
                         for q in ("sp", "pool", "act")}
        self.state = {}
        self.last_on = {}
        self.dma_since_bar = []

    def add(self, eng, fn, reads=(), writes=(), dma=False, extra_deps=()):
        op = dict(eng=eng, fn=fn, dma=dma, idx=len(self.ops), signal=False)
        deps = set(extra_deps)
        st = self.state
        for k in reads:
            w, rd = st.setdefault(k, [None, {}])
            if w is not None:
                deps.add(w)
        for k in writes:
            w, rd = st.setdefault(k, [None, {}])
            if w is not None:
                deps.add(w)
            deps.update(rd.values())
        me = (eng, "dma", op["idx"]) if dma else eng
        for k in reads:
            st[k][1][me] = op["idx"]
        for k in writes:
            st[k][0] = op["idx"]
            st[k][1] = {}
        real = set()
        for d in deps:
            dop = self.ops[d]
            if (not dop["dma"]) and (not dma) and dop["eng"] == "pe" and eng == "pe":
                continue
            real.add(d)
            dop["signal"] = True
        op["deps"] = real
        self.ops.append(op)
        if dma:
            self.dma_since_bar.append(op["idx"])
        elif fn is not None:
            self.last_on[eng] = op["idx"]
        return op

    def barrier(self):
        lasts = dict(self.last_on)
        dmas = list(self.dma_since_bar)
        self.dma_since_bar = []
        for eng in ("pe", "act", "dve", "pool", "sp"):
            deps = [v for e, v in lasts.items() if e != eng] + dmas
            self.add(eng, None, extra_deps=deps)
        self.state = {}

    def emit(self, block):
        cnt = {e: 0 for e in self.COMPUTE}
        dcnt = {q: 0 for q in self.dma_sems}
        for op in self.ops:
            if not op["signal"]:
                continue
            if op["dma"]:
                q = op["eng"]
                i = dcnt[q]
                dcnt[q] += 1
                op["sem"] = self.dma_sems[q][i % self.n_dma_sems]
                op["val"] = 16 * (i // self.n_dma_sems + 1)
            else:
                e = op["eng"]
                assert op["fn"] is not None
                cnt[e] += 1
                op["sem"] = self.eng_sem[e]
                op["val"] = cnt[e]
        self.stats = dict(cnt=cnt, dcnt=dcnt, nops=len(self.ops))
        per_eng = {e: [] for e in ("pe", "act", "dve", "pool", "sp")}
        for op in self.ops:
            per_eng[op["eng"]].append(op)
        ops = self.ops

        def run(eng_name, engine):
            seen = {}
            for op in per_eng[eng_name]:
                need = {}
                for d in op["deps"]:
                    dop = ops[d]
                    s, v = dop["sem"], dop["val"]
                    if need.get(s.num, (None, 0))[1] < v:
                        need[s.num] = (s, v)
                for num, (s, v) in need.items():
                    if seen.get(num, 0) >= v:
                        continue
                    engine.wait_ge(s, v)
                    seen[num] = v
                if op["fn"] is None:
                    continue
                ins = op["fn"](engine)
                if op["signal"]:
                    ins.then_inc(op["sem"], 16 if op["dma"] else 1)

        block.tensor(lambda e: run("pe", e))
        block.scalar(lambda e: run("act", e))
        block.vector(lambda e: run("dve", e))
        block.gpsimd(lambda e: run("pool", e))
        block.sync(lambda e: run("sp", e))


class Arena:
    def __init__(self, big, size):
        self.big = big
        self.size = size
        self.top = 0

    def alloc(self, free_shape, dtype):
        esz = {F32: 4, BF16: 2, I32: 4, U32: 4, U8: 1}[dtype]
        n = int(np.prod(free_shape))
        nbytes = (n * esz + 63) // 64 * 64
        off = self.top
        self.top += nbytes
        assert self.top <= self.size, "SBUF arena overflow %d > %d" % (self.top, self.size)
        v = self.big[:, off:off + n * esz]
        if dtype != U8:
            v = v.bitcast(dtype)
        if len(free_shape) == 2:
            v = v.rearrange("p (a b) -> p a b", b=free_shape[1])
        elif len(free_shape) == 3:
            v = v.rearrange("p (a b c) -> p a b c", b=free_shape[1], c=free_shape[2])
        return v

    def mark(self):
        return self.top

    def release(self, m):
        self.top = m


def build_nc(stage=99, dbg=False):
    sub = int(os.environ.get('KSUB', '99'))
    nc = bass.Bass("TRN2", target_bir_lowering=False)
    es = ExitStack()

    def din(name, shape, dt=F32):
        return nc.dram_tensor(name, list(shape), dt, kind="ExternalInput").ap()

    def dscr(name, shape, dt):
        kind = "ExternalOutput" if dbg else "Internal"
        return nc.dram_tensor(name, list(shape), dt, kind=kind).ap()

    x_d = din("x", [S, D])
    cfm_d = din("cfm", [128, 8])
    pos_d = din("pos", [1, S], I32)
    wada_d = din("wada", [128, 8 * 6144])
    badar_d = din("bada_row", [1, 6144])
    gmixr_d = din("gmix_row", [1, D])
    gffnr_d = din("gffn_row", [1, D])
    gfin_d = din("gfinal_row", [1, D])
    win_d = din("win", [128, 8 * WC])
    gq_d = din("gq_fm", [128, 2])
    gkv_d = din("gkv_fm", [128, 1])
    wuq_d = din("wuq", [128, 2 * 768])
    wuqs_d = din("wuq_sw", [128, 2 * 768])
    wuk_d = din("wuk", [128, 512])
    wuv_d = din("wuv", [128, 512])
    rconst_d = din("rconst", [128, 2])
    ident_d = din("ident", [128, 128])
    sel_d = din("sel", [128, 96])
    biasT_d = din("biasT", [128, 8 * 640])
    woa_d = din("woa", [128, 4 * D])
    wob_d = din("wob", [128, 4 * D])
    wout_d = din("wout", [128, 8 * D])
    wr_d = din("wr", [128, 8 * 36])
    rb_d = din("rb", [1, 36])
    iota_d = din("iota_e", [128, 32])
    lst_d = din("lst", [128, 128])
    jv_d = din("jv", [128, NTILE])
    pidx_d = din("pidx", [128, 1])
    wg_d = din("wgl", [32 * 128, 2048])
    wu_d = din("wul", [32 * 128, 2048])
    wdn_d = din("wdl", [32 * 128, 2048])
    out_d = nc.dram_tensor("out", [S, D], F32, kind="ExternalOutput").ap()

    tabc_d = dscr("tabc", [128, 512], F32)
    tabsp_d = dscr("tabsp", [128, 512], F32)
    tabsn_d = dscr("tabsn", [128, 512], F32)
    qT_d = dscr("qT", [96, 8 * S], BF16)
    kT_d = dscr("kT", [96, 8 * S], BF16)
    va_d = dscr("va", [S, 520], BF16)
    qbT_d = dscr("qbT", [128, 4 * S], BF16)
    kbT_d = dscr("kbT", [128, 4 * S], BF16)
    vb_d = dscr("vb", [S, 520], BF16)
    ga_d = dscr("gaT", [128, 8 * S], F32)
    gb_d = dscr("gbT", [128, 8 * S], F32)
    moddbg_d = dscr("moddbg", [128, 48], F32) if dbg else None
    x1_d = dscr("x1s", [S, D], F32)
    h2_d = dscr("h2s", [S, D], BF16)
    xs_d = dscr("xs", [NSLOT, D], BF16)
    ys_d = dscr("ys", [NSLOT, D], F32)
    wx_d = nc.dram_tensor("wx", [32 * 128, 6144], BF16, kind="Internal").ap()

    SB_BYTES = 212480
    big = nc.alloc_sbuf_tensor("big", [128, SB_BYTES], U8)
    A = Arena(big, SB_BYTES)
    banks = [nc.alloc_psum_tensor("psb%d" % i, [128, 512], F32).ap() for i in range(8)]
    sc = Sched(nc, es)

    psn = [0]

    def ps_next():
        i = psn[0] % 8
        psn[0] += 1
        return banks[i], ("ps", i)

    def dma(q, out, in_, reads, writes, **kw):
        sc.add(q, lambda e: e.dma_start(out=out, in_=in_, **kw), reads, writes, dma=True)

    def mm(out, lhsT, rhs, start, stop, reads, writes):
        sc.add("pe", lambda e: e.matmul(out, lhsT, rhs, start=start, stop=stop), reads, writes)

    def tr(out, in_, ident, reads, writes):
        sc.add("pe", lambda e: e.transpose(out, in_, ident), reads, writes)

    def act(out, in_, func, reads, writes, **kw):
        sc.add("act", lambda e: e.activation(out=out, in_=in_, func=func, **kw), reads, writes)

    def tcopy(eng, out, in_, reads, writes):
        if eng == "act":
            sc.add(eng, lambda e: e.activation(out=out, in_=in_, func=AF.Copy), reads, writes)
        else:
            sc.add(eng, lambda e: e.tensor_copy(out=out, in_=in_), reads, writes)

    def tt(eng, out, in0, in1, op, reads, writes):
        sc.add(eng, lambda e: e.tensor_tensor(out=out, in0=in0, in1=in1, op=op), reads, writes)

    def ts(eng, out, in0, s1, s2, op0, op1, reads, writes):
        if s2 is None:
            sc.add(eng, lambda e: e.tensor_scalar(out=out, in0=in0, scalar1=s1, scalar2=None, op0=op0),
                   reads, writes)
        else:
            sc.add(eng, lambda e: e.tensor_scalar(out=out, in0=in0, scalar1=s1, scalar2=s2, op0=op0, op1=op1),
                   reads, writes)

    def stt(out, in0, scalar, in1, op0, op1, reads, writes):
        sc.add("dve", lambda e: e.scalar_tensor_tensor(out=out, in0=in0, scalar=scalar, in1=in1, op0=op0, op1=op1),
               reads, writes)

    def memset(eng, ap, val, writes):
        sc.add(eng, lambda e: e.memset(ap, val), (), writes)

    def recip(out, in_, reads, writes):
        sc.add("dve", lambda e: e.reciprocal(out=out, in_=in_), reads, writes)

    ident_f = A.alloc([128], F32)
    ident_b = A.alloc([128], BF16)
    ones_f = A.alloc([128], F32)
    ones_b = A.alloc([128], BF16)
    eps_c = A.alloc([1], F32)
    modfm = A.alloc([48], F32)
    s1_fm = A.alloc([8], F32)
    s2_fm = A.alloc([8], F32)
    gt1_bc = A.alloc([D], F32)
    gt2_bc = A.alloc([D], F32)
    s2_bc = A.alloc([D], F32)
    b2_bc = A.alloc([D], F32)

    stg = A.alloc([2048], BF16)
    pre_steps = []
    for mi, wsrc in enumerate((wg_d, wu_d, wdn_d)):
        for e_ in range(32):
            pre_steps.append(("ld", mi, wsrc, e_))
            pre_steps.append(("st", mi, wsrc, e_))
    pre_pos = [0]

    def precast_step():
        if pre_pos[0] >= len(pre_steps):
            return
        kind, mi, wsrc, e_ = pre_steps[pre_pos[0]]
        pre_pos[0] += 1
        if kind == "ld":
            dma("pool", stg, wsrc[e_ * 128:(e_ + 1) * 128, :], (), ["stg"])
        else:
            dma("sp", wx_d[e_ * 128:(e_ + 1) * 128, mi * 2048:(mi + 1) * 2048], stg, ["stg"], [("wx", mi, e_)])

    dma("sp", ident_f, ident_d, (), ["ident_f"])
    tcopy("dve", ident_b, ident_f, ["ident_f"], ["ident_b"])
    memset("dve", ones_f, 1.0, ["ones_f"])
    memset("dve", ones_b, 1.0, ["ones_b"])
    memset("dve", eps_c, EPS, ["eps_c"])

    pP = A.mark()
    wuq_b = A.alloc([2, 768], BF16)
    wuqs_b = A.alloc([2, 768], BF16)
    wukp_b = A.alloc([8, 96], BF16)
    wuv_b = A.alloc([512], BF16)
    sel_b = A.alloc([96], BF16)
    gq = A.alloc([2], F32)
    gkv = A.alloc([1], F32)
    win_b = A.alloc([8, WC], BF16)
    winv = win_d.rearrange("p (k n) -> p k n", k=8)
    for kc in range(8):
        for c0 in range(0, WC, 1048):
            dma("pool", win_b[:, kc, c0:c0 + 1048], winv[:, kc, c0:c0 + 1048], (), [("win", kc, c0)])
    p0 = A.mark()
    cfm = A.alloc([8], F32)
    cact = A.alloc([8], F32)
    c_rep = A.alloc([8, 128], F32)
    bada_bc = A.alloc([6144], F32)
    gmix_bc = A.alloc([D], F32)
    gffn_bc = A.alloc([D], F32)
    sh1_r = A.alloc([D], F32)
    sc1_r = A.alloc([D], F32)
    sc2_r = A.alloc([D], F32)
    dtmp = A.alloc([128], F32)
    wbuf = [A.alloc([3072], F32) for _ in range(2)]
    dma("sp", cfm, cfm_d, (), ["cfm"])
    dma("sp", bada_bc, badar_d[0:1, :].to_broadcast([128, 6144]), (), ["bada_bc"])
    dma("sp", gmix_bc, gmixr_d[0:1, :].to_broadcast([128, D]), (), ["gmix_bc"])
    dma("sp", gffn_bc, gffnr_d[0:1, :].to_broadcast([128, D]), (), ["gffn_bc"])
    act(cact, cfm, AF.Silu, ["cfm"], ["cact"])
    tcopy("dve", c_rep, cact[:, :, None].broadcast_to([128, 8, 128]), ["cact"], ["c_rep"])
    wv = wada_d.rearrange("p (k n) -> p k n", k=8)
    dests = [sh1_r, sc1_r, gt1_bc, b2_bc, sc2_r, gt2_bc]
    dkeys = ["sh1_r", "sc1_r", "gt1_bc", "b2_bc", "sc2_r", "gt2_bc"]
    wcn = 0
    for hf_ in range(2):
        for kc in range(8):
            wb = wbuf[wcn % 2]
            wk = ("wbuf", wcn % 2)
            wcn += 1
            dma("sp", wb, wv[:, kc, hf_ * 3072:(hf_ + 1) * 3072], (), [wk])
            for nt in range(6):
                mm(banks[nt], c_rep[:, kc, :], wb[:, nt * 512:(nt + 1) * 512], kc == 0, kc == 7,
                   [wk, "c_rep"], [("ps", nt)])
        for nt in range(6):
            n0 = hf_ * 3072 + nt * 512
            di = n0 // 1024
            tt("dve", dests[di][:, n0 % 1024:n0 % 1024 + 512], banks[nt], bada_bc[:, n0:n0 + 512], ALU.add,
               [("ps", nt), "bada_bc"], [(dkeys[di], (n0 % 1024) // 512)])
    K2 = lambda nm: [(nm, 0), (nm, 1)]
    stt(sc1_r, sc1_r, 1.0, gmix_bc, ALU.add, ALU.mult, K2("sc1_r") + ["gmix_bc"], ["s1_r"])
    stt(s2_bc, sc2_r, 1.0, gffn_bc, ALU.add, ALU.mult, K2("sc2_r") + ["gffn_bc"], ["s2_bc"])
    for (row, rkeys, dst_fm, dk) in ((sc1_r, ["s1_r"], s1_fm, "s1"), (sh1_r, K2("sh1_r"), modfm, "modfm")):
        for kc in range(8):
            tt("dve", dtmp, row[:, kc * 128:(kc + 1) * 128], ident_f, ALU.mult, rkeys + ["ident_f"], ["dtmp"])
            sc.add("dve", lambda e, dst_fm=dst_fm, kc=kc: e.tensor_reduce(out=dst_fm[:, kc:kc + 1], in_=dtmp, axis=AX.X,
                                                                         op=ALU.add), ["dtmp"], [dk])

    rconst = A.alloc([2], F32)
    dma("sp", rconst, rconst_d, (), ["rconst"])
    HALF = 512
    posi = A.alloc([HALF], I32)
    ang = A.alloc([HALF], F32)
    halfpi = A.alloc([1], F32)
    memset("dve", halfpi, PI / 2, ["halfpi"])
    tmpS = [A.alloc([HALF], F32) for _ in range(4)]
    kiS = A.alloc([HALF], I32)
    for cb in range(8):
        dma("sp", posi[cb * 16:(cb + 1) * 16, :], pos_d[0:1, cb * 512:(cb + 1) * 512].to_broadcast([16, 512]), (),
            [("posi", cb)])
    POSI = [("posi", cb) for cb in range(8)]
    tcopy("dve", ang, posi, POSI, ["ang"])
    ts("dve", ang, ang, rconst[:, 0:1], None, ALU.mult, None, ["ang", "rconst"], ["ang"])
    t1, r0, mk, t2 = tmpS
    ts("dve", t1, ang, 1.0 / (2 * PI), None, ALU.mult, None, ["ang"], ["s_t1"])
    tcopy("dve", kiS, t1, ["s_t1"], ["s_ki"])
    stt(r0, kiS, -2 * PI, ang, ALU.mult, ALU.add, ["s_ki", "ang"], ["s_r0"])
    ts("dve", mk, r0, PI, -2 * PI, ALU.is_gt, ALU.mult, ["s_r0"], ["s_mk"])
    tt("dve", r0, r0, mk, ALU.add, ["s_r0", "s_mk"], ["s_r0"])
    ts("dve", mk, r0, -PI, 2 * PI, ALU.is_lt, ALU.mult, ["s_r0"], ["s_mk"])
    tt("dve", r0, r0, mk, ALU.add, ["s_r0", "s_mk"], ["s_r0"])
    ts("dve", r0, r0, PI, -PI, ALU.min, ALU.max, ["s_r0"], ["s_r0"])
    act(t1, r0, AF.Sin, ["s_r0"], ["s_t1"])
    dma("sp", tabsp_d, t1, ["s_t1"], ["tabsp"])
    act(t2, r0, AF.Sin, ["s_r0"], ["s_t2"], scale=-1.0)
    dma("sp", tabsn_d, t2, ["s_t2"], ["tabsn"])
    ts("dve", t1, ang, PI / 2, 1.0 / (2 * PI), ALU.add, ALU.mult, ["ang", "s_t1"], ["s_t1"])
    tcopy("dve", kiS, t1, ["s_t1"], ["s_ki"])
    stt(r0, kiS, -2 * PI, ang, ALU.mult, ALU.add, ["s_ki", "ang", "s_r0"], ["s_r0"])
    ts("dve", mk, r0, PI / 2, -2 * PI, ALU.is_gt, ALU.mult, ["s_r0"], ["s_mk"])
    tt("dve", r0, r0, mk, ALU.add, ["s_r0", "s_mk"], ["s_r0"])
    ts("dve", mk, r0, -1.5 * PI, 2 * PI, ALU.is_lt, ALU.mult, ["s_r0"], ["s_mk"])
    tt("dve", r0, r0, mk, ALU.add, ["s_r0", "s_mk"], ["s_r0"])
    ts("dve", r0, r0, PI / 2, -1.5 * PI, ALU.min, ALU.max, ["s_r0"], ["s_r0"])
    act(t1, r0, AF.Sin, ["s_r0", "halfpi"], ["s_t1"], bias=halfpi[:, 0:1])
    dma("sp", tabc_d, t1, ["s_t1"], ["tabc"])

    wtmp = A.alloc([2, 768], F32)
    wtmp2 = A.alloc([2, 768], F32)
    wtmp3 = A.alloc([512], F32)
    wtmp4 = A.alloc([512], F32)
    seltmp = A.alloc([96], F32)
    dma("sp", gq, gq_d, (), ["gq"])
    dma("sp", gkv, gkv_d, (), ["gkv"])
    dma("sp", wtmp, wuq_d.rearrange("p (k n) -> p k n", k=2), (), ["wtmp"])
    dma("sp", wtmp2, wuqs_d.rearrange("p (k n) -> p k n", k=2), (), ["wtmp2"])
    dma("sp", wtmp3, wuk_d, (), ["wtmp3"])
    dma("sp", wtmp4, wuv_d, (), ["wtmp4"])
    dma("sp", seltmp, sel_d, (), ["seltmp"])
    for kc in range(2):
        ts("dve", wuq_b[:, kc, :], wtmp[:, kc, :], gq[:, kc:kc + 1], None, ALU.mult, None, ["wtmp", "gq"], ["wuq_b"])
        ts("dve", wuqs_b[:, kc, :], wtmp2[:, kc, :], gq[:, kc:kc + 1], None, ALU.mult, None, ["wtmp2", "gq"], ["wuqs_b"])
    memset("dve", wukp_b, 0.0, ["wukp_b"])
    ts("dve", wukp_b[:, :, 0:64], wtmp3.rearrange("p (h d) -> p h d", d=64), gkv[:, 0:1], None, ALU.mult, None,
       ["wtmp3", "gkv", "wukp_b"], ["wukp_b"])
    ts("dve", wuv_b, wtmp4, gkv[:, 0:1], None, ALU.mult, None, ["wtmp4", "gkv"], ["wuv_b"])
    tcopy("dve", sel_b[0:96], seltmp[0:96], ["seltmp"], ["sel_b"])

    sc.barrier()
    A.release(p0)
    if stage <= 0:
        return finish(nc, es, sc, None)

    WIN = []

    xb = [A.alloc([D], F32) for _ in range(2)]
    junk = A.alloc([D], BF16)
    ssq = A.alloc([4], F32)
    rstd = A.alloc([4], F32)
    xn = [A.alloc([D], F32) for _ in range(2)]
    hT = [A.alloc([8, TT], BF16) for _ in range(2)]
    ctab = A.alloc([TT], F32)
    stab = A.alloc([TT], F32)
    qlat = A.alloc([2, TT], F32)
    qsq = A.alloc([2, TT], F32)
    kvlat = A.alloc([TT], F32)
    kvsq = A.alloc([TT], F32)
    rbc = [A.alloc([TT], F32) for _ in range(2)]
    qln = A.alloc([2, TT], BF16)
    kvn = A.alloc([TT], BF16)
    krr = A.alloc([TT], BF16)
    rt1 = [A.alloc([TT], F32) for _ in range(2)]
    rt2 = [A.alloc([TT], F32) for _ in range(2)]
    qT_s = A.alloc([8, TT], BF16)
    kT_s = A.alloc([8, TT], BF16)
    qbT_s = A.alloc([4, TT], BF16)
    kbT_s = A.alloc([4, TT], BF16)
    va_s = A.alloc([4, 520], BF16)
    vb_s = A.alloc([4, 520], BF16)
    gst = [A.alloc([TT], F32) for _ in range(4)]
    memset("dve", ctab[0:64], 1.0, ["ctab0"])
    memset("dve", stab[0:64], 0.0, ["stab0"])
    memset("dve", va_s, 1.0, ["va_s"])
    memset("dve", vb_s, 1.0, ["vb_s"])

    qT_v = qT_d.rearrange("p (h t) -> p h t", h=8)
    kT_v = kT_d.rearrange("p (h t) -> p h t", h=8)
    qbT_v = qbT_d.rearrange("p (h t) -> p h t", h=4)
    kbT_v = kbT_d.rearrange("p (h t) -> p h t", h=4)
    ga_v = ga_d.rearrange("p (c t) -> p c t", c=8)
    gb_v = gb_d.rearrange("p (c t) -> p c t", c=8)
    gcnt = [0]
    ecnt = [0]

    def evac_copy(out, in_, reads, writes):
        eng = "act" if ecnt[0] % 2 == 0 else "dve"
        ecnt[0] += 1
        tcopy(eng, out, in_, reads, writes)

    NT1 = NT if stage > 1 else 1

    def prep_stats(ti, bi):
        t0 = ti * TT
        g = ti * 4 + bi
        xt = xb[g % 2]
        xk = ("xb", g % 2)
        dma("sp", xt, x_d[t0 + bi * 128:t0 + (bi + 1) * 128, :], (), [xk])
        act(junk, xt, AF.Square, [xk], [("ssq", bi)], accum_out=ssq[:, bi:bi + 1])
        act(rstd[:, bi:bi + 1], ssq[:, bi:bi + 1], AF.Sqrt, [("ssq", bi), "eps_c"], [("rstd", bi)],
            scale=1.0 / D, bias=eps_c[:, 0:1])
        recip(rstd[:, bi:bi + 1], rstd[:, bi:bi + 1], [("rstd", bi)], [("rstd", bi)])
        ts("dve", xn[g % 2], xt, rstd[:, bi:bi + 1], None, ALU.mult, None, [xk, ("rstd", bi)], [("xn", g % 2)])

    def prep_tr(ti, bi):
        g = ti * 4 + bi
        xnt = xn[g % 2]
        nk = ("xn", g % 2)
        h_t = hT[ti % 2]
        for half in range(2):
            pb, pk = ps_next()
            for q in range(4):
                kc = half * 4 + q
                tr(pb[:, q * 128:(q + 1) * 128], xnt[:, kc * 128:(kc + 1) * 128], ident_f, [nk, "ident_f"], [pk])
            for q in range(4):
                kc = half * 4 + q
                dst = h_t[:, kc, bi * 128:(bi + 1) * 128]
                hk = ("hT", ti % 2, bi, q % 2)
                act(dst, pb[:, q * 128:(q + 1) * 128], AF.Identity, [pk, "s1", "modfm"], [hk],
                    scale=s1_fm[:, kc:kc + 1], bias=modfm[:, kc:kc + 1])

    def prep_all(ti):
        prep_stats(ti, 0)
        prep_stats(ti, 1)
        prep_tr(ti, 0)
        prep_stats(ti, 2)
        prep_tr(ti, 1)
        prep_stats(ti, 3)
        prep_tr(ti, 2)
        prep_tr(ti, 3)

    prep_all(0)
    for ti in range(NT1):
        t0 = ti * TT
        h_t = hT[ti % 2]
        HK = [("hT", ti % 2, bi_, q_) for bi_ in range(4) for q_ in range(2)]
        nxt = ti + 1 if ti + 1 < NT1 else None
        dma("sp", ctab[64:80], tabc_d[ti * 16:(ti + 1) * 16, :], (), ["ctab"])
        dma("sp", ctab[80:96], tabc_d[ti * 16:(ti + 1) * 16, :], (), ["ctab2"])
        dma("sp", stab[64:80], tabsn_d[ti * 16:(ti + 1) * 16, :], (), ["stab"])
        dma("sp", stab[80:96], tabsp_d[ti * 16:(ti + 1) * 16, :], (), ["stab2"])

        def proj(c0, m):
            pb, pk = ps_next()
            for kc in range(8):
                mm(pb[0:m, :], win_b[:, kc, c0:c0 + m], h_t[:, kc, :], kc == 0, kc == 7, HK + WIN, [pk])
            return pb, pk

        for c in range(2):
            pb, pk = proj(c * 128, 128)
            act(qlat[:, c, :], pb, AF.Copy, [pk], [("qlat", c)])
            act(qsq[:, c, :], pb, AF.Square, [pk], [("qsq", c)])
        pb, pk = proj(256, 128)
        act(kvlat, pb, AF.Copy, [pk], ["kvlat"])
        act(kvsq, pb, AF.Square, [pk], ["kvsq"])
        pa, pka = proj(C_KRP, 96)
        pbb, pkb = proj(C_KRS, 96)
        tt("dve", rt1[0][0:96], pa[0:96, :], ctab[0:96], ALU.mult, [pka, "ctab", "ctab2", "ctab0"], [("rt1", 0)])
        tt("dve", rt2[0][0:96], pbb[0:96, :], stab[0:96], ALU.mult, [pkb, "stab", "stab2", "stab0"], [("rt2", 0)])
        tt("pool", krr[0:96], rt1[0][0:96], rt2[0][0:96], ALU.add, [("rt1", 0), ("rt2", 0)], ["krr"])
        pq, pkq = ps_next()
        for c in range(2):
            mm(pq, ones_f, qsq[:, c, :], c == 0, c == 1, ["ones_f", ("qsq", c)], [pkq])
        act(rbc[0], pq, AF.Sqrt, [pkq, "eps_c"], [("rbc", 0)], scale=1.0 / 256, bias=eps_c[:, 0:1])
        recip(rbc[0], rbc[0], [("rbc", 0)], [("rbc", 0)])
        for c in range(2):
            tt("dve", qln[:, c, :], qlat[:, c, :], rbc[0], ALU.mult, [("qlat", c), ("rbc", 0)], [("qln", c)])
        pkv, pkkv = ps_next()
        mm(pkv, ones_f, kvsq, True, True, ["ones_f", "kvsq"], [pkkv])
        act(rbc[1], pkv, AF.Sqrt, [pkkv, "eps_c"], [("rbc", 1)], scale=1.0 / 128, bias=eps_c[:, 0:1])
        recip(rbc[1], rbc[1], [("rbc", 1)], [("rbc", 1)])
        tt("dve", kvn, kvlat, rbc[1], ALU.mult, ["kvlat", ("rbc", 1)], ["kvn"])
        if nxt is not None:
            prep_stats(nxt, 0)
            prep_stats(nxt, 1)
        for i in range(4):
            pb, pk = proj(C_QB + i * 128, 128)
            evac_copy(qbT_s[:, i, :], pb, [pk], ["qbT_s"])
        dma("sp", qbT_v[:, :, t0:t0 + TT], qbT_s, ["qbT_s"], [("qbT_d", ti)])
        for h in range(8):
            pa, pka = ps_next()
            for c in range(2):
                mm(pa[0:96, :], wuq_b[:, c, h * 96:(h + 1) * 96], qln[:, c, :], c == 0, c == 1,
                   ["wuq_b", ("qln", c)], [pka])
            pbb, pkb = ps_next()
            for c in range(2):
                mm(pbb[0:96, :], wuqs_b[:, c, h * 96:(h + 1) * 96], qln[:, c, :], c == 0, c == 1,
                   ["wuqs_b", ("qln", c)], [pkb])
            i2 = h % 2
            tt("dve", rt1[i2][0:96], pa[0:96, :], ctab[0:96], ALU.mult, [pka, "ctab", "ctab2", "ctab0"], [("rt1", i2)])
            tt("dve", rt2[i2][0:96], pbb[0:96, :], stab[0:96], ALU.mult, [pkb, "stab", "stab2", "stab0"], [("rt2", i2)])
            tt("pool", qT_s[0:96, h, :], rt1[i2][0:96], rt2[i2][0:96], ALU.add, [("rt1", i2), ("rt2", i2)], ["qT_s"])
            pk_, pkk = ps_next()
            mm(pk_[0:96, :], wukp_b[:, h, :], kvn, True, False, ["wukp_b", "kvn"], [pkk])
            mm(pk_[0:96, :], sel_b[0:96, :], krr[0:96, :], False, True, ["sel_b", "krr"], [pkk])
            evac_copy(kT_s[0:96, h, :], pk_[0:96, :], [pkk], ["kT_s"])
        for bi in range(4):
            pv, pkv_ = ps_next()
            mm(pv, kvn[:, bi * 128:(bi + 1) * 128], wuv_b, True, True, ["kvn", "wuv_b"], [pkv_])
            evac_copy(va_s[:, bi, :].rearrange("p (h d) -> p h d", d=65)[:, :, 0:64],
                      pv.rearrange("p (h d) -> p h d", d=64), [pkv_], ["va_s"])
        dma("sp", qT_v[:, :, t0:t0 + TT], qT_s[0:96], ["qT_s"], [("qT_d", ti)])
        dma("sp", kT_v[:, :, t0:t0 + TT], kT_s[0:96], ["kT_s"], [("kT_d", ti)])
        dma("sp", va_d[t0:t0 + TT, :].rearrange("(b p) f -> p b f", p=128), va_s, ["va_s"], [("va_d", ti)])
        if nxt is not None:
            prep_tr(nxt, 0)
            prep_stats(nxt, 2)
        for i in range(4):
            pb, pk = proj(C_KB + i * 128, 128)
            evac_copy(kbT_s[:, i, :], pb, [pk], ["kbT_s"])
        dma("sp", kbT_v[:, :, t0:t0 + TT], kbT_s, ["kbT_s"], [("kbT_d", ti)])
        if nxt is not None:
            prep_tr(nxt, 1)
            prep_stats(nxt, 3)
        for bi in range(4):
            pv, pkv_ = ps_next()
            for kc in range(8):
                mm(pv, h_t[:, kc, bi * 128:(bi + 1) * 128], win_b[:, kc, C_VB:C_VB + 512], kc == 0, kc == 7,
                   HK + WIN, [pkv_])
            evac_copy(vb_s[:, bi, :].rearrange("p (h d) -> p h d", d=65)[:, :, 0:64],
                      pv.rearrange("p (h d) -> p h d", d=64), [pkv_], ["vb_s"])
        dma("sp", vb_d[t0:t0 + TT, :].rearrange("(b p) f -> p b f", p=128), vb_s, ["vb_s"], [("vb_d", ti)])
        if nxt is not None:
            prep_tr(nxt, 2)
        for gix, (cbase, gv, nm) in enumerate(((C_GA, ga_v, "ga"), (C_GB, gb_v, "gb"))):
            for c in range(8):
                pb, pk = proj(cbase + c * 128, 128)
                gi = gcnt[0] % 4
                gcnt[0] += 1
                act(gst[gi], pb, AF.Sigmoid, [pk], [("gst", gi)])
                dma("sp", gv[:, c, t0:t0 + TT], gst[gi], [("gst", gi)], [(nm, ti, c)])
            if gix == 0 and nxt is not None:
                prep_tr(nxt, 3)

    sc.barrier()
    if stage <= 2:
        return finish(nc, es, sc, None)

    A.release(pP)
    o_a = A.alloc([NB, 512], BF16)
    o_b = A.alloc([NB, 512], BF16)
    p2 = A.mark()
    kT_r = A.alloc([8, S], BF16)
    va_r = A.alloc([NB, 520], BF16)
    qT_t = [A.alloc([8, TT], BF16) for _ in range(2)]
    E_t = [A.alloc([TT], BF16) for _ in range(6)]
    rden = [A.alloc([4], F32) for _ in range(2)]
    for h in range(8):
        dma("sp", kT_r[0:96, h, :], kT_v[:, h, :], (), [("kT_r", h)])
    va_v = va_d.rearrange("(b p) f -> p b f", p=128)
    for q4 in range(4):
        dma("sp", va_r[:, q4 * 8:(q4 + 1) * 8, :], va_v[:, q4 * 8:(q4 + 1) * 8, :], (), [("va_r", q4)])
    SCALE_A = 96 ** -0.5
    sbank = [0]
    ecnt2 = [0]
    accn = [0]
    NQT = NT if stage > 3 else 2
    LOOK = 3
    stageA, stageB = [], []
    for qt in range(NQT):
        for h in range(8):
            nkt = 4 * qt + 4
            for kt in range(nkt):
                stageA.append((qt, h, kt))
    grp = {}

    def emitA(rec):
        qt, h, kt = rec
        qtt = qT_t[qt % 2]
        qk = ("qT_t", qt % 2)
        if h == 0 and kt == 0:
            dma("sp", qtt[0:96], qT_v[:, :, qt * TT:(qt + 1) * TT], (), [qk])
        r = kt - 4 * qt
        c0 = 128 * r if r > 0 else 0
        sb = sbank[0] % 5
        sbank[0] += 1
        ps_, psk = banks[sb], ("ps", sb)
        mm(ps_[:, c0:TT], kT_r[0:96, h, kt * 128:(kt + 1) * 128], qtt[0:96, h, c0:TT], True, True,
           [("kT_r", h), qk], [psk])
        ei = ecnt2[0] % len(E_t)
        ecnt2[0] += 1
        Et, Ek = E_t[ei], ("E", ei)
        act(Et[:, c0:TT], ps_[:, c0:TT], AF.Exp, [psk], [Ek], scale=SCALE_A)
        if r >= 0:
            memset("dve", Et[64:128, c0:c0 + 64], 0.0, [Ek])
        grp[rec] = (Et, Ek)

    def emitB(rec):
        qt, h, kt = rec
        nkt = 4 * qt + 4
        r = kt - 4 * qt
        if kt == 0:
            ab = 5 + accn[0] % 2
            accn[0] += 1
            grp["acc"] = (banks[ab], ("ps", ab))
        acc, acck = grp["acc"]
        Et, Ek = grp.pop(rec)
        for qb in range(max(r, 0), 4):
            first = (kt == 0) and (qb == 0)
            last = (kt == nkt - 1) and (qb == 3)
            mm(acc[:, qb * 65:(qb + 1) * 65], Et[:, qb * 128:(qb + 1) * 128],
               va_r[:, kt, h * 65:(h + 1) * 65], first, last, [Ek, ("va_r", kt // 8)], [acck])
        if kt == nkt - 1:
            rd = rden[h % 2]
            rk = ("rden", h % 2)
            accv = acc[:, 0:260].rearrange("p (b d) -> p b d", d=65)
            recip(rd, accv[:, :, 64], [acck], [rk])
            tt("dve", o_a[:, qt * 4:(qt + 1) * 4, h * 64:(h + 1) * 64], accv[:, :, 0:64],
               rd[:, :, None].broadcast_to([128, 4, 64]), ALU.mult, [acck, rk], [("o_a", qt)])

    for i in range(len(stageA) + LOOK):
        if i % 8 == 3:
            precast_step()
        if i < len(stageA):
            emitA(stageA[i])
        if i >= LOOK:
            emitB(stageA[i - LOOK])
    if dbg:
        oa_dbg = dscr("oa_dbg", [S, 512], BF16)
        dma("sp", oa_dbg.rearrange("(b p) f -> p b f", p=128), o_a, [("o_a", q) for q in range(NQT)], ["oa_dbg"])
    sc.barrier()
    if stage <= 4:
        return finish(nc, es, sc, None)

    A.release(p2)
    qb_p = [A.alloc([S], BF16) for _ in range(2)]
    kb_p = [A.alloc([S], BF16) for _ in range(2)]
    vb_r = A.alloc([NB, 520], BF16)
    bias_r = A.alloc([8, 640], F32)
    Eb = [A.alloc([640], BF16) for _ in range(10)]
    stmp = [A.alloc([640], F32) for _ in range(4)]
    rden3 = [A.alloc([4], F32) for _ in range(2)]
    vb_v = vb_d.rearrange("(b p) f -> p b f", p=128)
    for q4 in range(4):
        dma("sp", vb_r[:, q4 * 8:(q4 + 1) * 8, :], vb_v[:, q4 * 8:(q4 + 1) * 8, :], (), [("vb_r", q4)])
    dma("sp", bias_r, biasT_d.rearrange("p (h q) -> p h q", h=8), (), ["bias_r"])
    for h_ in range(8):
        act(bias_r[:, h_, :], bias_r[:, h_, :], AF.Exp, ["bias_r"], ["bias_r"])
    sbank[0] = 0
    ecnt3 = 0
    stc = 0
    NJ = NB if stage > 5 else 8
    recs3 = [(h, j) for h in range(8) for j in range(NJ)]
    ering = {}
    st3 = dict(ecnt=0, stc=0, bs=0)

    def emitA3(rec):
        h, j = rec
        pr, po = h // 2, (h % 2) * 64
        qb_r, kb_r = qb_p[pr % 2], kb_p[pr % 2]
        if h % 2 == 0 and j == 0:
            dma("sp", qb_r, qbT_v[:, pr, :], (), [("qb_r", pr % 2)])
            dma("sp", kb_r, kbT_v[:, pr, :], (), [("kb_r", pr % 2)])
        nq = min(640, S - 128 * j)
        n1 = min(nq, 512)
        sb = sbank[0] % 4
        sbank[0] += 1
        psA, pkA = banks[sb], ("ps", sb)
        st_ = stmp[st3["stc"] % 4]
        stk = ("stmp", st3["stc"] % 4)
        st3["stc"] += 1
        mm(psA[:, 0:n1], kb_r[po:po + 64, 128 * j:128 * j + 128], qb_r[po:po + 64, 128 * j:128 * j + n1],
           True, True, [("kb_r", pr % 2), ("qb_r", pr % 2)], [pkA])
        act(st_[:, 0:n1], psA[:, 0:n1], AF.Exp, [pkA], [(stk, 0)], scale=0.125)
        if nq > 512:
            bslot = (4, 7)[st3["bs"] % 2]
            st3["bs"] += 1
            psB, pkB = banks[bslot], ("ps", bslot)
            mm(psB[:, 0:nq - 512], kb_r[po:po + 64, 128 * j:128 * j + 128],
               qb_r[po:po + 64, 128 * j + 512:128 * j + nq], True, True, [("kb_r", pr % 2), ("qb_r", pr % 2)], [pkB])
            act(st_[:, 512:nq], psB[:, 0:nq - 512], AF.Exp, [pkB], [(stk, 1)], scale=0.125)
        ei = st3["ecnt"] % len(Eb)
        st3["ecnt"] += 1
        Et, Ek = Eb[ei], ("Eb", ei)
        tt("dve" if st3["ecnt"] % 2 == 0 else "pool", Et[:, 0:nq], st_[:, 0:nq], bias_r[:, h, 0:nq], ALU.mult,
           [(stk, 0), (stk, 1), "bias_r"], [Ek])
        ering[(h, j)] = (Et, Ek)

    def emitB3(rec):
        h, j = rec
        if j % 4 == 0:
            ab = 5 + accn[0] % 2
            accn[0] += 1
            ering["acc"] = (banks[ab], ("ps", ab))
        acc, acck = ering["acc"]
        jj0 = max(0, j - 4)
        for jj in range(jj0, j + 1):
            Ej, Ejk = ering[(h, jj)]
            off = (j - jj) * 128
            mm(acc[:, (j % 4) * 65:(j % 4 + 1) * 65], Ej[:, off:off + 128], vb_r[:, jj, h * 65:(h + 1) * 65],
               (j % 4 == 0) and (jj == jj0), (j % 4 == 3) and (jj == j), [Ejk, ("vb_r", jj // 8)], [acck])
        if j % 4 == 3:
            rd = rden3[(j // 4) % 2]
            rk = ("rden3", (j // 4) % 2)
            accv = acc[:, 0:260].rearrange("p (b d) -> p b d", d=65)
            recip(rd, accv[:, :, 64], [acck], [rk])
            tt("dve", o_b[:, j - 3:j + 1, h * 64:(h + 1) * 64], accv[:, :, 0:64],
               rd[:, :, None].broadcast_to([128, 4, 64]), ALU.mult, [acck, rk], [("o_b", j // 4)])

    LOOK3 = 3
    for i in range(len(recs3) + LOOK3):
        if i % 5 == 2:
            precast_step()
        if i < len(recs3):
            emitA3(recs3[i])
        if i >= LOOK3:
            emitB3(recs3[i - LOOK3])
    if dbg:
        ob_dbg = dscr("ob_dbg", [S, 512], BF16)
        dma("sp", ob_dbg.rearrange("(b p) f -> p b f", p=128), o_b, [("o_b", q) for q in range(NJ // 4)], ["ob_dbg"])
    sc.barrier()
    if stage <= 6:
        return finish(nc, es, sc, None)

    A.release(p2)
    cw_all = A.alloc([NB, 2], F32)
    e_all = A.alloc([NB, 2], F32)
    r_all = A.alloc([NB, 2], F32)
    A1all = A.alloc([NB, 32], F32)
    A2all = A.alloc([NB, 32], F32)
    carry = A.alloc([32], F32)
    iota_e = A.alloc([32], F32)
    rbias = A.alloc([36], F32)
    Lst = A.alloc([128], BF16)
    p4 = A.mark()
    woa_b = A.alloc([4, D], BF16)
    wob_b = A.alloc([4, D], BF16)
    wout_b = A.alloc([8, D], BF16)
    wr_f = A.alloc([8, 36], F32)
    for c in range(4):
        dma("pool", woa_b[:, c, :], woa_d.rearrange("p (k n) -> p k n", k=4)[:, c, :], (), [("woa", c)])
        dma("pool", wob_b[:, c, :], wob_d.rearrange("p (k n) -> p k n", k=4)[:, c, :], (), [("wob", c)])
    WOA = [("woa", c) for c in range(4)]
    WOB = [("wob", c) for c in range(4)]
    WOUT = [("wout", c) for c in range(8)]
    dma("sp", wr_f, wr_d.rearrange("p (k n) -> p k n", k=8), (), ["wr_f"])
    dma("sp", rbias, rb_d[0:1, :].to_broadcast([128, 36]), (), ["rbias"])
    dma("sp", iota_e, iota_d, (), ["iota_e"])
    ltmp = A.alloc([128], F32)
    dma("sp", ltmp, lst_d, (), ["ltmp"])
    tcopy("dve", Lst, ltmp, ["ltmp"], ["Lst"])
    memset("dve", carry, 0.0, ["carry"])

    oaT2 = [A.alloc([4, TT], BF16) for _ in range(2)]
    obT2 = [A.alloc([4, TT], BF16) for _ in range(2)]
    mT = A.alloc([8, TT], BF16)
    gat = [A.alloc([TT], F32) for _ in range(2)]
    gbt = [A.alloc([TT], F32) for _ in range(2)]
    mt1 = [A.alloc([TT], F32) for _ in range(2)]
    mt2 = [A.alloc([TT], F32) for _ in range(2)]
    R4 = 2
    RX = 3
    xr = [A.alloc([D], F32) for _ in range(RX)]
    uu = [A.alloc([D], F32) for _ in range(R4)]
    h2b = [A.alloc([D], BF16) for _ in range(2)]
    h2T = [A.alloc([8, 128], F32) for _ in range(2)]
    rs_t = A.alloc([2, 4], F32)
    wr_s = A.alloc([8, 36], F32)
    s2_fm8 = A.alloc([8], F32)
    b2_fm8 = A.alloc([8], F32)
    dtmp4 = A.alloc([128], F32)
    sm = A.alloc([16], F32)
    lg = A.alloc([4, 36], F32)
    dg = A.alloc([4, 4], F32)
    ge = A.alloc([4, 4], F32)
    pen = A.alloc([4, 4], F32)
    msk = A.alloc([4, 32], F32)
    top8a = A.alloc([4, 8], F32)
    idx8a = A.alloc([4, 8], U32)
    r4s = A.alloc([8, 4], F32)
    Ab = A.alloc([4, 32], BF16)
    Pt = A.alloc([4, 32], F32)
    ptm = A.alloc([4, 32], F32)
    woutv = wout_d.rearrange("p (k n) -> p k n", k=8)
    for c in range(8):
        stb = xr[c % RX]
        stk_ = [("x1", c % RX, 0), ("x1", c % RX, 1)]
        dma("sp", stb, woutv[:, c, :], (), stk_)
        tt("dve", wout_b[:, c, :], stb, gt1_bc, ALU.mult, stk_, [("wout", c)])
    for (row, rk_, dst_fm, dk) in ((s2_bc, [], s2_fm8, "s2_fm8"), (b2_bc, [], b2_fm8, "b2_fm8")):
        for kc in range(8):
            tt("dve", dtmp4, row[:, kc * 128:(kc + 1) * 128], ident_f, ALU.mult, ["ident_f"], ["dtmp4"])
            sc.add("dve", lambda e, dst_fm=dst_fm, kc=kc: e.tensor_reduce(out=dst_fm[:, kc:kc + 1], in_=dtmp4, axis=AX.X,
                                                                         op=ALU.add), ["dtmp4"], [dk])
    for kc in range(8):
        ts("dve", wr_s[:, kc, :], wr_f[:, kc, :], s2_fm8[:, kc:kc + 1], None, ALU.mult, None, ["wr_f", "s2_fm8"], ["wr_s"])
    b2rep = uu[0].rearrange("p (k m) -> p k m", k=8)
    tcopy("dve", b2rep, b2_fm8[:, :, None].broadcast_to([128, 8, 128]), ["b2_fm8"], [("uu", 0)])
    for kc in range(8):
        mm(banks[7][:, 0:36], b2rep[:, kc, :], wr_f[:, kc, :], kc == 0, kc == 7, [("uu", 0), "wr_f"], [("ps", 7)])
    tt("dve", rbias, banks[7][:, 0:36], rbias, ALU.add, [("ps", 7), "rbias"], ["rbias"])
    RB = 6
    rot = [0]

    def ps_rot():
        i = rot[0] % RB
        rot[0] += 1
        return banks[i], ("ps", i)

    pp, pkp = banks[6], ("ps", 6)
    rl, pkr = banks[7], ("ps", 7)
    NT4 = NT if stage > 7 else 1

    def S1(ti, bi):
        blk = ti * 4 + bi
        t0 = ti * TT
        r3 = blk % R4
        rx = blk % RX
        g2 = blk % 2
        tp = ti % 2
        xk = [("x1", rx, 0), ("x1", rx, 1)]
        rsk = ("rs_t", tp, bi)
        dma("sp", xr[rx], x_d[t0 + bi * 128:t0 + (bi + 1) * 128, :], (), xk)
        for half in range(2):
            pm, pkm = ps_rot()
            for m in range(8):
                mm(pm, mT[:, m, bi * 128:(bi + 1) * 128], wout_b[:, m, half * 512:(half + 1) * 512],
                   m == 0, m == 7, [("mT", m_) for m_ in range(8)] + WOUT, [pkm])
            hs = slice(half * 512, (half + 1) * 512)
            tt("dve", xr[rx][:, hs], pm, xr[rx][:, hs], ALU.add, [pkm, ("x1", rx, half)], [("x1", rx, half)])
        dma("sp", x1_d[t0 + bi * 128:t0 + (bi + 1) * 128, :], xr[rx], xk, [("x1_d", blk)])
        act(h2b[g2], xr[rx], AF.Square, xk, [("ssq4", g2), ("h2b", g2)], accum_out=sm[:, g2:g2 + 1])
        act(sm[:, 2 + g2:3 + g2], sm[:, g2:g2 + 1], AF.Ln, [("ssq4", g2), "eps_c"], [("rs4", g2)],
            scale=1.0 / D, bias=eps_c[:, 0:1])
        act(rs_t[:, tp, bi:bi + 1], sm[:, 2 + g2:3 + g2], AF.Exp, [("rs4", g2)], [rsk], scale=-0.5)
        tt("pool", uu[r3], xr[rx], s2_bc, ALU.mult, xk, [("uu", r3)])
        stt(uu[r3], uu[r3], rs_t[:, tp, bi:bi + 1], b2_bc, ALU.mult, ALU.add, [("uu", r3), rsk], [("uu", r3)])
        tcopy("act", h2b[g2], uu[r3], [("uu", r3), ("ssq4", g2)], [("h2b", g2)])
        dma("sp", h2_d[t0 + bi * 128:t0 + (bi + 1) * 128, :], h2b[g2], [("h2b", g2)], [("h2_d", blk)])

    def S2a(ti, bi):
        blk = ti * 4 + bi
        rx = blk % RX
        hb = blk % 2
        xk = [("x1", rx, 0), ("x1", rx, 1)]
        for half in range(2):
            pb, pk = ps_rot()
            for q in range(4):
                kc = half * 4 + q
                tr(pb[:, q * 128:(q + 1) * 128], xr[rx][:, kc * 128:(kc + 1) * 128], ident_f, xk, [pk])
            evac_copy(h2T[hb][:, half * 4:(half + 1) * 4, :], pb.rearrange("p (q t) -> p q t", q=4), [pk],
                      [("h2T", hb, half)])
        for kc in range(8):
            mm(rl[:, bi * 36:(bi + 1) * 36], h2T[hb][:, kc, :], wr_s[:, kc, :], (bi == 0) and (kc == 0),
               (bi == 3) and (kc == 7), [("h2T", hb, kc // 4), "wr_s"], [pkr])

    def dve(fn, reads, writes):
        sc.add("dve", fn, reads, writes)

    def S2b1(ti):
        b0 = ti * 4
        bs = slice(b0, b0 + 4)
        tt("dve", lg, rl[:, 0:144].rearrange("p (b n) -> p b n", n=36),
           rs_t[:, ti % 2, :, None].broadcast_to([128, 4, 36]), ALU.mult,
           [pkr] + [("rs_t", ti % 2, b_) for b_ in range(4)], ["lg"])
        tt("dve", lg, lg, rbias[:, None, :].broadcast_to([128, 4, 36]), ALU.add, ["lg", "rbias"], ["lg"])
        gmax = r4s[:, 0, :]
        dve(lambda e: e.tensor_reduce(out=gmax, in_=lg[:, :, 0:4], axis=AX.X, op=ALU.max), ["lg"], ["gmax"])
        tt("dve", dg, lg[:, :, 0:4], gmax[:, :, None].broadcast_to([128, 4, 4]), ALU.subtract, ["lg", "gmax"], ["dg"])
        act(ge, dg, AF.Exp, ["dg"], ["ge"])
        gsum = r4s[:, 1, :]
        dve(lambda e: e.tensor_reduce(out=gsum, in_=ge, axis=AX.X, op=ALU.add), ["ge"], ["gsum"])
        gw = r4s[:, 2, :]
        recip(gw, gsum, ["gsum"], ["gw"])
        ts("dve", pen, dg, 0.0, None, ALU.is_equal, None, ["dg"], ["pen"])
        ts("dve", pen, pen, -1.0, 1e30, ALU.add, ALU.mult, ["pen"], ["pen"])
        tt("dve", msk.rearrange("p b (g e) -> p b g e", e=8), lg[:, :, 4:36].rearrange("p b (g e) -> p b g e", e=8),
           pen[:, :, :, None].broadcast_to([128, 4, 4, 8]), ALU.add, ["lg", "pen"], ["msk"])
        for b in range(4):
            dve(lambda e, b=b: e.max(out=top8a[:, b, :], in_=msk[:, b, :]), ["msk"], [("top8", b)])
            dve(lambda e, b=b: e.max_index(out=idx8a[:, b, :], in_max=top8a[:, b, :], in_values=msk[:, b, :]),
                ["msk", ("top8", b)], [("idx8", b)])
        T8 = [("top8", b) for b in range(4)]
        I8 = [("idx8", b) for b in range(4)]
        tcopy("dve", e_all[:, bs, :], idx8a[:, :, 0:2], I8, [("e_all", ti)])
        dlt = r4s[:, 3, :]
        tt("dve", dlt, top8a[:, :, 1], top8a[:, :, 0], ALU.subtract, T8, ["dlt"])
        ex_ = r4s[:, 4, :]
        act(ex_, dlt, AF.Exp, ["dlt"], ["ex_"])
        den = r4s[:, 5, :]
        ts("dve", den, ex_, 1.0, None, ALU.add, None, ["ex_"], ["den"])
        recip(den, den, ["den"], ["den"])
        tt("dve", cw_all[:, bs, 0], den, gw, ALU.mult, ["den", "gw"], [("cw", ti, 0)])
        tt("dve", ex_, ex_, den, ALU.mult, ["ex_", "den"], ["ex_"])
        tt("dve", cw_all[:, bs, 1], ex_, gw, ALU.mult, ["ex_", "gw"], [("cw", ti, 1)])
        iob = iota_e[:, None, :].broadcast_to([128, 4, 32])
        tt("dve", A1all[:, bs, :], iob, e_all[:, bs, 0:1].broadcast_to([128, 4, 32]), ALU.is_equal,
           ["iota_e", ("e_all", ti)], [("A1", ti)])
        tt("dve", A2all[:, bs, :], iob, e_all[:, bs, 1:2].broadcast_to([128, 4, 32]), ALU.is_equal,
           ["iota_e", ("e_all", ti)], [("A2", ti)])
        tt("dve", Ab, A1all[:, bs, :], A2all[:, bs, :], ALU.add, [("A1", ti), ("A2", ti)], ["Ab"])

    def S2b2(ti):
        b0 = ti * 4
        bs = slice(b0, b0 + 4)
        n_mm = 0
        tot_mm = 4 + 6 + 4
        for b in range(4):
            mm(pp[:, b * 32:(b + 1) * 32], Lst, Ab[:, b, :], n_mm == 0, False, ["Lst", "Ab"], [pkp])
            n_mm += 1
            for b_ in range(b):
                mm(pp[:, b * 32:(b + 1) * 32], ones_b, Ab[:, b_, :], False, False, ["ones_b", "Ab"], [pkp])
                n_mm += 1
        for b in range(4):
            n_mm += 1
            mm(pp[:, 128:160], ones_b, Ab[:, b, :], False, n_mm == tot_mm, ["ones_b", "Ab"], [pkp])
        tt("dve", Pt, pp[:, 0:128].rearrange("p (b n) -> p b n", n=32), carry[:, None, :].broadcast_to([128, 4, 32]),
           ALU.add, [pkp, "carry"], ["Pt"])
        tt("dve", carry, pp[:, 128:160], carry, ALU.add, [pkp, "carry", "Pt"], ["carry"])
        tt("dve", ptm, Pt, A1all[:, bs, :], ALU.mult, ["Pt", ("A1", ti)], ["ptm"])
        dve(lambda e: e.tensor_reduce(out=r_all[:, bs, 0], in_=ptm, axis=AX.X, op=ALU.add), ["ptm"], [("r_all", ti, 0)])
        tt("dve", ptm, Pt, A2all[:, bs, :], ALU.mult, ["Pt", ("A2", ti), ("r_all", ti, 0)], ["ptm"])
        dve(lambda e: e.tensor_reduce(out=r_all[:, bs, 1], in_=ptm, axis=AX.X, op=ALU.add), ["ptm"], [("r_all", ti, 1)])

    gcn4 = [0]

    def otrans(ti):
        ob2 = ti % 2
        for (osrc, odst, onm) in ((o_a, oaT2[ob2], "oaT"), (o_b, obT2[ob2], "obT")):
            for c in range(4):
                pb, pk = ps_rot()
                pbb = pb.bitcast(BF16)
                for bi in range(4):
                    tr(pbb[:, bi * 128:(bi + 1) * 128], osrc[:, ti * 4 + bi, c * 128:(c + 1) * 128], ident_b,
                       [("o_x",), "ident_b"], [pk])
                evac_copy(odst[:, c, :], pbb[:, 0:TT], [pk], [(onm, ob2, c)])

    def projmerge(ti):
        t0 = ti * TT
        ob2 = ti % 2
        oaT, obT = oaT2[ob2], obT2[ob2]
        for m in range(8):
            gi = gcn4[0] % 2
            gcn4[0] += 1
            dma("sp", gat[gi], ga_v[:, m, t0:t0 + TT], (), [("gat", gi)])
            dma("sp", gbt[gi], gb_v[:, m, t0:t0 + TT], (), [("gbt", gi)])
            pa, pka = ps_rot()
            for c in range(4):
                mm(pa, woa_b[:, c, m * 128:(m + 1) * 128], oaT[:, c, :], c == 0, c == 3, WOA + [("oaT", ob2, c)], [pka])
            pbk, pkb = ps_rot()
            for c in range(4):
                mm(pbk, wob_b[:, c, m * 128:(m + 1) * 128], obT[:, c, :], c == 0, c == 3, WOB + [("obT", ob2, c)], [pkb])
            i2 = m % 2
            tt("dve", mt1[i2], pa, gat[gi], ALU.mult, [pka, ("gat", gi)], [("mt1", i2)])
            tt("dve", mt2[i2], pbk, gbt[gi], ALU.mult, [pkb, ("gbt", gi)], [("mt2", i2)])
            tt("pool", mT[:, m, :], mt1[i2], mt2[i2], ALU.add, [("mt1", i2), ("mt2", i2)], [("mT", m)])

    otrans(0)
    projmerge(0)
    if NT4 > 1:
        otrans(1)
    for ti in range(NT4):
        S1(ti, 0)
        S1(ti, 1)
        S2a(ti, 0)
        S1(ti, 2)
        S2a(ti, 1)
        S1(ti, 3)
        S2a(ti, 2)
        if ti >= 1:
            S2b2(ti - 1)
        if ti + 1 < NT4:
            projmerge(ti + 1)
        S2a(ti, 3)
        if ti + 2 < NT4:
            otrans(ti + 2)
        S2b1(ti)
    S2b2(NT4 - 1)
    if dbg:
        rt_dbg = dscr("rt_dbg", [128, NB * 6], F32)
        rv = rt_dbg.rearrange("p (b s) -> p b s", s=6)
        ALLK = [("cw", t_, k_) for t_ in range(NT4) for k_ in range(2)] + [("e_all", t_) for t_ in range(NT4)] + [("r_all", t_, k_) for t_ in range(NT4) for k_ in range(2)]
        dma("sp", rv[:, :, 0:2], cw_all, ALLK, ["rt1"])
        dma("sp", rv[:, :, 2:4], e_all, ALLK, ["rt2"])
        dma("sp", rv[:, :, 4:6], r_all, ALLK, ["rt3"])
    sc.barrier()
    if stage <= 8:
        return finish(nc, es, sc, None)

    A.release(p4)
    padf = A.alloc([32], F32)
    padi = A.alloc([32], I32)
    incl = A.alloc([32], F32)
    offs = A.alloc([32], F32)
    ones32 = A.alloc([32], F32)
    big3 = A.alloc([NTILE, 32], F32)
    slotf = A.alloc([NB, 2], F32)
    slot_i = A.alloc([NB * 2], I32)
    jv = A.alloc([NTILE], F32)
    pidx = A.alloc([1], F32)
    tef = A.alloc([NTILE], F32)
    tesh = A.alloc([NTILE], F32)
    widx_i = A.alloc([NTILE], I32)
    p5 = A.mark()
    dma("sp", jv, jv_d, (), ["jv"])
    dma("sp", pidx, pidx_d, (), ["pidx"])
    memset("dve", ones32, 1.0, ["ones32"])
    ts("dve", padf, carry, float(SLOT_T - 1), None, ALU.add, None, ["carry"], ["padf"])
    tcopy("dve", padi, padf, ["padf"], ["padi"])
    ts("dve", padi, padi, SH, None, ALU.arith_shift_right, None, ["padi"], ["padi"])
    ts("dve", padi, padi, SH, None, ALU.logical_shift_left, None, ["padi"], ["padi"])
    tcopy("dve", padf, padi, ["padi"], ["padf"])
    sc.add("dve", lambda e: e.tensor_tensor_scan(out=incl, data0=ones32, data1=padf, initial=0.0,
                                                  op0=ALU.mult, op1=ALU.add), ["ones32", "padf"], ["incl"])
    tt("dve", offs, incl, padf, ALU.subtract, ["incl", "padf"], ["offs"])
    for k, Aall in ((0, A1all), (1, A2all)):
        tt("dve", big3[:, 0:NB, :], Aall, offs[:, None, :].broadcast_to([128, NB, 32]), ALU.mult, ["offs"], ["big3"])
        sc.add("dve", lambda e, k=k: e.tensor_reduce(out=slotf[:, :, k], in_=big3[:, 0:NB, :], axis=AX.X, op=ALU.add),
               ["big3"], [("slotf", k)])
    tt("dve", slotf, slotf, r_all, ALU.add, [("slotf", 0), ("slotf", 1)], ["slotf"])
    tcopy("dve", slot_i, slotf.rearrange("p b k -> p (b k)"), ["slotf"], ["slot_i"])
    tt("dve", big3, incl[:, None, :].broadcast_to([128, NTILE, 32]), jv[:, :, None].broadcast_to([128, NTILE, 32]),
       ALU.is_le, ["incl", "jv", ("slotf", 0), ("slotf", 1)], ["big3"])
    sc.add("dve", lambda e: e.tensor_reduce(out=tef, in_=big3, axis=AX.X, op=ALU.add), ["big3"], ["tef"])
    ts("dve", tef, tef, 31.0, None, ALU.min, None, ["tef"], ["tef"])
    memset("dve", tesh, -1.0, ["tesh"])
    tcopy("dve", tesh[:, 3:NTILE], tef[:, 0:NTILE - 3], ["tef", "tesh"], ["tesh"])
    tt("dve", tesh, tesh, tef, ALU.is_equal, ["tesh", "tef"], ["tesh"])
    ts("dve", tef, tef, 128.0, pidx[:, 0:1], ALU.mult, ALU.add, ["tef", "pidx"], ["tef"])
    stt(tef, tesh, 100000.0, tef, ALU.mult, ALU.add, ["tesh", "tef"], ["tef"])
    tcopy("dve", widx_i, tef, ["tef"], ["widx_i"])
    if dbg:
        sl_dbg = dscr("sl_dbg", [128, NB * 2 + NTILE], I32)
        dma("sp", sl_dbg[:, 0:NB * 2], slot_i, ["slot_i"], ["sl1"])
        dma("sp", sl_dbg[:, NB * 2:], widx_i, ["widx_i"], ["sl2"])
    hrow = [A.alloc([D], BF16) for _ in range(3)]
    for blk in range(NB):
        hr = hrow[blk % 3]
        dma("sp", hr, h2_d[blk * 128:(blk + 1) * 128, :], (), [("hrow", blk % 3)])
        for k in range(2):
            off_ap = slot_i[:, blk * 2 + k:blk * 2 + k + 1]
            sc.add("pool", lambda e, hr=hr, off_ap=off_ap: e.indirect_dma_start(
                out=xs_d[:, :], out_offset=bass.IndirectOffsetOnAxis(ap=off_ap, axis=0), in_=hr, in_offset=None),
                [("hrow", blk % 3), "slot_i"], [("xs_d", blk, k)], dma=True)
    sc.barrier()
    if stage <= 9:
        return finish(nc, es, sc, None)

    A.release(p5)
    while pre_pos[0] < len(pre_steps):
        precast_step()
    NW = 3
    wall_t = [A.alloc([6144], BF16) for _ in range(NW)]
    wg_t = [w[:, 0:2048] for w in wall_t]
    wu_t = [w[:, 2048:4096] for w in wall_t]
    wd_t = [w[:, 4096:6144] for w in wall_t]
    xs_t = [A.alloc([2, D], BF16) for _ in range(NW)]
    xsT = [A.alloc([8, SLOT_T], BF16) for _ in range(2)]
    sil = [A.alloc([2, SLOT_T], F32) for _ in range(2)]
    hidT = [A.alloc([2, SLOT_T], BF16) for _ in range(2)]
    yt = [A.alloc([D], F32) for _ in range(3)]
    ycn = 0
    breg = {}
    NTL = NTILE if stage > 10 else 4
    WXALL = [("wx", mi, e_) for mi in range(3) for e_ in range(32)]

    def prefetch6(j):
        b3 = j % NW
        wi = widx_i[:, j:j + 1]

        def wgather(e, wi=wi, b3=b3):
            if "r" not in breg:
                breg["r"] = e.alloc_register("wbound")
                e.reg_mov(breg["r"], 32 * 128 - 1)
            return e.indirect_dma_start(
                out=wall_t[b3], out_offset=None, in_=wx_d[:, :], in_offset=bass.IndirectOffsetOnAxis(ap=wi, axis=0),
                bounds_check=breg["r"], oob_is_err=False)
        sc.add("pool", wgather, (), [("wg", b3), ("wu", b3), ("wd", b3)], dma=True)
        dma("sp", xs_t[b3], xs_d[j * SLOT_T:(j + 1) * SLOT_T, :].rearrange("(s p) f -> p s f", p=128), (), [("xs_t", b3)])

    for j in range(min(2, NTL)):
        prefetch6(j)
    for j in range(NTL):
        if j + 2 < NTL:
            prefetch6(j + 2)
        b2 = j % 2
        b3 = j % NW
        for sbk in range(2):
            for half in range(2):
                pb, pk = ps_next()
                pbb = pb.bitcast(BF16)
                for q in range(4):
                    kc = half * 4 + q
                    tr(pbb[:, q * 128:(q + 1) * 128], xs_t[b3][:, sbk, kc * 128:(kc + 1) * 128], ident_b,
                       [("xs_t", b3), "ident_b"], [pk])
                evac_copy(xsT[b2][:, half * 4:(half + 1) * 4, sbk * 128:(sbk + 1) * 128],
                          pbb[:, 0:512].rearrange("p (q t) -> p q t", q=4), [pk], [("xsT", b2, sbk, half)])
        XST = [("xsT", b2, a, b) for a in range(2) for b in range(2)]
        pg, pkg = ps_next()
        for f in range(2):
            for kc in range(8):
                mm(pg[:, f * SLOT_T:(f + 1) * SLOT_T], wg_t[b3][:, kc * 256 + f * 128:kc * 256 + (f + 1) * 128],
                   xsT[b2][:, kc, :], kc == 0, kc == 7, [("wg", b3)] + XST, [pkg])
        pu, pku = ps_next()
        for f in range(2):
            for kc in range(8):
                mm(pu[:, f * SLOT_T:(f + 1) * SLOT_T], wu_t[b3][:, kc * 256 + f * 128:kc * 256 + (f + 1) * 128],
                   xsT[b2][:, kc, :], kc == 0, kc == 7, [("wu", b3)] + XST, [pku])
        act(sil[b2].rearrange("p f s -> p (f s)"), pg, AF.Silu, [pkg], [("sil", b2)])
        tt("dve", hidT[b2].rearrange("p f s -> p (f s)"), pu, sil[b2].rearrange("p f s -> p (f s)"), ALU.mult,
           [pku, ("sil", b2)], [("hidT", b2)])
        for sbk in range(2):
            y_ = yt[ycn % 3]
            yk = ("yt", ycn % 3)
            ycn += 1
            for half in range(2):
                py, pky = ps_next()
                for f in range(2):
                    mm(py, hidT[b2][:, f, sbk * 128:(sbk + 1) * 128], wd_t[b3][:, f * 1024 + half * 512:f * 1024 + (half + 1) * 512],
                       f == 0, f == 1, [("hidT", b2), ("wd", b3)], [pky])
                tt("dve", y_[:, half * 512:(half + 1) * 512], py, gt2_bc[:, half * 512:(half + 1) * 512], ALU.mult,
                   [pky], [(yk, half)])
            dma("sp", ys_d[j * SLOT_T + sbk * 128:j * SLOT_T + (sbk + 1) * 128, :], y_, [(yk, 0), (yk, 1)], [("ys_d", j, sbk)])
    sc.barrier()
    if stage <= 11:
        return finish(nc, es, sc, None)

    A.release(p5)
    gfin = A.alloc([D], F32)
    dma("sp", gfin, gfin_d[0:1, :].to_broadcast([128, D]), (), ["gfin"])
    R7 = 4
    g1 = [A.alloc([D], F32) for _ in range(R7)]
    g2_ = [A.alloc([D], F32) for _ in range(R7)]
    x1r = [A.alloc([D], F32) for _ in range(R7)]
    ot = [A.alloc([D], F32) for _ in range(2)]
    junk7 = A.alloc([D], BF16)
    sm7 = A.alloc([8], F32)

    def load7(blk):
        i4 = blk % R7
        for k, gt_ in ((0, g1), (1, g2_)):
            off_ap = slot_i[:, blk * 2 + k:blk * 2 + k + 1]
            sc.add("pool", lambda e, gt_=gt_, off_ap=off_ap, i4=i4: e.indirect_dma_start(
                out=gt_[i4], out_offset=None, in_=ys_d[:, :], in_offset=bass.IndirectOffsetOnAxis(ap=off_ap, axis=0)),
                (), [("g", k, i4)], dma=True)
        dma("sp", x1r[i4], x1_d[blk * 128:(blk + 1) * 128, :], (), [("x1r", i4)])

    for blk in range(2):
        load7(blk)
    for blk in range(NB):
        if blk + 2 < NB:
            load7(blk + 2)
        i4 = blk % R7
        i2 = blk % 2
        stt(x1r[i4], g1[i4], cw_all[:, blk, 0:1], x1r[i4], ALU.mult, ALU.add, [("g", 0, i4), ("x1r", i4)], [("x1r", i4)])
        stt(x1r[i4], g2_[i4], cw_all[:, blk, 1:2], x1r[i4], ALU.mult, ALU.add, [("g", 1, i4), ("x1r", i4)], [("x1r", i4)])
        act(junk7, x1r[i4], AF.Square, [("x1r", i4)], [("ss7", i2)], accum_out=sm7[:, i2:i2 + 1])
        act(sm7[:, 2 + i2:3 + i2], sm7[:, i2:i2 + 1], AF.Sqrt, [("ss7", i2)], [("rs7", i2)], scale=1.0 / D, bias=eps_c[:, 0:1])
        recip(sm7[:, 2 + i2:3 + i2], sm7[:, 2 + i2:3 + i2], [("rs7", i2)], [("rs7", i2)])
        stt(ot[i2], x1r[i4], sm7[:, 2 + i2:3 + i2], gfin, ALU.mult, ALU.mult, [("x1r", i4), ("rs7", i2), "gfin"], [("ot", i2)])
        dma("sp", out_d[blk * 128:(blk + 1) * 128, :], ot[i2], [("ot", i2)], [("out", blk)])
    sc.barrier()
    return finish(nc, es, sc, None)


def finish(nc, es, sc, _):
    block = es.enter_context(nc.Block())
    sc.emit(block)
    es.close()
    return nc


def fm(v, k):
    return np.ascontiguousarray(np.asarray(v, np.float32).reshape(k, 128).T)


def kmajor(w):
    K, N = w.shape
    return np.ascontiguousarray(w.reshape(K // 128, 128, N).transpose(1, 0, 2).reshape(128, (K // 128) * N))


def host_inputs(I):
    shared = {}
    shared["wada"] = kmajor(I["w_ada"][0])
    shared["bada_row"] = np.ascontiguousarray(I["b_ada"][0].reshape(1, 6144).astype(np.float32))
    shared["gmix_row"] = np.ascontiguousarray(I["g_mix"][0].reshape(1, D).astype(np.float32))
    shared["gffn_row"] = np.ascontiguousarray(I["g_ffn"][0].reshape(1, D).astype(np.float32))
    shared["gfinal_row"] = np.ascontiguousarray(I["g_final"].reshape(1, D).astype(np.float32))
    w_in = I["w_in"][0]
    kr = w_in[:, 384:416]
    z64 = np.zeros((D, 64), np.float32)
    kr_sw = np.concatenate([kr[:, 16:32], kr[:, 0:16]], axis=1)
    win_ext = np.concatenate([w_in, z64, kr, z64, kr_sw], axis=1)
    assert win_ext.shape[1] == WC
    shared["win"] = kmajor(win_ext)
    shared["gq_fm"] = fm(I["g_q"][0], 2)
    shared["gkv_fm"] = fm(I["g_kv"][0], 1)
    wuq = I["w_uq"][0]
    wuq_sw = wuq.reshape(256, 8, 96).copy()
    wuq_sw[:, :, 64:80] = wuq.reshape(256, 8, 96)[:, :, 80:96]
    wuq_sw[:, :, 80:96] = wuq.reshape(256, 8, 96)[:, :, 64:80]
    shared["wuq"] = kmajor(wuq)
    shared["wuq_sw"] = kmajor(wuq_sw.reshape(256, 768))
    shared["wuk"] = np.ascontiguousarray(I["w_uk"][0])
    shared["wuv"] = np.ascontiguousarray(I["w_uv"][0])
    rc = np.zeros((128, 2), np.float32)
    inv = (10000.0 ** (-np.arange(16, dtype=np.float32) / 16)).astype(np.float32)
    for p in range(128):
        rc[p, 0] = inv[p % 16]
    shared["rconst"] = rc
    shared["ident"] = np.eye(128, dtype=np.float32)
    sel = np.zeros((128, 96), np.float32)
    for p in range(64, 96):
        sel[p, p] = 1.0
    shared["sel"] = sel
    kk = np.arange(128)[:, None]
    qq = np.arange(640)[None, :]
    idx = np.clip(qq - kk, -256, 256) + 256
    dq = qq // 64 - kk // 64
    valid = (dq >= 0) & (dq <= 8)
    rb = I["rel_bias"][0]
    bt = np.where(valid[None], rb[:, idx], np.float32(-1e30)).astype(np.float32)
    shared["biasT"] = np.ascontiguousarray(bt.transpose(1, 0, 2).reshape(128, 8 * 640))
    shared["woa"] = kmajor(I["w_oa"][0])
    shared["wob"] = kmajor(I["w_ob"][0])
    shared["wout"] = kmajor(I["w_out"][0])
    shared["wr"] = kmajor(np.concatenate([I["w_rg"][0], I["w_re"][0]], axis=1))
    shared["rb"] = np.ascontiguousarray(np.concatenate([I["b_rg"][0], I["b_re"][0]]).reshape(1, 36).astype(np.float32))
    shared["iota_e"] = np.ascontiguousarray(np.broadcast_to(np.arange(32, dtype=np.float32), (128, 32)))
    shared["lst"] = np.triu(np.ones((128, 128), np.float32), 1)
    shared["jv"] = np.ascontiguousarray(np.broadcast_to((np.arange(NTILE, dtype=np.float32) * SLOT_T), (128, NTILE)))
    shared["pidx"] = np.arange(128, dtype=np.float32).reshape(128, 1)
    shared["wgl"] = np.ascontiguousarray(I["w_gate"][0].reshape(32, 8, 128, 256).transpose(0, 2, 1, 3).reshape(32 * 128, 2048))
    shared["wul"] = np.ascontiguousarray(I["w_up"][0].reshape(32, 8, 128, 256).transpose(0, 2, 1, 3).reshape(32 * 128, 2048))
    shared["wdl"] = np.ascontiguousarray(I["w_down"][0].reshape(32, 2, 128, 1024).transpose(0, 2, 1, 3).reshape(32 * 128, 2048))
    per_core = []
    for b in range(8):
        d = dict(shared)
        d["x"] = np.ascontiguousarray(I["x"][b])
        d["cfm"] = fm(I["c"][b], 8)
        d["pos"] = np.ascontiguousarray(I["positions"][b].reshape(1, S).astype(np.int32))
        per_core.append(d)
    return per_core


_NC_CACHE = {}


def kernel(**inputs):
    I = {k: np.asarray(v) for k, v in inputs.items()}
    in_maps = host_inputs(I)
    if "nc" not in _NC_CACHE:
        _NC_CACHE["nc"] = build_nc()
    nc = _NC_CACHE["nc"]
    res = run_bass_kernel_spmd(nc, in_maps, core_ids=list(range(8)))
    return np.stack([r["out"] for r in res.results], axis=0).astype(np.float32)
```

```python
import os
import math
from contextlib import ExitStack

import numpy as np
import concourse.bass as bass
import concourse.mybir as mybir
from concourse.bass_utils import run_bass_kernel_spmd

F32 = mybir.dt.float32
BF16 = mybir.dt.bfloat16
I32 = mybir.dt.int32
U32 = mybir.dt.uint32
U8 = mybir.dt.uint8
AF = mybir.ActivationFunctionType
ALU = mybir.AluOpType
AX = mybir.AxisListType

S = 4096
D = 1024
TT = 512
NT = S // TT
NB = S // 128
EPS = 1e-6
WC = 4192
C_QB, C_KB, C_VB, C_GA, C_GB, C_KRP, C_KRS = 416, 928, 1440, 1952, 2976, 4000, 4096
PI = math.pi
SLOT_T = 256
SH = 8
NTILE = 64
NSLOT = NTILE * SLOT_T


class Sched:
    COMPUTE = ("pe", "act", "dve", "pool")

    def __init__(self, nc, es, n_dma_sems=8):
        self.nc = nc
        self.ops = []
        self.n_dma_sems = n_dma_sems
        self.eng_sem = {e: es.enter_context(nc.semaphore("c_" + e)) for e in self.COMPUTE}
        self.dma_sems = {q: [es.enter_context(nc.semaphore("d_%s%d" % (q, i))) for i in range(n_dma_sems)]
                         for q in ("sp", "pool", "act")}
        self.state = {}
        self.last_on = {}
        self.dma_since_bar = []

    def add(self, eng, fn, reads=(), writes=(), dma=False, extra_deps=()):
        op = dict(eng=eng, fn=fn, dma=dma, idx=len(self.ops), signal=False)
        deps = set(extra_deps)
        st = self.state
        for k in reads:
            w, rd = st.setdefault(k, [None, {}])
            if w is not None:
                deps.add(w)
        for k in writes:
            w, rd = st.setdefault(k, [None, {}])
            if w is not None:
                deps.add(w)
            deps.update(rd.values())
        me = (eng, "dma", op["idx"]) if dma else eng
        for k in reads:
            st[k][1][me] = op["idx"]
        for k in writes:
            st[k][0] = op["idx"]
            st[k][1] = {}
        real = set()
        for d in deps:
            dop = self.ops[d]
            if (not dop["dma"]) and (not dma) and dop["eng"] == "pe" and eng == "pe":
                continue
            real.add(d)
            dop["signal"] = True
        op["deps"] = real
        self.ops.append(op)
        if dma:
            self.dma_since_bar.append(op["idx"])
        elif fn is not None:
            self.last_on[eng] = op["idx"]
        return op

    def barrier(self):
        lasts = dict(self.last_on)
        dmas = list(self.dma_since_bar)
        self.dma_since_bar = []
        for eng in ("pe", "act", "dve", "pool", "sp"):
            deps = [v for e, v in lasts.items() if e != eng] + dmas
            self.add(eng, None, extra_deps=deps)
        self.state = {}

    def emit(self, block):
        cnt = {e: 0 for e in self.COMPUTE}
        dcnt = {q: 0 for q in self.dma_sems}
        for op in self.ops:
            if not op["signal"]:
                continue
            if op["dma"]:
                q = op["eng"]
                i = dcnt[q]
                dcnt[q] += 1
                op["sem"] = self.dma_sems[q][i % self.n_dma_sems]
                op["val"] = 16 * (i // self.n_dma_sems + 1)
            else:
                e = op["eng"]
                assert op["fn"] is not None
                cnt[e] += 1
                op["sem"] = self.eng_sem[e]
                op["val"] = cnt[e]
        self.stats = dict(cnt=cnt, dcnt=dcnt, nops=len(self.ops))
        per_eng = {e: [] for e in ("pe", "act", "dve", "pool", "sp")}
        for op in self.ops:
            per_eng[op["eng"]].append(op)
        ops = self.ops

        def run(eng_name, engine):
            seen = {}
            for op in per_eng[eng_name]:
                need = {}
                for d in op["deps"]:
                    dop = ops[d]
                    s, v = dop["sem"], dop["val"]
                    if need.get(s.num, (None, 0))[1] < v:
                        need[s.num] = (s, v)
                for num, (s, v) in need.items():
                    if seen.get(num, 0) >= v:
                        continue
                    engine.wait_ge(s, v)
                    seen[num] = v
                if op["fn"] is None:
                    continue
                ins = op["fn"](engine)
                if op["signal"]:
                    ins.then_inc(op["sem"], 16 if op["dma"] else 1)

        block.tensor(lambda e: run("pe", e))
        block.scalar(lambda e: run("act", e))
        block.vector(lambda e: run("dve", e))
        block.gpsimd(lambda e: run("pool", e))
        block.sync(lambda e: run("sp", e))


class Arena:
    def __init__(self, big, size):
        self.big = big
        self.size = size
        self.top = 0

    def alloc(self, free_shape, dtype):
        esz = {F32: 4, BF16: 2, I32: 4, U32: 4, U8: 1}[dtype]
        n = int(np.prod(free_shape))
        nbytes = (n * esz + 63) // 64 * 64
        off = self.top
        self.top += nbytes
        assert self.top <= self.size, "SBUF arena overflow %d > %d" % (self.top, self.size)
        v = self.big[:, off:off + n * esz]
        if dtype != U8:
            v = v.bitcast(dtype)
        if len(free_shape) == 2:
            v = v.rearrange("p (a b) -> p a b", b=free_shape[1])
        elif len(free_shape) == 3:
            v = v.rearrange("p (a b c) -> p a b c", b=free_shape[1], c=free_shape[2])
        return v

    def mark(self):
        return self.top

    def release(self, m):
        self.top = m


def build_nc(stage=99, dbg=False):
    sub = int(os.environ.get('KSUB', '99'))
    nc = bass.Bass("TRN2", target_bir_lowering=False)
    es = ExitStack()

    def din(name, shape, dt=F32):
        return nc.dram_tensor(name, list(shape), dt, kind="ExternalInput").ap()

    def dscr(name, shape, dt):
        kind = "ExternalOutput" if dbg else "Internal"
        return nc.dram_tensor(name, list(shape), dt, kind=kind).ap()

    x_d = din("x", [S, D])
    cfm_d = din("cfm", [128, 8])
    pos_d = din("pos", [1, S], I32)
    wada_d = din("wada", [128, 8 * 6144])
    badar_d = din("bada_row", [1, 6144])
    gmixr_d = din("gmix_row", [1, D])
    gffnr_d = din("gffn_row", [1, D])
    gfin_d = din("gfinal_row", [1, D])
    win_d = din("win", [128, 8 * WC])
    gq_d = din("gq_fm", [128, 2])
    gkv_d = din("gkv_fm", [128, 1])
    wuq_d = din("wuq", [128, 2 * 768])
    wuqs_d = din("wuq_sw", [128, 2 * 768])
    wuk_d = din("wuk", [128, 512])
    wuv_d = din("wuv", [128, 512])
    rconst_d = din("rconst", [128, 2])
    ident_d = din("ident", [128, 128])
    sel_d = din("sel", [128, 96])
    biasT_d = din("biasT", [128, 8 * 640])
    woa_d = din("woa", [128, 4 * D])
    wob_d = din("wob", [128, 4 * D])
    wout_d = din("wout", [128, 8 * D])
    wr_d = din("wr", [128, 8 * 36])
    rb_d = din("rb", [1, 36])
    iota_d = din("iota_e", [128, 32])
    lst_d = din("lst", [128, 128])
    jv_d = din("jv", [128, NTILE])
    pidx_d = din("pidx", [128, 1])
    wg_d = din("wgl", [32 * 128, 2048])
    wu_d = din("wul", [32 * 128, 2048])
    wdn_d = din("wdl", [32 * 128, 2048])
    out_d = nc.dram_tensor("out", [S, D], F32, kind="ExternalOutput").ap()

    tabc_d = dscr("tabc", [128, 512], F32)
    tabsp_d = dscr("tabsp", [128, 512], F32)
    tabsn_d = dscr("tabsn", [128, 512], F32)
    qT_d = dscr("qT", [96, 8 * S], BF16)
    kT_d = dscr("kT", [96, 8 * S], BF16)
    va_d = dscr("va", [S, 520], BF16)
    qbT_d = dscr("qbT", [128, 4 * S], BF16)
    kbT_d = dscr("kbT", [128, 4 * S], BF16)
    vb_d = dscr("vb", [S, 520], BF16)
    ga_d = dscr("gaT", [128, 8 * S], F32)
    gb_d = dscr("gbT", [128, 8 * S], F32)
    moddbg_d = dscr("moddbg", [128, 48], F32) if dbg else None
    x1_d = dscr("x1s", [S, D], F32)
    h2_d = dscr("h2s", [S, D], BF16)
    xs_d = dscr("xs", [NSLOT, D], BF16)
    ys_d = dscr("ys", [NSLOT, D], F32)
    wx_d = nc.dram_tensor("wx", [32 * 128, 6144], BF16, kind="Internal").ap()

    SB_BYTES = 212480
    big = nc.alloc_sbuf_tensor("big", [128, SB_BYTES], U8)
    A = Arena(big, SB_BYTES)
    banks = [nc.alloc_psum_tensor("psb%d" % i, [128, 512], F32).ap() for i in range(8)]
    sc = Sched(nc, es)

    psn = [0]

    def ps_next():
        i = psn[0] % 8
        psn[0] += 1
        return banks[i], ("ps", i)

    def dma(q, out, in_, reads, writes, **kw):
        sc.add(q, lambda e: e.dma_start(out=out, in_=in_, **kw), reads, writes, dma=True)

    def mm(out, lhsT, rhs, start, stop, reads, writes):
        sc.add("pe", lambda e: e.matmul(out, lhsT, rhs, start=start, stop=stop), reads, writes)

    def tr(out, in_, ident, reads, writes):
        sc.add("pe", lambda e: e.transpose(out, in_, ident), reads, writes)

    def act(out, in_, func, reads, writes, **kw):
        sc.add("act", lambda e: e.activation(out=out, in_=in_, func=func, **kw), reads, writes)

    def tcopy(eng, out, in_, reads, writes):
        if eng == "act":
            sc.add(eng, lambda e: e.activation(out=out, in_=in_, func=AF.Copy), reads, writes)
        else:
            sc.add(eng, lambda e: e.tensor_copy(out=out, in_=in_), reads, writes)

    def tt(eng, out, in0, in1, op, reads, writes):
        sc.add(eng, lambda e: e.tensor_tensor(out=out, in0=in0, in1=in1, op=op), reads, writes)

    def ts(eng, out, in0, s1, s2, op0, op1, reads, writes):
        if s2 is None:
            sc.add(eng, lambda e: e.tensor_scalar(out=out, in0=in0, scalar1=s1, scalar2=None, op0=op0),
                   reads, writes)
        else:
            sc.add(eng, lambda e: e.tensor_scalar(out=out, in0=in0, scalar1=s1, scalar2=s2, op0=op0, op1=op1),
                   reads, writes)

    def stt(out, in0, scalar, in1, op0, op1, reads, writes):
        sc.add("dve", lambda e: e.scalar_tensor_tensor(out=out, in0=in0, scalar=scalar, in1=in1, op0=op0, op1=op1),
               reads, writes)

    def memset(eng, ap, val, writes):
        sc.add(eng, lambda e: e.memset(ap, val), (), writes)

    def recip(out, in_, reads, writes):
        sc.add("dve", lambda e: e.reciprocal(out=out, in_=in_), reads, writes)

    ident_f = A.alloc([128], F32)
    ident_b = A.alloc([128], BF16)
    ones_f = A.alloc([128], F32)
    ones_b = A.alloc([128], BF16)
    eps_c = A.alloc([1], F32)
    modfm = A.alloc([48], F32)
    s1_fm = A.alloc([8], F32)
    s2_fm = A.alloc([8], F32)
    gt1_bc = A.alloc([D], F32)
    gt2_bc = A.alloc([D], F32)
    s2_bc = A.alloc([D], F32)
    b2_bc = A.alloc([D], F32)

    stg = A.alloc([2048], BF16)
    pre_steps = []
    for mi, wsrc in enumerate((wg_d, wu_d, wdn_d)):
        for e_ in range(32):
            pre_steps.append(("ld", mi, wsrc, e_))
            pre_steps.append(("st", mi, wsrc, e_))
    pre_pos = [0]

    def precast_step():
        if pre_pos[0] >= len(pre_steps):
            return
        kind, mi, wsrc, e_ = pre_steps[pre_pos[0]]
        pre_pos[0] += 1
        if kind == "ld":
            dma("pool", stg, wsrc[e_ * 128:(e_ + 1) * 128, :], (), ["stg"])
        else:
            dma("sp", wx_d[e_ * 128:(e_ + 1) * 128, mi * 2048:(mi + 1) * 2048], stg, ["stg"], [("wx", mi, e_)])

    dma("sp", ident_f, ident_d, (), ["ident_f"])
    tcopy("dve", ident_b, ident_f, ["ident_f"], ["ident_b"])
    memset("dve", ones_f, 1.0, ["ones_f"])
    memset("dve", ones_b, 1.0, ["ones_b"])
    memset("dve", eps_c, EPS, ["eps_c"])

    pP = A.mark()
    wuq_b = A.alloc([2, 768], BF16)
    wuqs_b = A.alloc([2, 768], BF16)
    wukp_b = A.alloc([8, 96], BF16)
    wuv_b = A.alloc([512], BF16)
    sel_b = A.alloc([96], BF16)
    gq = A.alloc([2], F32)
    gkv = A.alloc([1], F32)
    win_b = A.alloc([8, WC], BF16)
    winv = win_d.rearrange("p (k n) -> p k n", k=8)
    for kc in range(8):
        for c0 in range(0, WC, 1048):
            dma("pool", win_b[:, kc, c0:c0 + 1048], winv[:, kc, c0:c0 + 1048], (), [("win", kc, c0)])
    p0 = A.mark()
    cfm = A.alloc([8], F32)
    cact = A.alloc([8], F32)
    c_rep = A.alloc([8, 128], F32)
    bada_bc = A.alloc([6144], F32)
    gmix_bc = A.alloc([D], F32)
    gffn_bc = A.alloc([D], F32)
    sh1_r = A.alloc([D], F32)
    sc1_r = A.alloc([D], F32)
    sc2_r = A.alloc([D], F32)
    dtmp = A.alloc([128], F32)
    wbuf = [A.alloc([3072], F32) for _ in range(2)]
    dma("sp", cfm, cfm_d, (), ["cfm"])
    dma("sp", bada_bc, badar_d[0:1, :].to_broadcast([128, 6144]), (), ["bada_bc"])
    dma("sp", gmix_bc, gmixr_d[0:1, :].to_broadcast([128, D]), (), ["gmix_bc"])
    dma("sp", gffn_bc, gffnr_d[0:1, :].to_broadcast([128, D]), (), ["gffn_bc"])
    act(cact, cfm, AF.Silu, ["cfm"], ["cact"])
    tcopy("dve", c_rep, cact[:, :, None].broadcast_to([128, 8, 128]), ["cact"], ["c_rep"])
    wv = wada_d.rearrange("p (k n) -> p k n", k=8)
    dests = [sh1_r, sc1_r, gt1_bc, b2_bc, sc2_r, gt2_bc]
    dkeys = ["sh1_r", "sc1_r", "gt1_bc", "b2_bc", "sc2_r", "gt2_bc"]
    wcn = 0
    for hf_ in range(2):
        for kc in range(8):
            wb = wbuf[wcn % 2]
            wk = ("wbuf", wcn % 2)
            wcn += 1
            dma("sp", wb, wv[:, kc, hf_ * 3072:(hf_ + 1) * 3072], (), [wk])
            for nt in range(6):
                mm(banks[nt], c_rep[:, kc, :], wb[:, nt * 512:(nt + 1) * 512], kc == 0, kc == 7,
                   [wk, "c_rep"], [("ps", nt)])
        for nt in range(6):
            n0 = hf_ * 3072 + nt * 512
            di = n0 // 1024
            tt("dve", dests[di][:, n0 % 1024:n0 % 1024 + 512], banks[nt], bada_bc[:, n0:n0 + 512], ALU.add,
               [("ps", nt), "bada_bc"], [(dkeys[di], (n0 % 1024) // 512)])
    K2 = lambda nm: [(nm, 0), (nm, 1)]
    stt(sc1_r, sc1_r, 1.0, gmix_bc, ALU.add, ALU.mult, K2("sc1_r") + ["gmix_bc"], ["s1_r"])
    stt(s2_bc, sc2_r, 1.0, gffn_bc, ALU.add, ALU.mult, K2("sc2_r") + ["gffn_bc"], ["s2_bc"])
    for (row, rkeys, dst_fm, dk) in ((sc1_r, ["s1_r"], s1_fm, "s1"), (sh1_r, K2("sh1_r"), modfm, "modfm")):
        for kc in range(8):
            tt("dve", dtmp, row[:, kc * 128:(kc + 1) * 128], ident_f, ALU.mult, rkeys + ["ident_f"], ["dtmp"])
            sc.add("dve", lambda e, dst_fm=dst_fm, kc=kc: e.tensor_reduce(out=dst_fm[:, kc:kc + 1], in_=dtmp, axis=AX.X,
                                                                         op=ALU.add), ["dtmp"], [dk])

    rconst = A.alloc([2], F32)
    dma("sp", rconst, rconst_d, (), ["rconst"])
    HALF = 512
    posi = A.alloc([HALF], I32)
    ang = A.alloc([HALF], F32)
    halfpi = A.alloc([1], F32)
    memset("dve", halfpi, PI / 2, ["halfpi"])
    tmpS = [A.alloc([HALF], F32) for _ in range(4)]
    kiS = A.alloc([HALF], I32)
    for cb in range(8):
        dma("sp", posi[cb * 16:(cb + 1) * 16, :], pos_d[0:1, cb * 512:(cb + 1) * 512].to_broadcast([16, 512]), (),
            [("posi", cb)])
    POSI = [("posi", cb) for cb in range(8)]
    tcopy("dve", ang, posi, POSI, ["ang"])
    ts("dve", ang, ang, rconst[:, 0:1], None, ALU.mult, None, ["ang", "rconst"], ["ang"])
    t1, r0, mk, t2 = tmpS
    ts("dve", t1, ang, 1.0 / (2 * PI), None, ALU.mult, None, ["ang"], ["s_t1"])
    tcopy("dve", kiS, t1, ["s_t1"], ["s_ki"])
    stt(r0, kiS, -2 * PI, ang, ALU.mult, ALU.add, ["s_ki", "ang"], ["s_r0"])
    ts("dve", mk, r0, PI, -2 * PI, ALU.is_gt, ALU.mult, ["s_r0"], ["s_mk"])
    tt("dve", r0, r0, mk, ALU.add, ["s_r0", "s_mk"], ["s_r0"])
    ts("dve", mk, r0, -PI, 2 * PI, ALU.is_lt, ALU.mult, ["s_r0"], ["s_mk"])
    tt("dve", r0, r0, mk, ALU.add, ["s_r0", "s_mk"], ["s_r0"])
    ts("dve", r0, r0, PI, -PI, ALU.min, ALU.max, ["s_r0"], ["s_r0"])
    act(t1, r0, AF.Sin, ["s_r0"], ["s_t1"])
    dma("sp", tabsp_d, t1, ["s_t1"], ["tabsp"])
    act(t2, r0, AF.Sin, ["s_r0"], ["s_t2"], scale=-1.0)
    dma("sp", tabsn_d, t2, ["s_t2"], ["tabsn"])
    ts("dve", t1, ang, PI / 2, 1.0 / (2 * PI), ALU.add, ALU.mult, ["ang", "s_t1"], ["s_t1"])
    tcopy("dve", kiS, t1, ["s_t1"], ["s_ki"])
    stt(r0, kiS, -2 * PI, ang, ALU.mult, ALU.add, ["s_ki", "ang", "s_r0"], ["s_r0"])
    ts("dve", mk, r0, PI / 2, -2 * PI, ALU.is_gt, ALU.mult, ["s_r0"], ["s_mk"])
    tt("dve", r0, r0, mk, ALU.add, ["s_r0", "s_mk"], ["s_r0"])
    ts("dve", mk, r0, -1.5 * PI, 2 * PI, ALU.is_lt, ALU.mult, ["s_r0"], ["s_mk"])
    tt("dve", r0, r0, mk, ALU.add, ["s_r0", "s_mk"], ["s_r0"])
    ts("dve", r0, r0, PI / 2, -1.5 * PI, ALU.min, ALU.max, ["s_r0"], ["s_r0"])
    act(t1, r0, AF.Sin, ["s_r0", "halfpi"], ["s_t1"], bias=halfpi[:, 0:1])
    dma("sp", tabc_d, t1, ["s_t1"], ["tabc"])

    wtmp = A.alloc([2, 768], F32)
    wtmp2 = A.alloc([2, 768], F32)
    wtmp3 = A.alloc([512], F32)
    wtmp4 = A.alloc([512], F32)
    seltmp = A.alloc([96], F32)
    dma("sp", gq, gq_d, (), ["gq"])
    dma("sp", gkv, gkv_d, (), ["gkv"])
    dma("sp", wtmp, wuq_d.rearrange("p (k n) -> p k n", k=2), (), ["wtmp"])
    dma("sp", wtmp2, wuqs_d.rearrange("p (k n) -> p k n", k=2), (), ["wtmp2"])
    dma("sp", wtmp3, wuk_d, (), ["wtmp3"])
    dma("sp", wtmp4, wuv_d, (), ["wtmp4"])
    dma("sp", seltmp, sel_d, (), ["seltmp"])
    for kc in range(2):
        ts("dve", wuq_b[:, kc, :], wtmp[:, kc, :], gq[:, kc:kc + 1], None, ALU.mult, None, ["wtmp", "gq"], ["wuq_b"])
        ts("dve", wuqs_b[:, kc, :], wtmp2[:, kc, :], gq[:, kc:kc + 1], None, ALU.mult, None, ["wtmp2", "gq"], ["wuqs_b"])
    memset("dve", wukp_b, 0.0, ["wukp_b"])
    ts("dve", wukp_b[:, :, 0:64], wtmp3.rearrange("p (h d) -> p h d", d=64), gkv[:, 0:1], None, ALU.mult, None,
       ["wtmp3", "gkv", "wukp_b"], ["wukp_b"])
    ts("dve", wuv_b, wtmp4, gkv[:, 0:1], None, ALU.mult, None, ["wtmp4", "gkv"], ["wuv_b"])
    tcopy("dve", sel_b[0:96], seltmp[0:96], ["seltmp"], ["sel_b"])

    sc.barrier()
    A.release(p0)
    if stage <= 0:
        return finish(nc, es, sc, None)

    WIN = []

    xb = [A.alloc([D], F32) for _ in range(2)]
    junk = A.alloc([D], BF16)
    ssq = A.alloc([4], F32)
    rstd = A.alloc([4], F32)
    xn = [A.alloc([D], F32) for _ in range(2)]
    hT = [A.alloc([8, TT], BF16) for _ in range(2)]
    ctab = A.alloc([TT], F32)
    stab = A.alloc([TT], F32)
    qlat = A.alloc([2, TT], F32)
    qsq = A.alloc([2, TT], F32)
    kvlat = A.alloc([TT], F32)
    kvsq = A.alloc([TT], F32)
    rbc = [A.alloc([TT], F32) for _ in range(2)]
    qln = A.alloc([2, TT], BF16)
    kvn = A.alloc([TT], BF16)
    krr = A.alloc([TT], BF16)
    rt1 = [A.alloc([TT], F32) for _ in range(2)]
    rt2 = [A.alloc([TT], F32) for _ in range(2)]
    qT_s = A.alloc([8, TT], BF16)
    kT_s = A.alloc([8, TT], BF16)
    qbT_s = A.alloc([4, TT], BF16)
    kbT_s = A.alloc([4, TT], BF16)
    va_s = A.alloc([4, 520], BF16)
    vb_s = A.alloc([4, 520], BF16)
    gst = [A.alloc([TT], F32) for _ in range(4)]
    memset("dve", ctab[0:64], 1.0, ["ctab0"])
    memset("dve", stab[0:64], 0.0, ["stab0"])
    memset("dve", va_s, 1.0, ["va_s"])
    memset("dve", vb_s, 1.0, ["vb_s"])

    qT_v = qT_d.rearrange("p (h t) -> p h t", h=8)
    kT_v = kT_d.rearrange("p (h t) -> p h t", h=8)
    qbT_v = qbT_d.rearrange("p (h t) -> p h t", h=4)
    kbT_v = kbT_d.rearrange("p (h t) -> p h t", h=4)
    ga_v = ga_d.rearrange("p (c t) -> p c t", c=8)
    gb_v = gb_d.rearrange("p (c t) -> p c t", c=8)
    gcnt = [0]
    ecnt = [0]

    def evac_copy(out, in_, reads, writes):
        eng = "act" if ecnt[0] % 2 == 0 else "dve"
        ecnt[0] += 1
        tcopy(eng, out, in_, reads, writes)

    NT1 = NT if stage > 1 else 1

    def prep_stats(ti, bi):
        t0 = ti * TT
        g = ti * 4 + bi
        xt = xb[g % 2]
        xk = ("xb", g % 2)
        dma("sp", xt, x_d[t0 + bi * 128:t0 + (bi + 1) * 128, :], (), [xk])
        act(junk, xt, AF.Square, [xk], [("ssq", bi)], accum_out=ssq[:, bi:bi + 1])
        act(rstd[:, bi:bi + 1], ssq[:, bi:bi + 1], AF.Sqrt, [("ssq", bi), "eps_c"], [("rstd", bi)],
            scale=1.0 / D, bias=eps_c[:, 0:1])
        recip(rstd[:, bi:bi + 1], rstd[:, bi:bi + 1], [("rstd", bi)], [("rstd", bi)])
        ts("dve", xn[g % 2], xt, rstd[:, bi:bi + 1], None, ALU.mult, None, [xk, ("rstd", bi)], [("xn", g % 2)])

    def prep_tr(ti, bi):
        g = ti * 4 + bi
        xnt = xn[g % 2]
        nk = ("xn", g % 2)
        h_t = hT[ti % 2]
        for half in range(2):
            pb, pk = ps_next()
            for q in range(4):
                kc = half * 4 + q
                tr(pb[:, q * 128:(q + 1) * 128], xnt[:, kc * 128:(kc + 1) * 128], ident_f, [nk, "ident_f"], [pk])
            for q in range(4):
                kc = half * 4 + q
                dst = h_t[:, kc, bi * 128:(bi + 1) * 128]
                hk = ("hT", ti % 2, bi, q % 2)
                act(dst, pb[:, q * 128:(q + 1) * 128], AF.Identity, [pk, "s1", "modfm"], [hk],
                    scale=s1_fm[:, kc:kc + 1], bias=modfm[:, kc:kc + 1])

    def prep_all(ti):
        prep_stats(ti, 0)
        prep_stats(ti, 1)
        prep_tr(ti, 0)
        prep_stats(ti, 2)
        prep_tr(ti, 1)
        prep_stats(ti, 3)
        prep_tr(ti, 2)
        prep_tr(ti, 3)

    prep_all(0)
    for ti in range(NT1):
        t0 = ti * TT
        h_t = hT[ti % 2]
        HK = [("hT", ti % 2, bi_, q_) for bi_ in range(4) for q_ in range(2)]
        nxt = ti + 1 if ti + 1 < NT1 else None
        dma("sp", ctab[64:80], tabc_d[ti * 16:(ti + 1) * 16, :], (), ["ctab"])
        dma("sp", ctab[80:96], tabc_d[ti * 16:(ti + 1) * 16, :], (), ["ctab2"])
        dma("sp", stab[64:80], tabsn_d[ti * 16:(ti + 1) * 16, :], (), ["stab"])
        dma("sp", stab[80:96], tabsp_d[ti * 16:(ti + 1) * 16, :], (), ["stab2"])

        def proj(c0, m):
            pb, pk = ps_next()
            for kc in range(8):
                mm(pb[0:m, :], win_b[:, kc, c0:c0 + m], h_t[:, kc, :], kc == 0, kc == 7, HK + WIN, [pk])
            return pb, pk

        for c in range(2):
            pb, pk = proj(c * 128, 128)
            act(qlat[:, c, :], pb, AF.Copy, [pk], [("qlat", c)])
            act(qsq[:, c, :], pb, AF.Square, [pk], [("qsq", c)])
        pb, pk = proj(256, 128)
        act(kvlat, pb, AF.Copy, [pk], ["kvlat"])
        act(kvsq, pb, AF.Square, [pk], ["kvsq"])
        pa, pka = proj(C_KRP, 96)
        pbb, pkb = proj(C_KRS, 96)
        tt("dve", rt1[0][0:96], pa[0:96, :], ctab[0:96], ALU.mult, [pka, "ctab", "ctab2", "ctab0"], [("rt1", 0)])
        tt("dve", rt2[0][0:96], pbb[0:96, :], stab[0:96], ALU.mult, [pkb, "stab", "stab2", "stab0"], [("rt2", 0)])
        tt("pool", krr[0:96], rt1[0][0:96], rt2[0][0:96], ALU.add, [("rt1", 0), ("rt2", 0)], ["krr"])
        pq, pkq = ps_next()
        for c in range(2):
            mm(pq, ones_f, qsq[:, c, :], c == 0, c == 1, ["ones_f", ("qsq", c)], [pkq])
        act(rbc[0], pq, AF.Sqrt, [pkq, "eps_c"], [("rbc", 0)], scale=1.0 / 256, bias=eps_c[:, 0:1])
        recip(rbc[0], rbc[0], [("rbc", 0)], [("rbc", 0)])
        for c in range(2):
            tt("dve", qln[:, c, :], qlat[:, c, :], rbc[0], ALU.mult, [("qlat", c), ("rbc", 0)], [("qln", c)])
        pkv, pkkv = ps_next()
        mm(pkv, ones_f, kvsq, True, True, ["ones_f", "kvsq"], [pkkv])
        act(rbc[1], pkv, AF.Sqrt, [pkkv, "eps_c"], [("rbc", 1)], scale=1.0 / 128, bias=eps_c[:, 0:1])
        recip(rbc[1], rbc[1], [("rbc", 1)], [("rbc", 1)])
        tt("dve", kvn, kvlat, rbc[1], ALU.mult, ["kvlat", ("rbc", 1)], ["kvn"])
        if nxt is not None:
            prep_stats(nxt, 0)
            prep_stats(nxt, 1)
        for i in range(4):
            pb, pk = proj(C_QB + i * 128, 128)
            evac_copy(qbT_s[:, i, :], pb, [pk], ["qbT_s"])
        dma("sp", qbT_v[:, :, t0:t0 + TT], qbT_s, ["qbT_s"], [("qbT_d", ti)])
        for h in range(8):
            pa, pka = ps_next()
            for c in range(2):
                mm(pa[0:96, :], wuq_b[:, c, h * 96:(h + 1) * 96], qln[:, c, :], c == 0, c == 1,
                   ["wuq_b", ("qln", c)], [pka])
            pbb, pkb = ps_next()
            for c in range(2):
                mm(pbb[0:96, :], wuqs_b[:, c, h * 96:(h + 1) * 96], qln[:, c, :], c == 0, c == 1,
                   ["wuqs_b", ("qln", c)], [pkb])
            i2 = h % 2
            tt("dve", rt1[i2][0:96], pa[0:96, :], ctab[0:96], ALU.mult, [pka, "ctab", "ctab2", "ctab0"], [("rt1", i2)])
            tt("dve", rt2[i2][0:96], pbb[0:96, :], stab[0:96], ALU.mult, [pkb, "stab", "stab2", "stab0"], [("rt2", i2)])
            tt("pool", qT_s[0:96, h, :], rt1[i2][0:96], rt2[i2][0:96], ALU.add, [("rt1", i2), ("rt2", i2)], ["qT_s"])
            pk_, pkk = ps_next()
            mm(pk_[0:96, :], wukp_b[:, h, :], kvn, True, False, ["wukp_b", "kvn"], [pkk])
            mm(pk_[0:96, :], sel_b[0:96, :], krr[0:96, :], False, True, ["sel_b", "krr"], [pkk])
            evac_copy(kT_s[0:96, h, :], pk_[0:96, :], [pkk], ["kT_s"])
        for bi in range(4):
            pv, pkv_ = ps_next()
            mm(pv, kvn[:, bi * 128:(bi + 1) * 128], wuv_b, True, True, ["kvn", "wuv_b"], [pkv_])
            evac_copy(va_s[:, bi, :].rearrange("p (h d) -> p h d", d=65)[:, :, 0:64],
                      pv.rearrange("p (h d) -> p h d", d=64), [pkv_], ["va_s"])
        dma("sp", qT_v[:, :, t0:t0 + TT], qT_s[0:96], ["qT_s"], [("qT_d", ti)])
        dma("sp", kT_v[:, :, t0:t0 + TT], kT_s[0:96], ["kT_s"], [("kT_d", ti)])
        dma("sp", va_d[t0:t0 + TT, :].rearrange("(b p) f -> p b f", p=128), va_s, ["va_s"], [("va_d", ti)])
        if nxt is not None:
            prep_tr(nxt, 0)
            prep_stats(nxt, 2)
        for i in range(4):
            pb, pk = proj(C_KB + i * 128, 128)
            evac_copy(kbT_s[:, i, :], pb, [pk], ["kbT_s"])
        dma("sp", kbT_v[:, :, t0:t0 + TT], kbT_s, ["kbT_s"], [("kbT_d", ti)])
        if nxt is not None:
            prep_tr(nxt, 1)
            prep_stats(nxt, 3)
        for bi in range(4):
            pv, pkv_ = ps_next()
            for kc in range(8):
                mm(pv, h_t[:, kc, bi * 128:(bi + 1) * 128], win_b[:, kc, C_VB:C_VB + 512], kc == 0, kc == 7,
                   HK + WIN, [pkv_])
            evac_copy(vb_s[:, bi, :].rearrange("p (h d) -> p h d", d=65)[:, :, 0:64],
                      pv.rearrange("p (h d) -> p h d", d=64), [pkv_], ["vb_s"])
        dma("sp", vb_d[t0:t0 + TT, :].rearrange("(b p) f -> p b f", p=128), vb_s, ["vb_s"], [("vb_d", ti)])
        if nxt is not None:
            prep_tr(nxt, 2)
        for gix, (cbase, gv, nm) in enumerate(((C_GA, ga_v, "ga"), (C_GB, gb_v, "gb"))):
            for c in range(8):
                pb, pk = proj(cbase + c * 128, 128)
                gi = gcnt[0] % 4
                gcnt[0] += 1
                act(gst[gi], pb, AF.Sigmoid, [pk], [("gst", gi)])
                dma("sp", gv[:, c, t0:t0 + TT], gst[gi], [("gst", gi)], [(nm, ti, c)])
            if gix == 0 and nxt is not None:
                prep_tr(nxt, 3)

    sc.barrier()
    if stage <= 2:
        return finish(nc, es, sc, None)

    A.release(pP)
    o_a = A.alloc([NB, 512], BF16)
    o_b = A.alloc([NB, 512], BF16)
    p2 = A.mark()
    kT_r = A.alloc([8, S], BF16)
    va_r = A.alloc([NB, 520], BF16)
    qT_t = [A.alloc([8, TT], BF16) for _ in range(2)]
    E_t = [A.alloc([TT], BF16) for _ in range(6)]
    rden = [A.alloc([4], F32) for _ in range(2)]
    for h in range(8):
        dma("sp", kT_r[0:96, h, :], kT_v[:, h, :], (), [("kT_r", h)])
    va_v = va_d.rearrange("(b p) f -> p b f", p=128)
    for q4 in range(4):
        dma("sp", va_r[:, q4 * 8:(q4 + 1) * 8, :], va_v[:, q4 * 8:(q4 + 1) * 8, :], (), [("va_r", q4)])
    SCALE_A = 96 ** -0.5
    sbank = [0]
    ecnt2 = [0]
    accn = [0]
    NQT = NT if stage > 3 else 2
    LOOK = 3
    stageA, stageB = [], []
    for qt in range(NQT):
        for h in range(8):
            nkt = 4 * qt + 4
            for kt in range(nkt):
                stageA.append((qt, h, kt))
    grp = {}

    def emitA(rec):
        qt, h, kt = rec
        qtt = qT_t[qt % 2]
        qk = ("qT_t", qt % 2)
        if h == 0 and kt == 0:
            dma("sp", qtt[0:96], qT_v[:, :, qt * TT:(qt + 1) * TT], (), [qk])
        r = kt - 4 * qt
        c0 = 128 * r if r > 0 else 0
        sb = sbank[0] % 5
        sbank[0] += 1
        ps_, psk = banks[sb], ("ps", sb)
        mm(ps_[:, c0:TT], kT_r[0:96, h, kt * 128:(kt + 1) * 128], qtt[0:96, h, c0:TT], True, True,
           [("kT_r", h), qk], [psk])
        ei = ecnt2[0] % len(E_t)
        ecnt2[0] += 1
        Et, Ek = E_t[ei], ("E", ei)
        act(Et[:, c0:TT], ps_[:, c0:TT], AF.Exp, [psk], [Ek], scale=SCALE_A)
        if r >= 0:
            memset("dve", Et[64:128, c0:c0 + 64], 0.0, [Ek])
        grp[rec] = (Et, Ek)

    def emitB(rec):
        qt, h, kt = rec
        nkt = 4 * qt + 4
        r = kt - 4 * qt
        if kt == 0:
            ab = 5 + accn[0] % 2
            accn[0] += 1
            grp["acc"] = (banks[ab], ("ps", ab))
        acc, acck = grp["acc"]
        Et, Ek = grp.pop(rec)
        for qb in range(max(r, 0), 4):
            first = (kt == 0) and (qb == 0)
            last = (kt == nkt - 1) and (qb == 3)
            mm(acc[:, qb * 65:(qb + 1) * 65], Et[:, qb * 128:(qb + 1) * 128],
               va_r[:, kt, h * 65:(h + 1) * 65], first, last, [Ek, ("va_r", kt // 8)], [acck])
        if kt == nkt - 1:
            rd = rden[h % 2]
            rk = ("rden", h % 2)
            accv = acc[:, 0:260].rearrange("p (b d) -> p b d", d=65)
            recip(rd, accv[:, :, 64], [acck], [rk])
            tt("dve", o_a[:, qt * 4:(qt + 1) * 4, h * 64:(h + 1) * 64], accv[:, :, 0:64],
               rd[:, :, None].broadcast_to([128, 4, 64]), ALU.mult, [acck, rk], [("o_a", qt)])

    for i in range(len(stageA) + LOOK):
        if i % 8 == 3:
            precast_step()
        if i < len(stageA):
            emitA(stageA[i])
        if i >= LOOK:
            emitB(stageA[i - LOOK])
    if dbg:
        oa_dbg = dscr("oa_dbg", [S, 512], BF16)
        dma("sp", oa_dbg.rearrange("(b p) f -> p b f", p=128), o_a, [("o_a", q) for q in range(NQT)], ["oa_dbg"])
    sc.barrier()
    if stage <= 4:
        return finish(nc, es, sc, None)

    A.release(p2)
    qb_p = [A.alloc([S], BF16) for _ in range(2)]
    kb_p = [A.alloc([S], BF16) for _ in range(2)]
    vb_r = A.alloc([NB, 520], BF16)
    bias_r = A.alloc([8, 640], F32)
    Eb = [A.alloc([640], BF16) for _ in range(10)]
    stmp = [A.alloc([640], F32) for _ in range(4)]
    rden3 = [A.alloc([4], F32) for _ in range(2)]
    vb_v = vb_d.rearrange("(b p) f -> p b f", p=128)
    for q4 in range(4):
        dma("sp", vb_r[:, q4 * 8:(q4 + 1) * 8, :], vb_v[:, q4 * 8:(q4 + 1) * 8, :], (), [("vb_r", q4)])
    dma("sp", bias_r, biasT_d.rearrange("p (h q) -> p h q", h=8), (), ["bias_r"])
    for h_ in range(8):
        act(bias_r[:, h_, :], bias_r[:, h_, :], AF.Exp, ["bias_r"], ["bias_r"])
    sbank[0] = 0
    ecnt3 = 0
    stc = 0
    NJ = NB if stage > 5 else 8
    recs3 = [(h, j) for h in range(8) for j in range(NJ)]
    ering = {}
    st3 = dict(ecnt=0, stc=0, bs=0)

    def emitA3(rec):
        h, j = rec
        pr, po = h // 2, (h % 2) * 64
        qb_r, kb_r = qb_p[pr % 2], kb_p[pr % 2]
        if h % 2 == 0 and j == 0:
            dma("sp", qb_r, qbT_v[:, pr, :], (), [("qb_r", pr % 2)])
            dma("sp", kb_r, kbT_v[:, pr, :], (), [("kb_r", pr % 2)])
        nq = min(640, S - 128 * j)
        n1 = min(nq, 512)
        sb = sbank[0] % 4
        sbank[0] += 1
        psA, pkA = banks[sb], ("ps", sb)
        st_ = stmp[st3["stc"] % 4]
        stk = ("stmp", st3["stc"] % 4)
        st3["stc"] += 1
        mm(psA[:, 0:n1], kb_r[po:po + 64, 128 * j:128 * j + 128], qb_r[po:po + 64, 128 * j:128 * j + n1],
           True, True, [("kb_r", pr % 2), ("qb_r", pr % 2)], [pkA])
        act(st_[:, 0:n1], psA[:, 0:n1], AF.Exp, [pkA], [(stk, 0)], scale=0.125)
        if nq > 512:
            bslot = (4, 7)[st3["bs"] % 2]
            st3["bs"] += 1
            psB, pkB = banks[bslot], ("ps", bslot)
            mm(psB[:, 0:nq - 512], kb_r[po:po + 64, 128 * j:128 * j + 128],
               qb_r[po:po + 64, 128 * j + 512:128 * j + nq], True, True, [("kb_r", pr % 2), ("qb_r", pr % 2)], [pkB])
            act(st_[:, 512:nq], psB[:, 0:nq - 512], AF.Exp, [pkB], [(stk, 1)], scale=0.125)
        ei = st3["ecnt"] % len(Eb)
        st3["ecnt"] += 1
        Et, Ek = Eb[ei], ("Eb", ei)
        tt("dve" if st3["ecnt"] % 2 == 0 else "pool", Et[:, 0:nq], st_[:, 0:nq], bias_r[:, h, 0:nq], ALU.mult,
           [(stk, 0), (stk, 1), "bias_r"], [Ek])
        ering[(h, j)] = (Et, Ek)

    def emitB3(rec):
        h, j = rec
        if j % 4 == 0:
            ab = 5 + accn[0] % 2
            accn[0] += 1
            ering["acc"] = (banks[ab], ("ps", ab))
        acc, acck = ering["acc"]
        jj0 = max(0, j - 4)
        for jj in range(jj0, j + 1):
            Ej, Ejk = ering[(h, jj)]
            off = (j - jj) * 128
            mm(acc[:, (j % 4) * 65:(j % 4 + 1) * 65], Ej[:, off:off + 128], vb_r[:, jj, h * 65:(h + 1) * 65],
               (j % 4 == 0) and (jj == jj0), (j % 4 == 3) and (jj == j), [Ejk, ("vb_r", jj // 8)], [acck])
        if j % 4 == 3:
            rd = rden3[(j // 4) % 2]
            rk = ("rden3", (j // 4) % 2)
            accv = acc[:, 0:260].rearrange("p (b d) -> p b d", d=65)
            recip(rd, accv[:, :, 64], [acck], [rk])
            tt("dve", o_b[:, j - 3:j + 1, h * 64:(h + 1) * 64], accv[:, :, 0:64],
               rd[:, :, None].broadcast_to([128, 4, 64]), ALU.mult, [acck, rk], [("o_b", j // 4)])

    LOOK3 = 3
    for i in range(len(recs3) + LOOK3):
        if i % 5 == 2:
            precast_step()
        if i < len(recs3):
            emitA3(recs3[i])
        if i >= LOOK3:
            emitB3(recs3[i - LOOK3])
    if dbg:
        ob_dbg = dscr("ob_dbg", [S, 512], BF16)
        dma("sp", ob_dbg.rearrange("(b p) f -> p b f", p=128), o_b, [("o_b", q) for q in range(NJ // 4)], ["ob_dbg"])
    sc.barrier()
    if stage <= 6:
        return finish(nc, es, sc, None)

    A.release(p2)
    cw_all = A.alloc([NB, 2], F32)
    e_all = A.alloc([NB, 2], F32)
    r_all = A.alloc([NB, 2], F32)
    A1all = A.alloc([NB, 32], F32)
    A2all = A.alloc([NB, 32], F32)
    carry = A.alloc([32], F32)
    iota_e = A.alloc([32], F32)
    rbias = A.alloc([36], F32)
    Lst = A.alloc([128], BF16)
    p4 = A.mark()
    woa_b = A.alloc([4, D], BF16)
    wob_b = A.alloc([4, D], BF16)
    wout_b = A.alloc([8, D], BF16)
    wr_f = A.alloc([8, 36], F32)
    for c in range(4):
        dma("pool", woa_b[:, c, :], woa_d.rearrange("p (k n) -> p k n", k=4)[:, c, :], (), [("woa", c)])
        dma("pool", wob_b[:, c, :], wob_d.rearrange("p (k n) -> p k n", k=4)[:, c, :], (), [("wob", c)])
    WOA = [("woa", c) for c in range(4)]
    WOB = [("wob", c) for c in range(4)]
    WOUT = [("wout", c) for c in range(8)]
    dma("sp", wr_f, wr_d.rearrange("p (k n) -> p k n", k=8), (), ["wr_f"])
    dma("sp", rbias, rb_d[0:1, :].to_broadcast([128, 36]), (), ["rbias"])
    dma("sp", iota_e, iota_d, (), ["iota_e"])
    ltmp = A.alloc([128], F32)
    dma("sp", ltmp, lst_d, (), ["ltmp"])
    tcopy("dve", Lst, ltmp, ["ltmp"], ["Lst"])
    memset("dve", carry, 0.0, ["carry"])

    oaT2 = [A.alloc([4, TT], BF16) for _ in range(2)]
    obT2 = [A.alloc([4, TT], BF16) for _ in range(2)]
    mT = A.alloc([8, TT], BF16)
    gat = [A.alloc([TT], F32) for _ in range(2)]
    gbt = [A.alloc([TT], F32) for _ in range(2)]
    mt1 = [A.alloc([TT], F32) for _ in range(2)]
    mt2 = [A.alloc([TT], F32) for _ in range(2)]
    R4 = 2
    RX = 3
    xr = [A.alloc([D], F32) for _ in range(RX)]
    uu = [A.alloc([D], F32) for _ in range(R4)]
    h2b = [A.alloc([D], BF16) for _ in range(2)]
    h2T = [A.alloc([8, 128], F32) for _ in range(2)]
    rs_t = A.alloc([2, 4], F32)
    wr_s = A.alloc([8, 36], F32)
    s2_fm8 = A.alloc([8], F32)
    b2_fm8 = A.alloc([8], F32)
    dtmp4 = A.alloc([128], F32)
    sm = A.alloc([16], F32)
    lg = A.alloc([4, 36], F32)
    dg = A.alloc([4, 4], F32)
    ge = A.alloc([4, 4], F32)
    pen = A.alloc([4, 4], F32)
    msk = A.alloc([4, 32], F32)
    top8a = A.alloc([4, 8], F32)
    idx8a = A.alloc([4, 8], U32)
    r4s = A.alloc([8, 4], F32)
    Ab = A.alloc([4, 32], BF16)
    Pt = A.alloc([4, 32], F32)
    ptm = A.alloc([4, 32], F32)
    woutv = wout_d.rearrange("p (k n) -> p k n", k=8)
    for c in range(8):
        stb = xr[c % RX]
        stk_ = [("x1", c % RX, 0), ("x1", c % RX, 1)]
        dma("sp", stb, woutv[:, c, :], (), stk_)
        tt("dve", wout_b[:, c, :], stb, gt1_bc, ALU.mult, stk_, [("wout", c)])
    for (row, rk_, dst_fm, dk) in ((s2_bc, [], s2_fm8, "s2_fm8"), (b2_bc, [], b2_fm8, "b2_fm8")):
        for kc in range(8):
            tt("dve", dtmp4, row[:, kc * 128:(kc + 1) * 128], ident_f, ALU.mult, ["ident_f"], ["dtmp4"])
            sc.add("dve", lambda e, dst_fm=dst_fm, kc=kc: e.tensor_reduce(out=dst_fm[:, kc:kc + 1], in_=dtmp4, axis=AX.X,
                                                                         op=ALU.add), ["dtmp4"], [dk])
    for kc in range(8):
        ts("dve", wr_s[:, kc, :], wr_f[:, kc, :], s2_fm8[:, kc:kc + 1], None, ALU.mult, None, ["wr_f", "s2_fm8"], ["wr_s"])
    b2rep = uu[0].rearrange("p (k m) -> p k m", k=8)
    tcopy("dve", b2rep, b2_fm8[:, :, None].broadcast_to([128, 8, 128]), ["b2_fm8"], [("uu", 0)])
    for kc in range(8):
        mm(banks[7][:, 0:36], b2rep[:, kc, :], wr_f[:, kc, :], kc == 0, kc == 7, [("uu", 0), "wr_f"], [("ps", 7)])
    tt("dve", rbias, banks[7][:, 0:36], rbias, ALU.add, [("ps", 7), "rbias"], ["rbias"])
    RB = 6
    rot = [0]

    def ps_rot():
        i = rot[0] % RB
        rot[0] += 1
        return banks[i], ("ps", i)

    pp, pkp = banks[6], ("ps", 6)
    rl, pkr = banks[7], ("ps", 7)
    NT4 = NT if stage > 7 else 1

    def S1(ti, bi):
        blk = ti * 4 + bi
        t0 = ti * TT
        r3 = blk % R4
        rx = blk % RX
        g2 = blk % 2
        tp = ti % 2
        xk = [("x1", rx, 0), ("x1", rx, 1)]
        rsk = ("rs_t", tp, bi)
        dma("sp", xr[rx], x_d[t0 + bi * 128:t0 + (bi + 1) * 128, :], (), xk)
        for half in range(2):
            pm, pkm = ps_rot()
            for m in range(8):
                mm(pm, mT[:, m, bi * 128:(bi + 1) * 128], wout_b[:, m, half * 512:(half + 1) * 512],
                   m == 0, m == 7, [("mT", m_) for m_ in range(8)] + WOUT, [pkm])
            hs = slice(half * 512, (half + 1) * 512)
            tt("dve", xr[rx][:, hs], pm, xr[rx][:, hs], ALU.add, [pkm, ("x1", rx, half)], [("x1", rx, half)])
        dma("sp", x1_d[t0 + bi * 128:t0 + (bi + 1) * 128, :], xr[rx], xk, [("x1_d", blk)])

    def S1b(ti, bi):
        blk = ti * 4 + bi
        t0 = ti * TT
        r3 = blk % R4
        rx = blk % RX
        g2 = blk % 2
        tp = ti % 2
        xk = [("x1", rx, 0), ("x1", rx, 1)]
        rsk = ("rs_t", tp, bi)
        act(h2b[g2], xr[rx], AF.Square, xk, [("ssq4", g2), ("h2b", g2)], accum_out=sm[:, g2:g2 + 1])
        act(sm[:, 2 + g2:3 + g2], sm[:, g2:g2 + 1], AF.Ln, [("ssq4", g2), "eps_c"], [("rs4", g2)],
            scale=1.0 / D, bias=eps_c[:, 0:1])
        act(rs_t[:, tp, bi:bi + 1], sm[:, 2 + g2:3 + g2], AF.Exp, [("rs4", g2)], [rsk], scale=-0.5)
        tt("pool", uu[r3], xr[rx], s2_bc, ALU.mult, xk, [("uu", r3)])
        stt(uu[r3], uu[r3], rs_t[:, tp, bi:bi + 1], b2_bc, ALU.mult, ALU.add, [("uu", r3), rsk], [("uu", r3)])
        tcopy("act", h2b[g2], uu[r3], [("uu", r3), ("ssq4", g2)], [("h2b", g2)])
        dma("sp", h2_d[t0 + bi * 128:t0 + (bi + 1) * 128, :], h2b[g2], [("h2b", g2)], [("h2_d", blk)])

    def S2a(ti, bi):
        blk = ti * 4 + bi
        rx = blk % RX
        hb = blk % 2
        xk = [("x1", rx, 0), ("x1", rx, 1)]
        for half in range(2):
            pb, pk = ps_rot()
            for q in range(4):
                kc = half * 4 + q
                tr(pb[:, q * 128:(q + 1) * 128], xr[rx][:, kc * 128:(kc + 1) * 128], ident_f, xk, [pk])
            evac_copy(h2T[hb][:, half * 4:(half + 1) * 4, :], pb.rearrange("p (q t) -> p q t", q=4), [pk],
                      [("h2T", hb, half)])
        for kc in range(8):
            mm(rl[:, bi * 36:(bi + 1) * 36], h2T[hb][:, kc, :], wr_s[:, kc, :], (bi == 0) and (kc == 0),
               (bi == 3) and (kc == 7), [("h2T", hb, kc // 4), "wr_s"], [pkr])

    def dve(fn, reads, writes):
        sc.add("dve", fn, reads, writes)

    def S2b1(ti):
        b0 = ti * 4
        bs = slice(b0, b0 + 4)
        tt("dve", lg, rl[:, 0:144].rearrange("p (b n) -> p b n", n=36),
           rs_t[:, ti % 2, :, None].broadcast_to([128, 4, 36]), ALU.mult,
           [pkr] + [("rs_t", ti % 2, b_) for b_ in range(4)], ["lg"])
        tt("dve", lg, lg, rbias[:, None, :].broadcast_to([128, 4, 36]), ALU.add, ["lg", "rbias"], ["lg"])
        gmax = r4s[:, 0, :]
        dve(lambda e: e.tensor_reduce(out=gmax, in_=lg[:, :, 0:4], axis=AX.X, op=ALU.max), ["lg"], ["gmax"])
        tt("dve", dg, lg[:, :, 0:4], gmax[:, :, None].broadcast_to([128, 4, 4]), ALU.subtract, ["lg", "gmax"], ["dg"])
        act(ge, dg, AF.Exp, ["dg"], ["ge"])
        gsum = r4s[:, 1, :]
        dve(lambda e: e.tensor_reduce(out=gsum, in_=ge, axis=AX.X, op=ALU.add), ["ge"], ["gsum"])
        gw = r4s[:, 2, :]
        recip(gw, gsum, ["gsum"], ["gw"])
        ts("dve", pen, dg, 0.0, None, ALU.is_equal, None, ["dg"], ["pen"])
        ts("dve", pen, pen, -1.0, 1e30, ALU.add, ALU.mult, ["pen"], ["pen"])
        tt("dve", msk.rearrange("p b (g e) -> p b g e", e=8), lg[:, :, 4:36].rearrange("p b (g e) -> p b g e", e=8),
           pen[:, :, :, None].broadcast_to([128, 4, 4, 8]), ALU.add, ["lg", "pen"], ["msk"])
        for b in range(4):
            dve(lambda e, b=b: e.max(out=top8a[:, b, :], in_=msk[:, b, :]), ["msk"], [("top8", b)])
            dve(lambda e, b=b: e.max_index(out=idx8a[:, b, :], in_max=top8a[:, b, :], in_values=msk[:, b, :]),
                ["msk", ("top8", b)], [("idx8", b)])
        T8 = [("top8", b) for b in range(4)]
        I8 = [("idx8", b) for b in range(4)]
        tcopy("dve", e_all[:, bs, :], idx8a[:, :, 0:2], I8, [("e_all", ti)])
        dlt = r4s[:, 3, :]
        tt("dve", dlt, top8a[:, :, 1], top8a[:, :, 0], ALU.subtract, T8, ["dlt"])
        ex_ = r4s[:, 4, :]
        act(ex_, dlt, AF.Exp, ["dlt"], ["ex_"])
        den = r4s[:, 5, :]
        ts("dve", den, ex_, 1.0, None, ALU.add, None, ["ex_"], ["den"])
        recip(den, den, ["den"], ["den"])
        tt("dve", cw_all[:, bs, 0], den, gw, ALU.mult, ["den", "gw"], [("cw", ti, 0)])
        tt("dve", ex_, ex_, den, ALU.mult, ["ex_", "den"], ["ex_"])
        tt("dve", cw_all[:, bs, 1], ex_, gw, ALU.mult, ["ex_", "gw"], [("cw", ti, 1)])
        iob = iota_e[:, None, :].broadcast_to([128, 4, 32])
        tt("dve", A1all[:, bs, :], iob, e_all[:, bs, 0:1].broadcast_to([128, 4, 32]), ALU.is_equal,
           ["iota_e", ("e_all", ti)], [("A1", ti)])
        tt("dve", A2all[:, bs, :], iob, e_all[:, bs, 1:2].broadcast_to([128, 4, 32]), ALU.is_equal,
           ["iota_e", ("e_all", ti)], [("A2", ti)])
        tt("dve", Ab, A1all[:, bs, :], A2all[:, bs, :], ALU.add, [("A1", ti), ("A2", ti)], ["Ab"])

    def S2b2(ti):
        b0 = ti * 4
        bs = slice(b0, b0 + 4)
        n_mm = 0
        tot_mm = 4 + 6 + 4
        for b in range(4):
            mm(pp[:, b * 32:(b + 1) * 32], Lst, Ab[:, b, :], n_mm == 0, False, ["Lst", "Ab"], [pkp])
            n_mm += 1
            for b_ in range(b):
                mm(pp[:, b * 32:(b + 1) * 32], ones_b, Ab[:, b_, :], False, False, ["ones_b", "Ab"], [pkp])
                n_mm += 1
        for b in range(4):
            n_mm += 1
            mm(pp[:, 128:160], ones_b, Ab[:, b, :], False, n_mm == tot_mm, ["ones_b", "Ab"], [pkp])
        tt("dve", Pt, pp[:, 0:128].rearrange("p (b n) -> p b n", n=32), carry[:, None, :].broadcast_to([128, 4, 32]),
           ALU.add, [pkp, "carry"], ["Pt"])
        tt("dve", carry, pp[:, 128:160], carry, ALU.add, [pkp, "carry", "Pt"], ["carry"])
        tt("dve", ptm, Pt, A1all[:, bs, :], ALU.mult, ["Pt", ("A1", ti)], ["ptm"])
        dve(lambda e: e.tensor_reduce(out=r_all[:, bs, 0], in_=ptm, axis=AX.X, op=ALU.add), ["ptm"], [("r_all", ti, 0)])
        tt("dve", ptm, Pt, A2all[:, bs, :], ALU.mult, ["Pt", ("A2", ti), ("r_all", ti, 0)], ["ptm"])
        dve(lambda e: e.tensor_reduce(out=r_all[:, bs, 1], in_=ptm, axis=AX.X, op=ALU.add), ["ptm"], [("r_all", ti, 1)])

    gcn4 = [0]

    def otrans(ti):
        ob2 = ti % 2
        for (osrc, odst, onm) in ((o_a, oaT2[ob2], "oaT"), (o_b, obT2[ob2], "obT")):
            for c in range(4):
                pb, pk = ps_rot()
                pbb = pb.bitcast(BF16)
                for bi in range(4):
                    tr(pbb[:, bi * 128:(bi + 1) * 128], osrc[:, ti * 4 + bi, c * 128:(c + 1) * 128], ident_b,
                       [("o_x",), "ident_b"], [pk])
                evac_copy(odst[:, c, :], pbb[:, 0:TT], [pk], [(onm, ob2, c)])

    def projmerge(ti):
        t0 = ti * TT
        ob2 = ti % 2
        oaT, obT = oaT2[ob2], obT2[ob2]
        for m in range(8):
            gi = gcn4[0] % 2
            gcn4[0] += 1
            dma("sp", gat[gi], ga_v[:, m, t0:t0 + TT], (), [("gat", gi)])
            dma("sp", gbt[gi], gb_v[:, m, t0:t0 + TT], (), [("gbt", gi)])
            pa, pka = ps_rot()
            for c in range(4):
                mm(pa, woa_b[:, c, m * 128:(m + 1) * 128], oaT[:, c, :], c == 0, c == 3, WOA + [("oaT", ob2, c)], [pka])
            pbk, pkb = ps_rot()
            for c in range(4):
                mm(pbk, wob_b[:, c, m * 128:(m + 1) * 128], obT[:, c, :], c == 0, c == 3, WOB + [("obT", ob2, c)], [pkb])
            i2 = m % 2
            tt("dve", mt1[i2], pa, gat[gi], ALU.mult, [pka, ("gat", gi)], [("mt1", i2)])
            tt("dve", mt2[i2], pbk, gbt[gi], ALU.mult, [pkb, ("gbt", gi)], [("mt2", i2)])
            tt("pool", mT[:, m, :], mt1[i2], mt2[i2], ALU.add, [("mt1", i2), ("mt2", i2)], [("mT", m)])

    otrans(0)
    projmerge(0)
    if NT4 > 1:
        otrans(1)
    for ti in range(NT4):
        S1(ti, 0)
        S1(ti, 1)
        S2a(ti, 0)
        S1b(ti, 0)
        S1(ti, 2)
        S2a(ti, 1)
        S1b(ti, 1)
        S1(ti, 3)
        S2a(ti, 2)
        S1b(ti, 2)
        if ti >= 1:
            S2b2(ti - 1)
        if ti + 1 < NT4:
            projmerge(ti + 1)
        S2a(ti, 3)
        S1b(ti, 3)
        if ti + 2 < NT4:
            otrans(ti + 2)
        S2b1(ti)
    S2b2(NT4 - 1)
    if dbg:
        rt_dbg = dscr("rt_dbg", [128, NB * 6], F32)
        rv = rt_dbg.rearrange("p (b s) -> p b s", s=6)
        ALLK = [("cw", t_, k_) for t_ in range(NT4) for k_ in range(2)] + [("e_all", t_) for t_ in range(NT4)] + [("r_all", t_, k_) for t_ in range(NT4) for k_ in range(2)]
        dma("sp", rv[:, :, 0:2], cw_all, ALLK, ["rt1"])
        dma("sp", rv[:, :, 2:4], e_all, ALLK, ["rt2"])
        dma("sp", rv[:, :, 4:6], r_all, ALLK, ["rt3"])
    sc.barrier()
    if stage <= 8:
        return finish(nc, es, sc, None)

    A.release(p4)
    padf = A.alloc([32], F32)
    padi = A.alloc([32], I32)
    incl = A.alloc([32], F32)
    offs = A.alloc([32], F32)
    ones32 = A.alloc([32], F32)
    big3 = A.alloc([NTILE, 32], F32)
    slotf = A.alloc([NB, 2], F32)
    slot_i = A.alloc([NB * 2], I32)
    jv = A.alloc([NTILE], F32)
    pidx = A.alloc([1], F32)
    tef = A.alloc([NTILE], F32)
    tesh = A.alloc([NTILE], F32)
    widx_i = A.alloc([NTILE], I32)
    p5 = A.mark()
    dma("sp", jv, jv_d, (), ["jv"])
    dma("sp", pidx, pidx_d, (), ["pidx"])
    memset("dve", ones32, 1.0, ["ones32"])
    ts("dve", padf, carry, float(SLOT_T - 1), None, ALU.add, None, ["carry"], ["padf"])
    tcopy("dve", padi, padf, ["padf"], ["padi"])
    ts("dve", padi, padi, SH, None, ALU.arith_shift_right, None, ["padi"], ["padi"])
    ts("dve", padi, padi, SH, None, ALU.logical_shift_left, None, ["padi"], ["padi"])
    tcopy("dve", padf, padi, ["padi"], ["padf"])
    sc.add("dve", lambda e: e.tensor_tensor_scan(out=incl, data0=ones32, data1=padf, initial=0.0,
                                                  op0=ALU.mult, op1=ALU.add), ["ones32", "padf"], ["incl"])
    tt("dve", offs, incl, padf, ALU.subtract, ["incl", "padf"], ["offs"])
    for k, Aall in ((0, A1all), (1, A2all)):
        tt("dve", big3[:, 0:NB, :], Aall, offs[:, None, :].broadcast_to([128, NB, 32]), ALU.mult, ["offs"], ["big3"])
        sc.add("dve", lambda e, k=k: e.tensor_reduce(out=slotf[:, :, k], in_=big3[:, 0:NB, :], axis=AX.X, op=ALU.add),
               ["big3"], [("slotf", k)])
    tt("dve", slotf, slotf, r_all, ALU.add, [("slotf", 0), ("slotf", 1)], ["slotf"])
    tcopy("dve", slot_i, slotf.rearrange("p b k -> p (b k)"), ["slotf"], ["slot_i"])
    tt("dve", big3, incl[:, None, :].broadcast_to([128, NTILE, 32]), jv[:, :, None].broadcast_to([128, NTILE, 32]),
       ALU.is_le, ["incl", "jv", ("slotf", 0), ("slotf", 1)], ["big3"])
    sc.add("dve", lambda e: e.tensor_reduce(out=tef, in_=big3, axis=AX.X, op=ALU.add), ["big3"], ["tef"])
    ts("dve", tef, tef, 31.0, None, ALU.min, None, ["tef"], ["tef"])
    memset("dve", tesh, -1.0, ["tesh"])
    tcopy("dve", tesh[:, 3:NTILE], tef[:, 0:NTILE - 3], ["tef", "tesh"], ["tesh"])
    tt("dve", tesh, tesh, tef, ALU.is_equal, ["tesh", "tef"], ["tesh"])
    ts("dve", tef, tef, 128.0, pidx[:, 0:1], ALU.mult, ALU.add, ["tef", "pidx"], ["tef"])
    stt(tef, tesh, 100000.0, tef, ALU.mult, ALU.add, ["tesh", "tef"], ["tef"])
    tcopy("dve", widx_i, tef, ["tef"], ["widx_i"])
    if dbg:
        sl_dbg = dscr("sl_dbg", [128, NB * 2 + NTILE], I32)
        dma("sp", sl_dbg[:, 0:NB * 2], slot_i, ["slot_i"], ["sl1"])
        dma("sp", sl_dbg[:, NB * 2:], widx_i, ["widx_i"], ["sl2"])
    hrow = [A.alloc([D], BF16) for _ in range(3)]
    for blk in range(NB):
        hr = hrow[blk % 3]
        dma("sp", hr, h2_d[blk * 128:(blk + 1) * 128, :], (), [("hrow", blk % 3)])
        for k in range(2):
            off_ap = slot_i[:, blk * 2 + k:blk * 2 + k + 1]
            sc.add("pool", lambda e, hr=hr, off_ap=off_ap: e.indirect_dma_start(
                out=xs_d[:, :], out_offset=bass.IndirectOffsetOnAxis(ap=off_ap, axis=0), in_=hr, in_offset=None),
                [("hrow", blk % 3), "slot_i"], [("xs_d", blk, k)], dma=True)
    sc.barrier()
    if stage <= 9:
        return finish(nc, es, sc, None)

    A.release(p5)
    while pre_pos[0] < len(pre_steps):
        precast_step()
    NW = 3
    wall_t = [A.alloc([6144], BF16) for _ in range(NW)]
    wg_t = [w[:, 0:2048] for w in wall_t]
    wu_t = [w[:, 2048:4096] for w in wall_t]
    wd_t = [w[:, 4096:6144] for w in wall_t]
    xs_t = [A.alloc([2, D], BF16) for _ in range(NW)]
    xsT = [A.alloc([8, SLOT_T], BF16) for _ in range(2)]
    sil = [A.alloc([2, SLOT_T], F32) for _ in range(2)]
    hidT = [A.alloc([2, SLOT_T], BF16) for _ in range(2)]
    yt = [A.alloc([D], F32) for _ in range(3)]
    ycn = 0
    breg = {}
    NTL = NTILE if stage > 10 else 4
    WXALL = [("wx", mi, e_) for mi in range(3) for e_ in range(32)]

    def prefetch6(j):
        b3 = j % NW
        wi = widx_i[:, j:j + 1]

        def wgather(e, wi=wi, b3=b3):
            if "r" not in breg:
                breg["r"] = e.alloc_register("wbound")
                e.reg_mov(breg["r"], 32 * 128 - 1)
            return e.indirect_dma_start(
                out=wall_t[b3], out_offset=None, in_=wx_d[:, :], in_offset=bass.IndirectOffsetOnAxis(ap=wi, axis=0),
                bounds_check=breg["r"], oob_is_err=False)
        sc.add("pool", wgather, (), [("wg", b3), ("wu", b3), ("wd", b3)], dma=True)
        dma("sp", xs_t[b3], xs_d[j * SLOT_T:(j + 1) * SLOT_T, :].rearrange("(s p) f -> p s f", p=128), (), [("xs_t", b3)])

    for j in range(min(2, NTL)):
        prefetch6(j)
    for j in range(NTL):
        if j + 2 < NTL:
            prefetch6(j + 2)
        b2 = j % 2
        b3 = j % NW
        for sbk in range(2):
            for half in range(2):
                pb, pk = ps_next()
                pbb = pb.bitcast(BF16)
                for q in range(4):
                    kc = half * 4 + q
                    tr(pbb[:, q * 128:(q + 1) * 128], xs_t[b3][:, sbk, kc * 128:(kc + 1) * 128], ident_b,
                       [("xs_t", b3), "ident_b"], [pk])
                evac_copy(xsT[b2][:, half * 4:(half + 1) * 4, sbk * 128:(sbk + 1) * 128],
                          pbb[:, 0:512].rearrange("p (q t) -> p q t", q=4), [pk], [("xsT", b2, sbk, half)])
        XST = [("xsT", b2, a, b) for a in range(2) for b in range(2)]
        pg, pkg = ps_next()
        for f in range(2):
            for kc in range(8):
                mm(pg[:, f * SLOT_T:(f + 1) * SLOT_T], wg_t[b3][:, kc * 256 + f * 128:kc * 256 + (f + 1) * 128],
                   xsT[b2][:, kc, :], kc == 0, kc == 7, [("wg", b3)] + XST, [pkg])
        pu, pku = ps_next()
        for f in range(2):
            for kc in range(8):
                mm(pu[:, f * SLOT_T:(f + 1) * SLOT_T], wu_t[b3][:, kc * 256 + f * 128:kc * 256 + (f + 1) * 128],
                   xsT[b2][:, kc, :], kc == 0, kc == 7, [("wu", b3)] + XST, [pku])
        act(sil[b2].rearrange("p f s -> p (f s)"), pg, AF.Silu, [pkg], [("sil", b2)])
        tt("dve", hidT[b2].rearrange("p f s -> p (f s)"), pu, sil[b2].rearrange("p f s -> p (f s)"), ALU.mult,
           [pku, ("sil", b2)], [("hidT", b2)])
        for sbk in range(2):
            y_ = yt[ycn % 3]
            yk = ("yt", ycn % 3)
            ycn += 1
            for half in range(2):
                py, pky = ps_next()
                for f in range(2):
                    mm(py, hidT[b2][:, f, sbk * 128:(sbk + 1) * 128], wd_t[b3][:, f * 1024 + half * 512:f * 1024 + (half + 1) * 512],
                       f == 0, f == 1, [("hidT", b2), ("wd", b3)], [pky])
                tt("dve", y_[:, half * 512:(half + 1) * 512], py, gt2_bc[:, half * 512:(half + 1) * 512], ALU.mult,
                   [pky], [(yk, half)])
            dma("sp", ys_d[j * SLOT_T + sbk * 128:j * SLOT_T + (sbk + 1) * 128, :], y_, [(yk, 0), (yk, 1)], [("ys_d", j, sbk)])
    sc.barrier()
    if stage <= 11:
        return finish(nc, es, sc, None)

    A.release(p5)
    gfin = A.alloc([D], F32)
    dma("sp", gfin, gfin_d[0:1, :].to_broadcast([128, D]), (), ["gfin"])
    R7 = 4
    g1 = [A.alloc([D], F32) for _ in range(R7)]
    g2_ = [A.alloc([D], F32) for _ in range(R7)]
    x1r = [A.alloc([D], F32) for _ in range(R7)]
    ot = [A.alloc([D], F32) for _ in range(2)]
    junk7 = A.alloc([D], BF16)
    sm7 = A.alloc([8], F32)

    def load7(blk):
        i4 = blk % R7
        for k, gt_ in ((0, g1), (1, g2_)):
            off_ap = slot_i[:, blk * 2 + k:blk * 2 + k + 1]
            sc.add("pool", lambda e, gt_=gt_, off_ap=off_ap, i4=i4: e.indirect_dma_start(
                out=gt_[i4], out_offset=None, in_=ys_d[:, :], in_offset=bass.IndirectOffsetOnAxis(ap=off_ap, axis=0)),
                (), [("g", k, i4)], dma=True)
        dma("sp", x1r[i4], x1_d[blk * 128:(blk + 1) * 128, :], (), [("x1r", i4)])

    for blk in range(2):
        load7(blk)
    for blk in range(NB):
        if blk + 2 < NB:
            load7(blk + 2)
        i4 = blk % R7
        i2 = blk % 2
        stt(x1r[i4], g1[i4], cw_all[:, blk, 0:1], x1r[i4], ALU.mult, ALU.add, [("g", 0, i4), ("x1r", i4)], [("x1r", i4)])
        stt(x1r[i4], g2_[i4], cw_all[:, blk, 1:2], x1r[i4], ALU.mult, ALU.add, [("g", 1, i4), ("x1r", i4)], [("x1r", i4)])
        act(junk7, x1r[i4], AF.Square, [("x1r", i4)], [("ss7", i2)], accum_out=sm7[:, i2:i2 + 1])
        act(sm7[:, 2 + i2:3 + i2], sm7[:, i2:i2 + 1], AF.Sqrt, [("ss7", i2)], [("rs7", i2)], scale=1.0 / D, bias=eps_c[:, 0:1])
        recip(sm7[:, 2 + i2:3 + i2], sm7[:, 2 + i2:3 + i2], [("rs7", i2)], [("rs7", i2)])
        stt(ot[i2], x1r[i4], sm7[:, 2 + i2:3 + i2], gfin, ALU.mult, ALU.mult, [("x1r", i4), ("rs7", i2), "gfin"], [("ot", i2)])
        dma("sp", out_d[blk * 128:(blk + 1) * 128, :], ot[i2], [("ot", i2)], [("out", blk)])
    sc.barrier()
    return finish(nc, es, sc, None)


def finish(nc, es, sc, _):
    block = es.enter_context(nc.Block())
    sc.emit(block)
    es.close()
    return nc


def fm(v, k):
    return np.ascontiguousarray(np.asarray(v, np.float32).reshape(k, 128).T)


def kmajor(w):
    K, N = w.shape
    return np.ascontiguousarray(w.reshape(K // 128, 128, N).transpose(1, 0, 2).reshape(128, (K // 128) * N))


def host_inputs(I):
    shared = {}
    shared["wada"] = kmajor(I["w_ada"][0])
    shared["bada_row"] = np.ascontiguousarray(I["b_ada"][0].reshape(1, 6144).astype(np.float32))
    shared["gmix_row"] = np.ascontiguousarray(I["g_mix"][0].reshape(1, D).astype(np.float32))
    shared["gffn_row"] = np.ascontiguousarray(I["g_ffn"][0].reshape(1, D).astype(np.float32))
    shared["gfinal_row"] = np.ascontiguousarray(I["g_final"].reshape(1, D).astype(np.float32))
    w_in = I["w_in"][0]
    kr = w_in[:, 384:416]
    z64 = np.zeros((D, 64), np.float32)
    kr_sw = np.concatenate([kr[:, 16:32], kr[:, 0:16]], axis=1)
    win_ext = np.concatenate([w_in, z64, kr, z64, kr_sw], axis=1)
    assert win_ext.shape[1] == WC
    shared["win"] = kmajor(win_ext)
    shared["gq_fm"] = fm(I["g_q"][0], 2)
    shared["gkv_fm"] = fm(I["g_kv"][0], 1)
    wuq = I["w_uq"][0]
    wuq_sw = wuq.reshape(256, 8, 96).copy()
    wuq_sw[:, :, 64:80] = wuq.reshape(256, 8, 96)[:, :, 80:96]
    wuq_sw[:, :, 80:96] = wuq.reshape(256, 8, 96)[:, :, 64:80]
    shared["wuq"] = kmajor(wuq)
    shared["wuq_sw"] = kmajor(wuq_sw.reshape(256, 768))
    shared["wuk"] = np.ascontiguousarray(I["w_uk"][0])
    shared["wuv"] = np.ascontiguousarray(I["w_uv"][0])
    rc = np.zeros((128, 2), np.float32)
    inv = (10000.0 ** (-np.arange(16, dtype=np.float32) / 16)).astype(np.float32)
    for p in range(128):
        rc[p, 0] = inv[p % 16]
    shared["rconst"] = rc
    shared["ident"] = np.eye(128, dtype=np.float32)
    sel = np.zeros((128, 96), np.float32)
    for p in range(64, 96):
        sel[p, p] = 1.0
    shared["sel"] = sel
    kk = np.arange(128)[:, None]
    qq = np.arange(640)[None, :]
    idx = np.clip(qq - kk, -256, 256) + 256
    dq = qq // 64 - kk // 64
    valid = (dq >= 0) & (dq <= 8)
    rb = I["rel_bias"][0]
    bt = np.where(valid[None], rb[:, idx], np.float32(-1e30)).astype(np.float32)
    shared["biasT"] = np.ascontiguousarray(bt.transpose(1, 0, 2).reshape(128, 8 * 640))
    shared["woa"] = kmajor(I["w_oa"][0])
    shared["wob"] = kmajor(I["w_ob"][0])
    shared["wout"] = kmajor(I["w_out"][0])
    shared["wr"] = kmajor(np.concatenate([I["w_rg"][0], I["w_re"][0]], axis=1))
    shared["rb"] = np.ascontiguousarray(np.concatenate([I["b_rg"][0], I["b_re"][0]]).reshape(1, 36).astype(np.float32))
    shared["iota_e"] = np.ascontiguousarray(np.broadcast_to(np.arange(32, dtype=np.float32), (128, 32)))
    shared["lst"] = np.triu(np.ones((128, 128), np.float32), 1)
    shared["jv"] = np.ascontiguousarray(np.broadcast_to((np.arange(NTILE, dtype=np.float32) * SLOT_T), (128, NTILE)))
    shared["pidx"] = np.arange(128, dtype=np.float32).reshape(128, 1)
    shared["wgl"] = np.ascontiguousarray(I["w_gate"][0].reshape(32, 8, 128, 256).transpose(0, 2, 1, 3).reshape(32 * 128, 2048))
    shared["wul"] = np.ascontiguousarray(I["w_up"][0].reshape(32, 8, 128, 256).transpose(0, 2, 1, 3).reshape(32 * 128, 2048))
    shared["wdl"] = np.ascontiguousarray(I["w_down"][0].reshape(32, 2, 128, 1024).transpose(0, 2, 1, 3).reshape(32 * 128, 2048))
    per_core = []
    for b in range(8):
        d = dict(shared)
        d["x"] = np.ascontiguousarray(I["x"][b])
        d["cfm"] = fm(I["c"][b], 8)
        d["pos"] = np.ascontiguousarray(I["positions"][b].reshape(1, S).astype(np.int32))
        per_core.append(d)
    return per_core


_NC_CACHE = {}


def kernel(**inputs):
    I = {k: np.asarray(v) for k, v in inputs.items()}
    in_maps = host_inputs(I)
    if "nc" not in _NC_CACHE:
        _NC_CACHE["nc"] = build_nc()
    nc = _NC_CACHE["nc"]
    res = run_bass_kernel_spmd(nc, in_maps, core_ids=list(range(8)))
    return np.stack([r["out"] for r in res.results], axis=0).astype(np.float32)
```

```python
import os
import math
from contextlib import ExitStack

import numpy as np
import concourse.bass as bass
import concourse.mybir as mybir
from concourse.bass_utils import run_bass_kernel_spmd

F32 = mybir.dt.float32
BF16 = mybir.dt.bfloat16
I32 = mybir.dt.int32
U32 = mybir.dt.uint32
U8 = mybir.dt.uint8
AF = mybir.ActivationFunctionType
ALU = mybir.AluOpType
AX = mybir.AxisListType

S = 4096
D = 1024
TT = 512
NT = S // TT
NB = S // 128
EPS = 1e-6
WC = 4192
C_QB, C_KB, C_VB, C_GA, C_GB, C_KRP, C_KRS = 416, 928, 1440, 1952, 2976, 4000, 4096
PI = math.pi
SLOT_T = 256
SH = 8
NTILE = 64
NSLOT = NTILE * SLOT_T


class Sched:
    COMPUTE = ("pe", "act", "dve", "pool")

    def __init__(self, nc, es, n_dma_sems=8):
        self.nc = nc
        self.ops = []
        self.n_dma_sems = n_dma_sems
        self.eng_sem = {e: es.enter_context(nc.semaphore("c_" + e)) for e in self.COMPUTE}
        self.dma_sems = {q: [es.enter_context(nc.semaphore("d_%s%d" % (q, i))) for i in range(n_dma_sems)]
                         for q in ("sp", "pool", "act")}
        self.state = {}
        self.last_on = {}
        self.dma_since_bar = []

    def add(self, eng, fn, reads=(), writes=(), dma=False, extra_deps=()):
        op = dict(eng=eng, fn=fn, dma=dma, idx=len(self.ops), signal=False)
        deps = set(extra_deps)
        st = self.state
        for k in reads:
            w, rd = st.setdefault(k, [None, {}])
            if w is not None:
                deps.add(w)
        for k in writes:
            w, rd = st.setdefault(k, [None, {}])
            if w is not None:
                deps.add(w)
            deps.update(rd.values())
        me = (eng, "dma", op["idx"]) if dma else eng
        for k in reads:
            st[k][1][me] = op["idx"]
        for k in writes:
            st[k][0] = op["idx"]
            st[k][1] = {}
        real = set()
        for d in deps:
            dop = self.ops[d]
            if (not dop["dma"]) and (not dma) and dop["eng"] == "pe" and eng == "pe":
                continue
            real.add(d)
            dop["signal"] = True
        op["deps"] = real
        self.ops.append(op)
        if dma:
            self.dma_since_bar.append(op["idx"])
        elif fn is not None:
            self.last_on[eng] = op["idx"]
        return op

    def barrier(self):
        lasts = dict(self.last_on)
        dmas = list(self.dma_since_bar)
        self.dma_since_bar = []
        for eng in ("pe", "act", "dve", "pool", "sp"):
            deps = [v for e, v in lasts.items() if e != eng] + dmas
            self.add(eng, None, extra_deps=deps)
        self.state = {}

    def emit(self, block):
        cnt = {e: 0 for e in self.COMPUTE}
        dcnt = {q: 0 for q in self.dma_sems}
        for op in self.ops:
            if not op["signal"]:
                continue
            if op["dma"]:
                q = op["eng"]
                i = dcnt[q]
                dcnt[q] += 1
                op["sem"] = self.dma_sems[q][i % self.n_dma_sems]
                op["val"] = 16 * (i // self.n_dma_sems + 1)
            else:
                e = op["eng"]
                assert op["fn"] is not None
                cnt[e] += 1
                op["sem"] = self.eng_sem[e]
                op["val"] = cnt[e]
        self.stats = dict(cnt=cnt, dcnt=dcnt, nops=len(self.ops))
        per_eng = {e: [] for e in ("pe", "act", "dve", "pool", "sp")}
        for op in self.ops:
            per_eng[op["eng"]].append(op)
        ops = self.ops

        def run(eng_name, engine):
            seen = {}
            for op in per_eng[eng_name]:
                need = {}
                for d in op["deps"]:
                    dop = ops[d]
                    s, v = dop["sem"], dop["val"]
                    if need.get(s.num, (None, 0))[1] < v:
                        need[s.num] = (s, v)
                for num, (s, v) in need.items():
                    if seen.get(num, 0) >= v:
                        continue
                    engine.wait_ge(s, v)
                    seen[num] = v
                if op["fn"] is None:
                    continue
                ins = op["fn"](engine)
                if op["signal"]:
                    ins.then_inc(op["sem"], 16 if op["dma"] else 1)

        block.tensor(lambda e: run("pe", e))
        block.scalar(lambda e: run("act", e))
        block.vector(lambda e: run("dve", e))
        block.gpsimd(lambda e: run("pool", e))
        block.sync(lambda e: run("sp", e))


class Arena:
    def __init__(self, big, size):
        self.big = big
        self.size = size
        self.top = 0

    def alloc(self, free_shape, dtype):
        esz = {F32: 4, BF16: 2, I32: 4, U32: 4, U8: 1}[dtype]
        n = int(np.prod(free_shape))
        nbytes = (n * esz + 63) // 64 * 64
        off = self.top
        self.top += nbytes
        assert self.top <= self.size, "SBUF arena overflow %d > %d" % (self.top, self.size)
        v = self.big[:, off:off + n * esz]
        if dtype != U8:
            v = v.bitcast(dtype)
        if len(free_shape) == 2:
            v = v.rearrange("p (a b) -> p a b", b=free_shape[1])
        elif len(free_shape) == 3:
            v = v.rearrange("p (a b c) -> p a b c", b=free_shape[1], c=free_shape[2])
        return v

    def mark(self):
        return self.top

    def release(self, m):
        self.top = m


def build_nc(stage=99, dbg=False):
    sub = int(os.environ.get('KSUB', '99'))
    nc = bass.Bass("TRN2", target_bir_lowering=False)
    es = ExitStack()

    def din(name, shape, dt=F32):
        return nc.dram_tensor(name, list(shape), dt, kind="ExternalInput").ap()

    def dscr(name, shape, dt):
        kind = "ExternalOutput" if dbg else "Internal"
        return nc.dram_tensor(name, list(shape), dt, kind=kind).ap()

    x_d = din("x", [S, D])
    cfm_d = din("cfm", [128, 8])
    pos_d = din("pos", [1, S], I32)
    wada_d = din("wada", [128, 8 * 6144])
    badar_d = din("bada_row", [1, 6144])
    gmixr_d = din("gmix_row", [1, D])
    gffnr_d = din("gffn_row", [1, D])
    gfin_d = din("gfinal_row", [1, D])
    win_d = din("win", [128, 8 * WC])
    gq_d = din("gq_fm", [128, 2])
    gkv_d = din("gkv_fm", [128, 1])
    wuq_d = din("wuq", [128, 2 * 768])
    wuqs_d = din("wuq_sw", [128, 2 * 768])
    wuk_d = din("wuk", [128, 512])
    wuv_d = din("wuv", [128, 512])
    rconst_d = din("rconst", [128, 2])
    ident_d = din("ident", [128, 128])
    sel_d = din("sel", [128, 96])
    biasT_d = din("biasT", [128, 8 * 640])
    woa_d = din("woa", [128, 4 * D])
    wob_d = din("wob", [128, 4 * D])
    wout_d = din("wout", [128, 8 * D])
    wr_d = din("wr", [128, 8 * 36])
    rb_d = din("rb", [1, 36])
    iota_d = din("iota_e", [128, 32])
    lst_d = din("lst", [128, 128])
    jv_d = din("jv", [128, NTILE])
    sval_d = din("sval", [128, 2 * NTILE])
    pidx_d = din("pidx", [128, 1])
    wg_d = din("wgl", [32 * 128, 2048])
    wu_d = din("wul", [32 * 128, 2048])
    wdn_d = din("wdl", [32 * 128, 2048])
    out_d = nc.dram_tensor("out", [S, D], F32, kind="ExternalOutput").ap()

    tabc_d = dscr("tabc", [128, 512], F32)
    tabsp_d = dscr("tabsp", [128, 512], F32)
    tabsn_d = dscr("tabsn", [128, 512], F32)
    qT_d = dscr("qT", [96, 8 * S], BF16)
    kT_d = dscr("kT", [96, 8 * S], BF16)
    va_d = dscr("va", [S, 520], BF16)
    qbT_d = dscr("qbT", [128, 4 * S], BF16)
    kbT_d = dscr("kbT", [128, 4 * S], BF16)
    vb_d = dscr("vb", [S, 520], BF16)
    ga_d = dscr("gaT", [128, 8 * S], F32)
    gb_d = dscr("gbT", [128, 8 * S], F32)
    moddbg_d = dscr("moddbg", [128, 48], F32) if dbg else None
    x1_d = dscr("x1s", [S, D], F32)
    h2_d = dscr("h2s", [S, D], BF16)
    xs_d = dscr("xs", [NSLOT, D], BF16)
    ys_d = dscr("ys", [NSLOT, D], F32)
    wx_d = nc.dram_tensor("wx", [32 * 128, 6144], BF16, kind="Internal").ap()

    SB_BYTES = 212480
    big = nc.alloc_sbuf_tensor("big", [128, SB_BYTES], U8)
    A = Arena(big, SB_BYTES)
    banks = [nc.alloc_psum_tensor("psb%d" % i, [128, 512], F32).ap() for i in range(8)]
    sc = Sched(nc, es)

    psn = [0]

    def ps_next():
        i = psn[0] % 8
        psn[0] += 1
        return banks[i], ("ps", i)

    def dma(q, out, in_, reads, writes, **kw):
        sc.add(q, lambda e: e.dma_start(out=out, in_=in_, **kw), reads, writes, dma=True)

    def mm(out, lhsT, rhs, start, stop, reads, writes):
        sc.add("pe", lambda e: e.matmul(out, lhsT, rhs, start=start, stop=stop), reads, writes)

    def tr(out, in_, ident, reads, writes):
        sc.add("pe", lambda e: e.transpose(out, in_, ident), reads, writes)

    def act(out, in_, func, reads, writes, **kw):
        sc.add("act", lambda e: e.activation(out=out, in_=in_, func=func, **kw), reads, writes)

    def tcopy(eng, out, in_, reads, writes):
        if eng == "act":
            sc.add(eng, lambda e: e.activation(out=out, in_=in_, func=AF.Copy), reads, writes)
        else:
            sc.add(eng, lambda e: e.tensor_copy(out=out, in_=in_), reads, writes)

    def tt(eng, out, in0, in1, op, reads, writes):
        sc.add(eng, lambda e: e.tensor_tensor(out=out, in0=in0, in1=in1, op=op), reads, writes)

    def ts(eng, out, in0, s1, s2, op0, op1, reads, writes):
        if s2 is None:
            sc.add(eng, lambda e: e.tensor_scalar(out=out, in0=in0, scalar1=s1, scalar2=None, op0=op0),
                   reads, writes)
        else:
            sc.add(eng, lambda e: e.tensor_scalar(out=out, in0=in0, scalar1=s1, scalar2=s2, op0=op0, op1=op1),
                   reads, writes)

    def stt(out, in0, scalar, in1, op0, op1, reads, writes):
        sc.add("dve", lambda e: e.scalar_tensor_tensor(out=out, in0=in0, scalar=scalar, in1=in1, op0=op0, op1=op1),
               reads, writes)

    def memset(eng, ap, val, writes):
        sc.add(eng, lambda e: e.memset(ap, val), (), writes)

    def recip(out, in_, reads, writes):
        sc.add("dve", lambda e: e.reciprocal(out=out, in_=in_), reads, writes)

    ident_f = A.alloc([128], F32)
    ident_b = A.alloc([128], BF16)
    ones_f = A.alloc([128], F32)
    ones_b = A.alloc([128], BF16)
    eps_c = A.alloc([1], F32)
    modfm = A.alloc([48], F32)
    s1_fm = A.alloc([8], F32)
    s2_fm = A.alloc([8], F32)
    gt1_bc = A.alloc([D], F32)
    gt2_bc = A.alloc([D], F32)
    s2_bc = A.alloc([D], F32)
    b2_bc = A.alloc([D], F32)

    stg = A.alloc([2048], BF16)
    pre_steps = []
    for mi, wsrc in enumerate((wg_d, wu_d, wdn_d)):
        for e_ in range(32):
            pre_steps.append(("ld", mi, wsrc, e_))
            pre_steps.append(("st", mi, wsrc, e_))
    pre_pos = [0]

    def precast_step():
        if pre_pos[0] >= len(pre_steps):
            return
        kind, mi, wsrc, e_ = pre_steps[pre_pos[0]]
        pre_pos[0] += 1
        if kind == "ld":
            dma("pool", stg, wsrc[e_ * 128:(e_ + 1) * 128, :], (), ["stg"])
        else:
            dma("sp", wx_d[e_ * 128:(e_ + 1) * 128, mi * 2048:(mi + 1) * 2048], stg, ["stg"], [("wx", mi, e_)])

    dma("sp", ident_f, ident_d, (), ["ident_f"])
    tcopy("dve", ident_b, ident_f, ["ident_f"], ["ident_b"])
    memset("dve", ones_f, 1.0, ["ones_f"])
    memset("dve", ones_b, 1.0, ["ones_b"])
    memset("dve", eps_c, EPS, ["eps_c"])

    pP = A.mark()
    wuq_b = A.alloc([2, 768], BF16)
    wuqs_b = A.alloc([2, 768], BF16)
    wukp_b = A.alloc([8, 96], BF16)
    wuv_b = A.alloc([512], BF16)
    sel_b = A.alloc([96], BF16)
    gq = A.alloc([2], F32)
    gkv = A.alloc([1], F32)
    win_b = A.alloc([8, WC], BF16)
    winv = win_d.rearrange("p (k n) -> p k n", k=8)
    for kc in range(8):
        for c0 in range(0, WC, 1048):
            dma("pool", win_b[:, kc, c0:c0 + 1048], winv[:, kc, c0:c0 + 1048], (), [("win", kc, c0)])
    p0 = A.mark()
    cfm = A.alloc([8], F32)
    cact = A.alloc([8], F32)
    c_rep = A.alloc([8, 128], F32)
    bada_bc = A.alloc([6144], F32)
    gmix_bc = A.alloc([D], F32)
    gffn_bc = A.alloc([D], F32)
    sh1_r = A.alloc([D], F32)
    sc1_r = A.alloc([D], F32)
    sc2_r = A.alloc([D], F32)
    dtmp = A.alloc([128], F32)
    wbuf = [A.alloc([3072], F32) for _ in range(2)]
    dma("sp", cfm, cfm_d, (), ["cfm"])
    dma("sp", bada_bc, badar_d[0:1, :].to_broadcast([128, 6144]), (), ["bada_bc"])
    dma("sp", gmix_bc, gmixr_d[0:1, :].to_broadcast([128, D]), (), ["gmix_bc"])
    dma("sp", gffn_bc, gffnr_d[0:1, :].to_broadcast([128, D]), (), ["gffn_bc"])
    act(cact, cfm, AF.Silu, ["cfm"], ["cact"])
    tcopy("dve", c_rep, cact[:, :, None].broadcast_to([128, 8, 128]), ["cact"], ["c_rep"])
    wv = wada_d.rearrange("p (k n) -> p k n", k=8)
    dests = [sh1_r, sc1_r, gt1_bc, b2_bc, sc2_r, gt2_bc]
    dkeys = ["sh1_r", "sc1_r", "gt1_bc", "b2_bc", "sc2_r", "gt2_bc"]
    wcn = 0
    for hf_ in range(2):
        for kc in range(8):
            wb = wbuf[wcn % 2]
            wk = ("wbuf", wcn % 2)
            wcn += 1
            dma("sp", wb, wv[:, kc, hf_ * 3072:(hf_ + 1) * 3072], (), [wk])
            for nt in range(6):
                mm(banks[nt], c_rep[:, kc, :], wb[:, nt * 512:(nt + 1) * 512], kc == 0, kc == 7,
                   [wk, "c_rep"], [("ps", nt)])
        for nt in range(6):
            n0 = hf_ * 3072 + nt * 512
            di = n0 // 1024
            tt("dve", dests[di][:, n0 % 1024:n0 % 1024 + 512], banks[nt], bada_bc[:, n0:n0 + 512], ALU.add,
               [("ps", nt), "bada_bc"], [(dkeys[di], (n0 % 1024) // 512)])
    K2 = lambda nm: [(nm, 0), (nm, 1)]
    stt(sc1_r, sc1_r, 1.0, gmix_bc, ALU.add, ALU.mult, K2("sc1_r") + ["gmix_bc"], ["s1_r"])
    stt(s2_bc, sc2_r, 1.0, gffn_bc, ALU.add, ALU.mult, K2("sc2_r") + ["gffn_bc"], ["s2_bc"])
    for (row, rkeys, dst_fm, dk) in ((sc1_r, ["s1_r"], s1_fm, "s1"), (sh1_r, K2("sh1_r"), modfm, "modfm")):
        for kc in range(8):
            tt("dve", dtmp, row[:, kc * 128:(kc + 1) * 128], ident_f, ALU.mult, rkeys + ["ident_f"], ["dtmp"])
            sc.add("dve", lambda e, dst_fm=dst_fm, kc=kc: e.tensor_reduce(out=dst_fm[:, kc:kc + 1], in_=dtmp, axis=AX.X,
                                                                         op=ALU.add), ["dtmp"], [dk])

    rconst = A.alloc([2], F32)
    dma("sp", rconst, rconst_d, (), ["rconst"])
    HALF = 512
    posi = A.alloc([HALF], I32)
    ang = A.alloc([HALF], F32)
    halfpi = A.alloc([1], F32)
    memset("dve", halfpi, PI / 2, ["halfpi"])
    tmpS = [A.alloc([HALF], F32) for _ in range(4)]
    kiS = A.alloc([HALF], I32)
    for cb in range(8):
        dma("sp", posi[cb * 16:(cb + 1) * 16, :], pos_d[0:1, cb * 512:(cb + 1) * 512].to_broadcast([16, 512]), (),
            [("posi", cb)])
    POSI = [("posi", cb) for cb in range(8)]
    tcopy("dve", ang, posi, POSI, ["ang"])
    ts("dve", ang, ang, rconst[:, 0:1], None, ALU.mult, None, ["ang", "rconst"], ["ang"])
    t1, r0, mk, t2 = tmpS
    ts("dve", t1, ang, 1.0 / (2 * PI), None, ALU.mult, None, ["ang"], ["s_t1"])
    tcopy("dve", kiS, t1, ["s_t1"], ["s_ki"])
    stt(r0, kiS, -2 * PI, ang, ALU.mult, ALU.add, ["s_ki", "ang"], ["s_r0"])
    ts("dve", mk, r0, PI, -2 * PI, ALU.is_gt, ALU.mult, ["s_r0"], ["s_mk"])
    tt("dve", r0, r0, mk, ALU.add, ["s_r0", "s_mk"], ["s_r0"])
    ts("dve", mk, r0, -PI, 2 * PI, ALU.is_lt, ALU.mult, ["s_r0"], ["s_mk"])
    tt("dve", r0, r0, mk, ALU.add, ["s_r0", "s_mk"], ["s_r0"])
    ts("dve", r0, r0, PI, -PI, ALU.min, ALU.max, ["s_r0"], ["s_r0"])
    act(t1, r0, AF.Sin, ["s_r0"], ["s_t1"])
    dma("sp", tabsp_d, t1, ["s_t1"], ["tabsp"])
    act(t2, r0, AF.Sin, ["s_r0"], ["s_t2"], scale=-1.0)
    dma("sp", tabsn_d, t2, ["s_t2"], ["tabsn"])
    ts("dve", t1, ang, PI / 2, 1.0 / (2 * PI), ALU.add, ALU.mult, ["ang", "s_t1"], ["s_t1"])
    tcopy("dve", kiS, t1, ["s_t1"], ["s_ki"])
    stt(r0, kiS, -2 * PI, ang, ALU.mult, ALU.add, ["s_ki", "ang", "s_r0"], ["s_r0"])
    ts("dve", mk, r0, PI / 2, -2 * PI, ALU.is_gt, ALU.mult, ["s_r0"], ["s_mk"])
    tt("dve", r0, r0, mk, ALU.add, ["s_r0", "s_mk"], ["s_r0"])
    ts("dve", mk, r0, -1.5 * PI, 2 * PI, ALU.is_lt, ALU.mult, ["s_r0"], ["s_mk"])
    tt("dve", r0, r0, mk, ALU.add, ["s_r0", "s_mk"], ["s_r0"])
    ts("dve", r0, r0, PI / 2, -1.5 * PI, ALU.min, ALU.max, ["s_r0"], ["s_r0"])
    act(t1, r0, AF.Sin, ["s_r0", "halfpi"], ["s_t1"], bias=halfpi[:, 0:1])
    dma("sp", tabc_d, t1, ["s_t1"], ["tabc"])

    wtmp = A.alloc([2, 768], F32)
    wtmp2 = A.alloc([2, 768], F32)
    wtmp3 = A.alloc([512], F32)
    wtmp4 = A.alloc([512], F32)
    seltmp = A.alloc([96], F32)
    dma("sp", gq, gq_d, (), ["gq"])
    dma("sp", gkv, gkv_d, (), ["gkv"])
    dma("sp", wtmp, wuq_d.rearrange("p (k n) -> p k n", k=2), (), ["wtmp"])
    dma("sp", wtmp2, wuqs_d.rearrange("p (k n) -> p k n", k=2), (), ["wtmp2"])
    dma("sp", wtmp3, wuk_d, (), ["wtmp3"])
    dma("sp", wtmp4, wuv_d, (), ["wtmp4"])
    dma("sp", seltmp, sel_d, (), ["seltmp"])
    for kc in range(2):
        ts("dve", wuq_b[:, kc, :], wtmp[:, kc, :], gq[:, kc:kc + 1], None, ALU.mult, None, ["wtmp", "gq"], ["wuq_b"])
        ts("dve", wuqs_b[:, kc, :], wtmp2[:, kc, :], gq[:, kc:kc + 1], None, ALU.mult, None, ["wtmp2", "gq"], ["wuqs_b"])
    memset("dve", wukp_b, 0.0, ["wukp_b"])
    ts("dve", wukp_b[:, :, 0:64], wtmp3.rearrange("p (h d) -> p h d", d=64), gkv[:, 0:1], None, ALU.mult, None,
       ["wtmp3", "gkv", "wukp_b"], ["wukp_b"])
    ts("dve", wuv_b, wtmp4, gkv[:, 0:1], None, ALU.mult, None, ["wtmp4", "gkv"], ["wuv_b"])
    tcopy("dve", sel_b[0:96], seltmp[0:96], ["seltmp"], ["sel_b"])

    sc.barrier()
    A.release(p0)
    if stage <= 0:
        return finish(nc, es, sc, None)

    WIN = []

    xb = [A.alloc([D], F32) for _ in range(2)]
    junk = A.alloc([D], BF16)
    ssq = A.alloc([4], F32)
    rstd = A.alloc([4], F32)
    xn = [A.alloc([D], F32) for _ in range(2)]
    hT = [A.alloc([8, TT], BF16) for _ in range(2)]
    ctab = A.alloc([TT], F32)
    stab = A.alloc([TT], F32)
    qlat = A.alloc([2, TT], F32)
    qsq = A.alloc([2, TT], F32)
    kvlat = A.alloc([TT], F32)
    kvsq = A.alloc([TT], F32)
    rbc = [A.alloc([TT], F32) for _ in range(2)]
    qln = A.alloc([2, TT], BF16)
    kvn = A.alloc([TT], BF16)
    krr = A.alloc([TT], BF16)
    rt1 = [A.alloc([TT], F32) for _ in range(2)]
    rt2 = [A.alloc([TT], F32) for _ in range(2)]
    qT_s = A.alloc([8, TT], BF16)
    kT_s = A.alloc([8, TT], BF16)
    qbT_s = A.alloc([4, TT], BF16)
    kbT_s = A.alloc([4, TT], BF16)
    va_s = A.alloc([4, 520], BF16)
    vb_s = A.alloc([4, 520], BF16)
    gst = [A.alloc([TT], F32) for _ in range(4)]
    memset("dve", ctab[0:64], 1.0, ["ctab0"])
    memset("dve", stab[0:64], 0.0, ["stab0"])
    memset("dve", va_s, 1.0, ["va_s"])
    memset("dve", vb_s, 1.0, ["vb_s"])

    qT_v = qT_d.rearrange("p (h t) -> p h t", h=8)
    kT_v = kT_d.rearrange("p (h t) -> p h t", h=8)
    qbT_v = qbT_d.rearrange("p (h t) -> p h t", h=4)
    kbT_v = kbT_d.rearrange("p (h t) -> p h t", h=4)
    ga_v = ga_d.rearrange("p (c t) -> p c t", c=8)
    gb_v = gb_d.rearrange("p (c t) -> p c t", c=8)
    gcnt = [0]
    ecnt = [0]

    def evac_copy(out, in_, reads, writes):
        eng = "act" if ecnt[0] % 2 == 0 else "dve"
        ecnt[0] += 1
        tcopy(eng, out, in_, reads, writes)

    NT1 = NT if stage > 1 else 1

    def prep_stats(ti, bi):
        t0 = ti * TT
        g = ti * 4 + bi
        xt = xb[g % 2]
        xk = ("xb", g % 2)
        dma("sp", xt, x_d[t0 + bi * 128:t0 + (bi + 1) * 128, :], (), [xk])
        act(junk, xt, AF.Square, [xk], [("ssq", bi)], accum_out=ssq[:, bi:bi + 1])
        act(rstd[:, bi:bi + 1], ssq[:, bi:bi + 1], AF.Sqrt, [("ssq", bi), "eps_c"], [("rstd", bi)],
            scale=1.0 / D, bias=eps_c[:, 0:1])
        recip(rstd[:, bi:bi + 1], rstd[:, bi:bi + 1], [("rstd", bi)], [("rstd", bi)])
        ts("dve", xn[g % 2], xt, rstd[:, bi:bi + 1], None, ALU.mult, None, [xk, ("rstd", bi)], [("xn", g % 2)])

    def prep_tr(ti, bi):
        g = ti * 4 + bi
        xnt = xn[g % 2]
        nk = ("xn", g % 2)
        h_t = hT[ti % 2]
        for half in range(2):
            pb, pk = ps_next()
            for q in range(4):
                kc = half * 4 + q
                tr(pb[:, q * 128:(q + 1) * 128], xnt[:, kc * 128:(kc + 1) * 128], ident_f, [nk, "ident_f"], [pk])
            for q in range(4):
                kc = half * 4 + q
                dst = h_t[:, kc, bi * 128:(bi + 1) * 128]
                hk = ("hT", ti % 2, bi, q % 2)
                act(dst, pb[:, q * 128:(q + 1) * 128], AF.Identity, [pk, "s1", "modfm"], [hk],
                    scale=s1_fm[:, kc:kc + 1], bias=modfm[:, kc:kc + 1])

    def prep_all(ti):
        prep_stats(ti, 0)
        prep_stats(ti, 1)
        prep_tr(ti, 0)
        prep_stats(ti, 2)
        prep_tr(ti, 1)
        prep_stats(ti, 3)
        prep_tr(ti, 2)
        prep_tr(ti, 3)

    prep_all(0)
    for ti in range(NT1):
        t0 = ti * TT
        h_t = hT[ti % 2]
        HK = [("hT", ti % 2, bi_, q_) for bi_ in range(4) for q_ in range(2)]
        nxt = ti + 1 if ti + 1 < NT1 else None
        dma("sp", ctab[64:80], tabc_d[ti * 16:(ti + 1) * 16, :], (), ["ctab"])
        dma("sp", ctab[80:96], tabc_d[ti * 16:(ti + 1) * 16, :], (), ["ctab2"])
        dma("sp", stab[64:80], tabsn_d[ti * 16:(ti + 1) * 16, :], (), ["stab"])
        dma("sp", stab[80:96], tabsp_d[ti * 16:(ti + 1) * 16, :], (), ["stab2"])

        def proj(c0, m):
            pb, pk = ps_next()
            for kc in range(8):
                mm(pb[0:m, :], win_b[:, kc, c0:c0 + m], h_t[:, kc, :], kc == 0, kc == 7, HK + WIN, [pk])
            return pb, pk

        for c in range(2):
            pb, pk = proj(c * 128, 128)
            act(qlat[:, c, :], pb, AF.Copy, [pk], [("qlat", c)])
            act(qsq[:, c, :], pb, AF.Square, [pk], [("qsq", c)])
        pb, pk = proj(256, 128)
        act(kvlat, pb, AF.Copy, [pk], ["kvlat"])
        act(kvsq, pb, AF.Square, [pk], ["kvsq"])
        pa, pka = proj(C_KRP, 96)
        pbb, pkb = proj(C_KRS, 96)
        tt("dve", rt1[0][0:96], pa[0:96, :], ctab[0:96], ALU.mult, [pka, "ctab", "ctab2", "ctab0"], [("rt1", 0)])
        tt("dve", rt2[0][0:96], pbb[0:96, :], stab[0:96], ALU.mult, [pkb, "stab", "stab2", "stab0"], [("rt2", 0)])
        tt("pool", krr[0:96], rt1[0][0:96], rt2[0][0:96], ALU.add, [("rt1", 0), ("rt2", 0)], ["krr"])
        pq, pkq = ps_next()
        for c in range(2):
            mm(pq, ones_f, qsq[:, c, :], c == 0, c == 1, ["ones_f", ("qsq", c)], [pkq])
        act(rbc[0], pq, AF.Sqrt, [pkq, "eps_c"], [("rbc", 0)], scale=1.0 / 256, bias=eps_c[:, 0:1])
        recip(rbc[0], rbc[0], [("rbc", 0)], [("rbc", 0)])
        for c in range(2):
            tt("dve", qln[:, c, :], qlat[:, c, :], rbc[0], ALU.mult, [("qlat", c), ("rbc", 0)], [("qln", c)])
        pkv, pkkv = ps_next()
        mm(pkv, ones_f, kvsq, True, True, ["ones_f", "kvsq"], [pkkv])
        act(rbc[1], pkv, AF.Sqrt, [pkkv, "eps_c"], [("rbc", 1)], scale=1.0 / 128, bias=eps_c[:, 0:1])
        recip(rbc[1], rbc[1], [("rbc", 1)], [("rbc", 1)])
        tt("dve", kvn, kvlat, rbc[1], ALU.mult, ["kvlat", ("rbc", 1)], ["kvn"])
        if nxt is not None:
            prep_stats(nxt, 0)
            prep_stats(nxt, 1)
        for i in range(4):
            pb, pk = proj(C_QB + i * 128, 128)
            evac_copy(qbT_s[:, i, :], pb, [pk], ["qbT_s"])
        dma("sp", qbT_v[:, :, t0:t0 + TT], qbT_s, ["qbT_s"], [("qbT_d", ti)])
        for h in range(8):
            pa, pka = ps_next()
            for c in range(2):
                mm(pa[0:96, :], wuq_b[:, c, h * 96:(h + 1) * 96], qln[:, c, :], c == 0, c == 1,
                   ["wuq_b", ("qln", c)], [pka])
            pbb, pkb = ps_next()
            for c in range(2):
                mm(pbb[0:96, :], wuqs_b[:, c, h * 96:(h + 1) * 96], qln[:, c, :], c == 0, c == 1,
                   ["wuqs_b", ("qln", c)], [pkb])
            i2 = h % 2
            tt("dve", rt1[i2][0:96], pa[0:96, :], ctab[0:96], ALU.mult, [pka, "ctab", "ctab2", "ctab0"], [("rt1", i2)])
            tt("dve", rt2[i2][0:96], pbb[0:96, :], stab[0:96], ALU.mult, [pkb, "stab", "stab2", "stab0"], [("rt2", i2)])
            tt("pool", qT_s[0:96, h, :], rt1[i2][0:96], rt2[i2][0:96], ALU.add, [("rt1", i2), ("rt2", i2)], ["qT_s"])
            pk_, pkk = ps_next()
            mm(pk_[0:96, :], wukp_b[:, h, :], kvn, True, False, ["wukp_b", "kvn"], [pkk])
            mm(pk_[0:96, :], sel_b[0:96, :], krr[0:96, :], False, True, ["sel_b", "krr"], [pkk])
            evac_copy(kT_s[0:96, h, :], pk_[0:96, :], [pkk], ["kT_s"])
        for bi in range(4):
            pv, pkv_ = ps_next()
            mm(pv, kvn[:, bi * 128:(bi + 1) * 128], wuv_b, True, True, ["kvn", "wuv_b"], [pkv_])
            evac_copy(va_s[:, bi, :].rearrange("p (h d) -> p h d", d=65)[:, :, 0:64],
                      pv.rearrange("p (h d) -> p h d", d=64), [pkv_], ["va_s"])
        dma("sp", qT_v[:, :, t0:t0 + TT], qT_s[0:96], ["qT_s"], [("qT_d", ti)])
        dma("sp", kT_v[:, :, t0:t0 + TT], kT_s[0:96], ["kT_s"], [("kT_d", ti)])
        dma("sp", va_d[t0:t0 + TT, :].rearrange("(b p) f -> p b f", p=128), va_s, ["va_s"], [("va_d", ti)])
        if nxt is not None:
            prep_tr(nxt, 0)
            prep_stats(nxt, 2)
        for i in range(4):
            pb, pk = proj(C_KB + i * 128, 128)
            evac_copy(kbT_s[:, i, :], pb, [pk], ["kbT_s"])
        dma("sp", kbT_v[:, :, t0:t0 + TT], kbT_s, ["kbT_s"], [("kbT_d", ti)])
        if nxt is not None:
            prep_tr(nxt, 1)
            prep_stats(nxt, 3)
        for bi in range(4):
            pv, pkv_ = ps_next()
            for kc in range(8):
                mm(pv, h_t[:, kc, bi * 128:(bi + 1) * 128], win_b[:, kc, C_VB:C_VB + 512], kc == 0, kc == 7,
                   HK + WIN, [pkv_])
            evac_copy(vb_s[:, bi, :].rearrange("p (h d) -> p h d", d=65)[:, :, 0:64],
                      pv.rearrange("p (h d) -> p h d", d=64), [pkv_], ["vb_s"])
        dma("sp", vb_d[t0:t0 + TT, :].rearrange("(b p) f -> p b f", p=128), vb_s, ["vb_s"], [("vb_d", ti)])
        if nxt is not None:
            prep_tr(nxt, 2)
        for gix, (cbase, gv, nm) in enumerate(((C_GA, ga_v, "ga"), (C_GB, gb_v, "gb"))):
            for c in range(8):
                pb, pk = proj(cbase + c * 128, 128)
                gi = gcnt[0] % 4
                gcnt[0] += 1
                act(gst[gi], pb, AF.Sigmoid, [pk], [("gst", gi)])
                dma("sp", gv[:, c, t0:t0 + TT], gst[gi], [("gst", gi)], [(nm, ti, c)])
            if gix == 0 and nxt is not None:
                prep_tr(nxt, 3)

    sc.barrier()
    if stage <= 2:
        return finish(nc, es, sc, None)

    A.release(pP)
    o_a = A.alloc([NB, 512], BF16)
    o_b = A.alloc([NB, 512], BF16)
    p2 = A.mark()
    kT_r = A.alloc([8, S], BF16)
    va_r = A.alloc([NB, 520], BF16)
    qT_t = [A.alloc([8, TT], BF16) for _ in range(2)]
    E_t = [A.alloc([TT], BF16) for _ in range(6)]
    rden = [A.alloc([4], F32) for _ in range(2)]
    for h in range(8):
        dma("sp", kT_r[0:96, h, :], kT_v[:, h, :], (), [("kT_r", h)])
    va_v = va_d.rearrange("(b p) f -> p b f", p=128)
    for q4 in range(4):
        dma("sp", va_r[:, q4 * 8:(q4 + 1) * 8, :], va_v[:, q4 * 8:(q4 + 1) * 8, :], (), [("va_r", q4)])
    SCALE_A = 96 ** -0.5
    sbank = [0]
    ecnt2 = [0]
    accn = [0]
    NQT = NT if stage > 3 else 2
    LOOK = 3
    stageA, stageB = [], []
    for qt in range(NQT):
        for h in range(8):
            nkt = 4 * qt + 4
            for kt in range(nkt):
                stageA.append((qt, h, kt))
    grp = {}

    def emitA(rec):
        qt, h, kt = rec
        qtt = qT_t[qt % 2]
        qk = ("qT_t", qt % 2)
        if h == 0 and kt == 0:
            dma("sp", qtt[0:96], qT_v[:, :, qt * TT:(qt + 1) * TT], (), [qk])
        r = kt - 4 * qt
        c0 = 128 * r if r > 0 else 0
        sb = sbank[0] % 5
        sbank[0] += 1
        ps_, psk = banks[sb], ("ps", sb)
        mm(ps_[:, c0:TT], kT_r[0:96, h, kt * 128:(kt + 1) * 128], qtt[0:96, h, c0:TT], True, True,
           [("kT_r", h), qk], [psk])
        ei = ecnt2[0] % len(E_t)
        ecnt2[0] += 1
        Et, Ek = E_t[ei], ("E", ei)
        act(Et[:, c0:TT], ps_[:, c0:TT], AF.Exp, [psk], [Ek], scale=SCALE_A)
        if r >= 0:
            memset("dve", Et[64:128, c0:c0 + 64], 0.0, [Ek])
        grp[rec] = (Et, Ek)

    def emitB(rec):
        qt, h, kt = rec
        nkt = 4 * qt + 4
        r = kt - 4 * qt
        if kt == 0:
            ab = 5 + accn[0] % 2
            accn[0] += 1
            grp["acc"] = (banks[ab], ("ps", ab))
        acc, acck = grp["acc"]
        Et, Ek = grp.pop(rec)
        for qb in range(max(r, 0), 4):
            first = (kt == 0) and (qb == 0)
            last = (kt == nkt - 1) and (qb == 3)
            mm(acc[:, qb * 65:(qb + 1) * 65], Et[:, qb * 128:(qb + 1) * 128],
               va_r[:, kt, h * 65:(h + 1) * 65], first, last, [Ek, ("va_r", kt // 8)], [acck])
        if kt == nkt - 1:
            rd = rden[h % 2]
            rk = ("rden", h % 2)
            accv = acc[:, 0:260].rearrange("p (b d) -> p b d", d=65)
            recip(rd, accv[:, :, 64], [acck], [rk])
            tt("dve", o_a[:, qt * 4:(qt + 1) * 4, h * 64:(h + 1) * 64], accv[:, :, 0:64],
               rd[:, :, None].broadcast_to([128, 4, 64]), ALU.mult, [acck, rk], [("o_a", qt)])

    for i in range(len(stageA) + LOOK):
        if i % 8 == 3:
            precast_step()
        if i < len(stageA):
            emitA(stageA[i])
        if i >= LOOK:
            emitB(stageA[i - LOOK])
    if dbg:
        oa_dbg = dscr("oa_dbg", [S, 512], BF16)
        dma("sp", oa_dbg.rearrange("(b p) f -> p b f", p=128), o_a, [("o_a", q) for q in range(NQT)], ["oa_dbg"])
    sc.barrier()
    if stage <= 4:
        return finish(nc, es, sc, None)

    A.release(p2)
    qb_p = [A.alloc([S], BF16) for _ in range(2)]
    kb_p = [A.alloc([S], BF16) for _ in range(2)]
    vb_r = A.alloc([NB, 520], BF16)
    bias_r = A.alloc([8, 640], F32)
    Eb = [A.alloc([640], BF16) for _ in range(10)]
    stmp = [A.alloc([640], F32) for _ in range(4)]
    rden3 = [A.alloc([4], F32) for _ in range(2)]
    vb_v = vb_d.rearrange("(b p) f -> p b f", p=128)
    for q4 in range(4):
        dma("sp", vb_r[:, q4 * 8:(q4 + 1) * 8, :], vb_v[:, q4 * 8:(q4 + 1) * 8, :], (), [("vb_r", q4)])
    dma("sp", bias_r, biasT_d.rearrange("p (h q) -> p h q", h=8), (), ["bias_r"])
    for h_ in range(8):
        act(bias_r[:, h_, :], bias_r[:, h_, :], AF.Exp, ["bias_r"], ["bias_r"])
    sbank[0] = 0
    ecnt3 = 0
    stc = 0
    NJ = NB if stage > 5 else 8
    recs3 = [(h, j) for h in range(8) for j in range(NJ)]
    ering = {}
    st3 = dict(ecnt=0, stc=0, bs=0)

    def emitA3(rec):
        h, j = rec
        pr, po = h // 2, (h % 2) * 64
        qb_r, kb_r = qb_p[pr % 2], kb_p[pr % 2]
        if h % 2 == 0 and j == 0:
            dma("sp", qb_r, qbT_v[:, pr, :], (), [("qb_r", pr % 2)])
            dma("sp", kb_r, kbT_v[:, pr, :], (), [("kb_r", pr % 2)])
        nq = min(640, S - 128 * j)
        n1 = min(nq, 512)
        sb = sbank[0] % 4
        sbank[0] += 1
        psA, pkA = banks[sb], ("ps", sb)
        st_ = stmp[st3["stc"] % 4]
        stk = ("stmp", st3["stc"] % 4)
        st3["stc"] += 1
        mm(psA[:, 0:n1], kb_r[po:po + 64, 128 * j:128 * j + 128], qb_r[po:po + 64, 128 * j:128 * j + n1],
           True, True, [("kb_r", pr % 2), ("qb_r", pr % 2)], [pkA])
        act(st_[:, 0:n1], psA[:, 0:n1], AF.Exp, [pkA], [(stk, 0)], scale=0.125)
        if nq > 512:
            bslot = (4, 7)[st3["bs"] % 2]
            st3["bs"] += 1
            psB, pkB = banks[bslot], ("ps", bslot)
            mm(psB[:, 0:nq - 512], kb_r[po:po + 64, 128 * j:128 * j + 128],
               qb_r[po:po + 64, 128 * j + 512:128 * j + nq], True, True, [("kb_r", pr % 2), ("qb_r", pr % 2)], [pkB])
            act(st_[:, 512:nq], psB[:, 0:nq - 512], AF.Exp, [pkB], [(stk, 1)], scale=0.125)
        ei = st3["ecnt"] % len(Eb)
        st3["ecnt"] += 1
        Et, Ek = Eb[ei], ("Eb", ei)
        tt("dve" if st3["ecnt"] % 2 == 0 else "pool", Et[:, 0:nq], st_[:, 0:nq], bias_r[:, h, 0:nq], ALU.mult,
           [(stk, 0), (stk, 1), "bias_r"], [Ek])
        ering[(h, j)] = (Et, Ek)

    def emitB3(rec):
        h, j = rec
        if j % 4 == 0:
            ab = 5 + accn[0] % 2
            accn[0] += 1
            ering["acc"] = (banks[ab], ("ps", ab))
        acc, acck = ering["acc"]
        jj0 = max(0, j - 4)
        for jj in range(jj0, j + 1):
            Ej, Ejk = ering[(h, jj)]
            off = (j - jj) * 128
            mm(acc[:, (j % 4) * 65:(j % 4 + 1) * 65], Ej[:, off:off + 128], vb_r[:, jj, h * 65:(h + 1) * 65],
               (j % 4 == 0) and (jj == jj0), (j % 4 == 3) and (jj == j), [Ejk, ("vb_r", jj // 8)], [acck])
        if j % 4 == 3:
            rd = rden3[(j // 4) % 2]
            rk = ("rden3", (j // 4) % 2)
            accv = acc[:, 0:260].rearrange("p (b d) -> p b d", d=65)
            recip(rd, accv[:, :, 64], [acck], [rk])
            tt("dve", o_b[:, j - 3:j + 1, h * 64:(h + 1) * 64], accv[:, :, 0:64],
               rd[:, :, None].broadcast_to([128, 4, 64]), ALU.mult, [acck, rk], [("o_b", j // 4)])

    LOOK3 = 3
    for i in range(len(recs3) + LOOK3):
        if i % 5 == 2:
            precast_step()
        if i < len(recs3):
            emitA3(recs3[i])
        if i >= LOOK3:
            emitB3(recs3[i - LOOK3])
    if dbg:
        ob_dbg = dscr("ob_dbg", [S, 512], BF16)
        dma("sp", ob_dbg.rearrange("(b p) f -> p b f", p=128), o_b, [("o_b", q) for q in range(NJ // 4)], ["ob_dbg"])
    sc.barrier()
    if stage <= 6:
        return finish(nc, es, sc, None)

    A.release(p2)
    cw_all = A.alloc([NB, 2], F32)
    e_all = A.alloc([NB, 2], F32)
    r_all = A.alloc([NB, 2], F32)
    A1all = A.alloc([NB, 32], F32)
    A2all = A.alloc([NB, 32], F32)
    carry = A.alloc([32], F32)
    iota_e = A.alloc([32], F32)
    rbias = A.alloc([36], F32)
    Lst = A.alloc([128], BF16)
    p4 = A.mark()
    woa_b = A.alloc([4, D], BF16)
    wob_b = A.alloc([4, D], BF16)
    wout_b = A.alloc([8, D], BF16)
    wr_f = A.alloc([8, 36], F32)
    for c in range(4):
        dma("pool", woa_b[:, c, :], woa_d.rearrange("p (k n) -> p k n", k=4)[:, c, :], (), [("woa", c)])
        dma("pool", wob_b[:, c, :], wob_d.rearrange("p (k n) -> p k n", k=4)[:, c, :], (), [("wob", c)])
    WOA = [("woa", c) for c in range(4)]
    WOB = [("wob", c) for c in range(4)]
    WOUT = [("wout", c) for c in range(8)]
    dma("sp", wr_f, wr_d.rearrange("p (k n) -> p k n", k=8), (), ["wr_f"])
    dma("sp", rbias, rb_d[0:1, :].to_broadcast([128, 36]), (), ["rbias"])
    dma("sp", iota_e, iota_d, (), ["iota_e"])
    ltmp = A.alloc([128], F32)
    dma("sp", ltmp, lst_d, (), ["ltmp"])
    tcopy("dve", Lst, ltmp, ["ltmp"], ["Lst"])
    memset("dve", carry, 0.0, ["carry"])

    oaT2 = [A.alloc([4, TT], BF16) for _ in range(2)]
    obT2 = [A.alloc([4, TT], BF16) for _ in range(2)]
    mT = A.alloc([8, TT], BF16)
    gat = [A.alloc([TT], F32) for _ in range(2)]
    gbt = [A.alloc([TT], F32) for _ in range(2)]
    mt1 = [A.alloc([TT], F32) for _ in range(2)]
    mt2 = [A.alloc([TT], F32) for _ in range(2)]
    R4 = 2
    RX = 3
    xr = [A.alloc([D], F32) for _ in range(RX)]
    uu = [A.alloc([D], F32) for _ in range(R4)]
    h2b = [A.alloc([D], BF16) for _ in range(2)]
    h2T = [A.alloc([8, 128], F32) for _ in range(2)]
    rs_t = A.alloc([2, 4], F32)
    wr_s = A.alloc([8, 36], F32)
    s2_fm8 = A.alloc([8], F32)
    b2_fm8 = A.alloc([8], F32)
    dtmp4 = A.alloc([128], F32)
    sm = A.alloc([16], F32)
    lg = A.alloc([4, 36], F32)
    dg = A.alloc([4, 4], F32)
    ge = A.alloc([4, 4], F32)
    pen = A.alloc([4, 4], F32)
    msk = A.alloc([4, 32], F32)
    top8a = A.alloc([4, 8], F32)
    idx8a = A.alloc([4, 8], U32)
    r4s = A.alloc([8, 4], F32)
    Ab = A.alloc([4, 32], BF16)
    Pt = A.alloc([4, 32], F32)
    ptm = A.alloc([4, 32], F32)
    woutv = wout_d.rearrange("p (k n) -> p k n", k=8)
    for c in range(8):
        stb = xr[c % RX]
        stk_ = [("x1", c % RX, 0), ("x1", c % RX, 1)]
        dma("sp", stb, woutv[:, c, :], (), stk_)
        tt("dve", wout_b[:, c, :], stb, gt1_bc, ALU.mult, stk_, [("wout", c)])
    for (row, rk_, dst_fm, dk) in ((s2_bc, [], s2_fm8, "s2_fm8"), (b2_bc, [], b2_fm8, "b2_fm8")):
        for kc in range(8):
            tt("dve", dtmp4, row[:, kc * 128:(kc + 1) * 128], ident_f, ALU.mult, ["ident_f"], ["dtmp4"])
            sc.add("dve", lambda e, dst_fm=dst_fm, kc=kc: e.tensor_reduce(out=dst_fm[:, kc:kc + 1], in_=dtmp4, axis=AX.X,
                                                                         op=ALU.add), ["dtmp4"], [dk])
    for kc in range(8):
        ts("dve", wr_s[:, kc, :], wr_f[:, kc, :], s2_fm8[:, kc:kc + 1], None, ALU.mult, None, ["wr_f", "s2_fm8"], ["wr_s"])
    b2rep = uu[0].rearrange("p (k m) -> p k m", k=8)
    tcopy("dve", b2rep, b2_fm8[:, :, None].broadcast_to([128, 8, 128]), ["b2_fm8"], [("uu", 0)])
    for kc in range(8):
        mm(banks[7][:, 0:36], b2rep[:, kc, :], wr_f[:, kc, :], kc == 0, kc == 7, [("uu", 0), "wr_f"], [("ps", 7)])
    tt("dve", rbias, banks[7][:, 0:36], rbias, ALU.add, [("ps", 7), "rbias"], ["rbias"])
    RB = 6
    rot = [0]

    def ps_rot():
        i = rot[0] % RB
        rot[0] += 1
        return banks[i], ("ps", i)

    pp, pkp = banks[6], ("ps", 6)
    rl, pkr = banks[7], ("ps", 7)
    NT4 = NT if stage > 7 else 1

    def S1(ti, bi):
        blk = ti * 4 + bi
        t0 = ti * TT
        r3 = blk % R4
        rx = blk % RX
        g2 = blk % 2
        tp = ti % 2
        xk = [("x1", rx, 0), ("x1", rx, 1)]
        rsk = ("rs_t", tp, bi)
        dma("sp", xr[rx], x_d[t0 + bi * 128:t0 + (bi + 1) * 128, :], (), xk)
        for half in range(2):
            pm, pkm = ps_rot()
            for m in range(8):
                mm(pm, mT[:, m, bi * 128:(bi + 1) * 128], wout_b[:, m, half * 512:(half + 1) * 512],
                   m == 0, m == 7, [("mT", m_) for m_ in range(8)] + WOUT, [pkm])
            hs = slice(half * 512, (half + 1) * 512)
            tt("dve", xr[rx][:, hs], pm, xr[rx][:, hs], ALU.add, [pkm, ("x1", rx, half)], [("x1", rx, half)])
        dma("sp", x1_d[t0 + bi * 128:t0 + (bi + 1) * 128, :], xr[rx], xk, [("x1_d", blk)])

    def S1b(ti, bi):
        blk = ti * 4 + bi
        t0 = ti * TT
        r3 = blk % R4
        rx = blk % RX
        g2 = blk % 2
        tp = ti % 2
        xk = [("x1", rx, 0), ("x1", rx, 1)]
        rsk = ("rs_t", tp, bi)
        act(h2b[g2], xr[rx], AF.Square, xk, [("ssq4", g2), ("h2b", g2)], accum_out=sm[:, g2:g2 + 1])
        act(sm[:, 2 + g2:3 + g2], sm[:, g2:g2 + 1], AF.Ln, [("ssq4", g2), "eps_c"], [("rs4", g2)],
            scale=1.0 / D, bias=eps_c[:, 0:1])
        act(rs_t[:, tp, bi:bi + 1], sm[:, 2 + g2:3 + g2], AF.Exp, [("rs4", g2)], [rsk], scale=-0.5)
        tt("pool", uu[r3], xr[rx], s2_bc, ALU.mult, xk, [("uu", r3)])
        stt(uu[r3], uu[r3], rs_t[:, tp, bi:bi + 1], b2_bc, ALU.mult, ALU.add, [("uu", r3), rsk], [("uu", r3)])
        tcopy("act", h2b[g2], uu[r3], [("uu", r3), ("ssq4", g2)], [("h2b", g2)])
        dma("sp", h2_d[t0 + bi * 128:t0 + (bi + 1) * 128, :], h2b[g2], [("h2b", g2)], [("h2_d", blk)])

    def S2a(ti, bi):
        blk = ti * 4 + bi
        rx = blk % RX
        hb = blk % 2
        xk = [("x1", rx, 0), ("x1", rx, 1)]
        for half in range(2):
            pb, pk = ps_rot()
            for q in range(4):
                kc = half * 4 + q
                tr(pb[:, q * 128:(q + 1) * 128], xr[rx][:, kc * 128:(kc + 1) * 128], ident_f, xk, [pk])
            evac_copy(h2T[hb][:, half * 4:(half + 1) * 4, :], pb.rearrange("p (q t) -> p q t", q=4), [pk],
                      [("h2T", hb, half)])
        for kc in range(8):
            mm(rl[:, bi * 36:(bi + 1) * 36], h2T[hb][:, kc, :], wr_s[:, kc, :], (bi == 0) and (kc == 0),
               (bi == 3) and (kc == 7), [("h2T", hb, kc // 4), "wr_s"], [pkr])

    def dve(fn, reads, writes):
        sc.add("dve", fn, reads, writes)

    def S2b1(ti):
        b0 = ti * 4
        bs = slice(b0, b0 + 4)
        tt("dve", lg, rl[:, 0:144].rearrange("p (b n) -> p b n", n=36),
           rs_t[:, ti % 2, :, None].broadcast_to([128, 4, 36]), ALU.mult,
           [pkr] + [("rs_t", ti % 2, b_) for b_ in range(4)], ["lg"])
        tt("dve", lg, lg, rbias[:, None, :].broadcast_to([128, 4, 36]), ALU.add, ["lg", "rbias"], ["lg"])
        gmax = r4s[:, 0, :]
        dve(lambda e: e.tensor_reduce(out=gmax, in_=lg[:, :, 0:4], axis=AX.X, op=ALU.max), ["lg"], ["gmax"])
        tt("dve", dg, lg[:, :, 0:4], gmax[:, :, None].broadcast_to([128, 4, 4]), ALU.subtract, ["lg", "gmax"], ["dg"])
        act(ge, dg, AF.Exp, ["dg"], ["ge"])
        gsum = r4s[:, 1, :]
        dve(lambda e: e.tensor_reduce(out=gsum, in_=ge, axis=AX.X, op=ALU.add), ["ge"], ["gsum"])
        gw = r4s[:, 2, :]
        recip(gw, gsum, ["gsum"], ["gw"])
        ts("dve", pen, dg, 0.0, None, ALU.is_equal, None, ["dg"], ["pen"])
        ts("dve", pen, pen, -1.0, 1e30, ALU.add, ALU.mult, ["pen"], ["pen"])
        tt("dve", msk.rearrange("p b (g e) -> p b g e", e=8), lg[:, :, 4:36].rearrange("p b (g e) -> p b g e", e=8),
           pen[:, :, :, None].broadcast_to([128, 4, 4, 8]), ALU.add, ["lg", "pen"], ["msk"])
        for b in range(4):
            dve(lambda e, b=b: e.max(out=top8a[:, b, :], in_=msk[:, b, :]), ["msk"], [("top8", b)])
            dve(lambda e, b=b: e.max_index(out=idx8a[:, b, :], in_max=top8a[:, b, :], in_values=msk[:, b, :]),
                ["msk", ("top8", b)], [("idx8", b)])
        T8 = [("top8", b) for b in range(4)]
        I8 = [("idx8", b) for b in range(4)]
        tcopy("dve", e_all[:, bs, :], idx8a[:, :, 0:2], I8, [("e_all", ti)])
        dlt = r4s[:, 3, :]
        tt("dve", dlt, top8a[:, :, 1], top8a[:, :, 0], ALU.subtract, T8, ["dlt"])
        ex_ = r4s[:, 4, :]
        act(ex_, dlt, AF.Exp, ["dlt"], ["ex_"])
        den = r4s[:, 5, :]
        ts("dve", den, ex_, 1.0, None, ALU.add, None, ["ex_"], ["den"])
        recip(den, den, ["den"], ["den"])
        tt("dve", cw_all[:, bs, 0], den, gw, ALU.mult, ["den", "gw"], [("cw", ti, 0)])
        tt("dve", ex_, ex_, den, ALU.mult, ["ex_", "den"], ["ex_"])
        tt("dve", cw_all[:, bs, 1], ex_, gw, ALU.mult, ["ex_", "gw"], [("cw", ti, 1)])
        iob = iota_e[:, None, :].broadcast_to([128, 4, 32])
        tt("dve", A1all[:, bs, :], iob, e_all[:, bs, 0:1].broadcast_to([128, 4, 32]), ALU.is_equal,
           ["iota_e", ("e_all", ti)], [("A1", ti)])
        tt("dve", A2all[:, bs, :], iob, e_all[:, bs, 1:2].broadcast_to([128, 4, 32]), ALU.is_equal,
           ["iota_e", ("e_all", ti)], [("A2", ti)])
        tt("dve", Ab, A1all[:, bs, :], A2all[:, bs, :], ALU.add, [("A1", ti), ("A2", ti)], ["Ab"])

    def S2b2(ti):
        b0 = ti * 4
        bs = slice(b0, b0 + 4)
        n_mm = 0
        tot_mm = 4 + 6 + 4
        for b in range(4):
            mm(pp[:, b * 32:(b + 1) * 32], Lst, Ab[:, b, :], n_mm == 0, False, ["Lst", "Ab"], [pkp])
            n_mm += 1
            for b_ in range(b):
                mm(pp[:, b * 32:(b + 1) * 32], ones_b, Ab[:, b_, :], False, False, ["ones_b", "Ab"], [pkp])
                n_mm += 1
        for b in range(4):
            n_mm += 1
            mm(pp[:, 128:160], ones_b, Ab[:, b, :], False, n_mm == tot_mm, ["ones_b", "Ab"], [pkp])
        tt("dve", Pt, pp[:, 0:128].rearrange("p (b n) -> p b n", n=32), carry[:, None, :].broadcast_to([128, 4, 32]),
           ALU.add, [pkp, "carry"], ["Pt"])
        tt("dve", carry, pp[:, 128:160], carry, ALU.add, [pkp, "carry", "Pt"], ["carry"])
        tt("dve", ptm, Pt, A1all[:, bs, :], ALU.mult, ["Pt", ("A1", ti)], ["ptm"])
        dve(lambda e: e.tensor_reduce(out=r_all[:, bs, 0], in_=ptm, axis=AX.X, op=ALU.add), ["ptm"], [("r_all", ti, 0)])
        tt("dve", ptm, Pt, A2all[:, bs, :], ALU.mult, ["Pt", ("A2", ti), ("r_all", ti, 0)], ["ptm"])
        dve(lambda e: e.tensor_reduce(out=r_all[:, bs, 1], in_=ptm, axis=AX.X, op=ALU.add), ["ptm"], [("r_all", ti, 1)])

    gcn4 = [0]

    def otrans(ti):
        ob2 = ti % 2
        for (osrc, odst, onm) in ((o_a, oaT2[ob2], "oaT"), (o_b, obT2[ob2], "obT")):
            for c in range(4):
                pb, pk = ps_rot()
                pbb = pb.bitcast(BF16)
                for bi in range(4):
                    tr(pbb[:, bi * 128:(bi + 1) * 128], osrc[:, ti * 4 + bi, c * 128:(c + 1) * 128], ident_b,
                       [("o_x",), "ident_b"], [pk])
                evac_copy(odst[:, c, :], pbb[:, 0:TT], [pk], [(onm, ob2, c)])

    def projmerge(ti):
        t0 = ti * TT
        ob2 = ti % 2
        oaT, obT = oaT2[ob2], obT2[ob2]
        for m in range(8):
            gi = gcn4[0] % 2
            gcn4[0] += 1
            dma("sp", gat[gi], ga_v[:, m, t0:t0 + TT], (), [("gat", gi)])
            dma("sp", gbt[gi], gb_v[:, m, t0:t0 + TT], (), [("gbt", gi)])
            pa, pka = ps_rot()
            for c in range(4):
                mm(pa, woa_b[:, c, m * 128:(m + 1) * 128], oaT[:, c, :], c == 0, c == 3, WOA + [("oaT", ob2, c)], [pka])
            pbk, pkb = ps_rot()
            for c in range(4):
                mm(pbk, wob_b[:, c, m * 128:(m + 1) * 128], obT[:, c, :], c == 0, c == 3, WOB + [("obT", ob2, c)], [pkb])
            i2 = m % 2
            tt("dve", mt1[i2], pa, gat[gi], ALU.mult, [pka, ("gat", gi)], [("mt1", i2)])
            tt("dve", mt2[i2], pbk, gbt[gi], ALU.mult, [pkb, ("gbt", gi)], [("mt2", i2)])
            tt("pool", mT[:, m, :], mt1[i2], mt2[i2], ALU.add, [("mt1", i2), ("mt2", i2)], [("mT", m)])

    otrans(0)
    projmerge(0)
    if NT4 > 1:
        otrans(1)
    for ti in range(NT4):
        S1(ti, 0)
        S1(ti, 1)
        S2a(ti, 0)
        S1b(ti, 0)
        S1(ti, 2)
        S2a(ti, 1)
        S1b(ti, 1)
        S1(ti, 3)
        S2a(ti, 2)
        S1b(ti, 2)
        if ti >= 1:
            S2b2(ti - 1)
        if ti + 1 < NT4:
            projmerge(ti + 1)
        S2a(ti, 3)
        S1b(ti, 3)
        if ti + 2 < NT4:
            otrans(ti + 2)
        S2b1(ti)
    S2b2(NT4 - 1)
    if dbg:
        rt_dbg = dscr("rt_dbg", [128, NB * 6], F32)
        rv = rt_dbg.rearrange("p (b s) -> p b s", s=6)
        ALLK = [("cw", t_, k_) for t_ in range(NT4) for k_ in range(2)] + [("e_all", t_) for t_ in range(NT4)] + [("r_all", t_, k_) for t_ in range(NT4) for k_ in range(2)]
        dma("sp", rv[:, :, 0:2], cw_all, ALLK, ["rt1"])
        dma("sp", rv[:, :, 2:4], e_all, ALLK, ["rt2"])
        dma("sp", rv[:, :, 4:6], r_all, ALLK, ["rt3"])
    sc.barrier()
    if stage <= 8:
        return finish(nc, es, sc, None)

    A.release(p4)
    padf = A.alloc([32], F32)
    padi = A.alloc([32], I32)
    incl = A.alloc([32], F32)
    offs = A.alloc([32], F32)
    ones32 = A.alloc([32], F32)
    big3 = A.alloc([NTILE, 32], F32)
    slotf = A.alloc([NB, 2], F32)
    slot_i = A.alloc([NB * 2], I32)
    jv = A.alloc([NTILE], F32)
    pidx = A.alloc([1], F32)
    tef = A.alloc([NTILE], F32)
    tesh = A.alloc([NTILE], F32)
    widx_i = A.alloc([NTILE], I32)
    yidx = A.alloc([2 * NTILE], I32)
    p5 = A.mark()
    dma("sp", jv, jv_d, (), ["jv"])
    dma("sp", pidx, pidx_d, (), ["pidx"])
    memset("dve", ones32, 1.0, ["ones32"])
    ts("dve", padf, carry, float(SLOT_T - 1), None, ALU.add, None, ["carry"], ["padf"])
    tcopy("dve", padi, padf, ["padf"], ["padi"])
    ts("dve", padi, padi, SH, None, ALU.arith_shift_right, None, ["padi"], ["padi"])
    ts("dve", padi, padi, SH, None, ALU.logical_shift_left, None, ["padi"], ["padi"])
    tcopy("dve", padf, padi, ["padi"], ["padf"])
    sc.add("dve", lambda e: e.tensor_tensor_scan(out=incl, data0=ones32, data1=padf, initial=0.0,
                                                  op0=ALU.mult, op1=ALU.add), ["ones32", "padf"], ["incl"])
    tt("dve", offs, incl, padf, ALU.subtract, ["incl", "padf"], ["offs"])
    for k, Aall in ((0, A1all), (1, A2all)):
        tt("dve", big3[:, 0:NB, :], Aall, offs[:, None, :].broadcast_to([128, NB, 32]), ALU.mult, ["offs"], ["big3"])
        sc.add("dve", lambda e, k=k: e.tensor_reduce(out=slotf[:, :, k], in_=big3[:, 0:NB, :], axis=AX.X, op=ALU.add),
               ["big3"], [("slotf", k)])
    tt("dve", slotf, slotf, r_all, ALU.add, [("slotf", 0), ("slotf", 1)], ["slotf"])
    tcopy("dve", slot_i, slotf.rearrange("p b k -> p (b k)"), ["slotf"], ["slot_i"])
    tt("dve", big3, incl[:, None, :].broadcast_to([128, NTILE, 32]), jv[:, :, None].broadcast_to([128, NTILE, 32]),
       ALU.is_le, ["incl", "jv", ("slotf", 0), ("slotf", 1)], ["big3"])
    sc.add("dve", lambda e: e.tensor_reduce(out=tef, in_=big3, axis=AX.X, op=ALU.add), ["big3"], ["tef"])
    ts("dve", tef, tef, 31.0, None, ALU.min, None, ["tef"], ["tef"])
    memset("dve", tesh, -1.0, ["tesh"])
    tcopy("dve", tesh[:, 3:NTILE], tef[:, 0:NTILE - 3], ["tef", "tesh"], ["tesh"])
    tt("dve", tesh, tesh, tef, ALU.is_equal, ["tesh", "tef"], ["tesh"])
    ts("dve", tef, tef, 128.0, pidx[:, 0:1], ALU.mult, ALU.add, ["tef", "pidx"], ["tef"])
    stt(tef, tesh, 100000.0, tef, ALU.mult, ALU.add, ["tesh", "tef"], ["tef"])
    tcopy("dve", widx_i, tef, ["tef"], ["widx_i"])
    NH = 2 * NTILE
    sval = A.alloc([NH], F32)
    endv = A.alloc([32], F32)
    cA = A.alloc([NH], F32)
    cB = A.alloc([NH], F32)
    bigv = A.alloc([NH, 32], F32)
    dma("sp", sval, sval_d, (), ["sval"])
    tt("dve", endv, offs, carry, ALU.add, ["offs", "carry"], ["endv"])
    tt("dve", bigv, offs[:, None, :].broadcast_to([128, NH, 32]), sval[:, :, None].broadcast_to([128, NH, 32]),
       ALU.is_le, ["offs", "sval"], ["bigv"])
    sc.add("dve", lambda e: e.tensor_reduce(out=cA, in_=bigv, axis=AX.X, op=ALU.add), ["bigv"], ["cA"])
    tt("dve", bigv, endv[:, None, :].broadcast_to([128, NH, 32]), sval[:, :, None].broadcast_to([128, NH, 32]),
       ALU.is_le, ["endv", "sval", "cA"], ["bigv"])
    sc.add("dve", lambda e: e.tensor_reduce(out=cB, in_=bigv, axis=AX.X, op=ALU.add), ["bigv"], ["cB"])
    tt("dve", cA, cA, cB, ALU.subtract, ["cA", "cB"], ["cA"])
    ts("dve", cA, cA, -1.0, -1.0e6, ALU.add, ALU.mult, ["cA"], ["cA"])
    tt("dve", cA, cA, sval, ALU.add, ["cA", "sval"], ["cA"])
    tcopy("dve", yidx, cA, ["cA"], ["yidx"])
    if dbg:
        sl_dbg = dscr("sl_dbg", [128, NB * 2 + NTILE], I32)
        dma("sp", sl_dbg[:, 0:NB * 2], slot_i, ["slot_i"], ["sl1"])
        dma("sp", sl_dbg[:, NB * 2:], widx_i, ["widx_i"], ["sl2"])
    hrow = [A.alloc([D], BF16) for _ in range(3)]
    for blk in range(NB):
        hr = hrow[blk % 3]
        dma("sp", hr, h2_d[blk * 128:(blk + 1) * 128, :], (), [("hrow", blk % 3)])
        for k in range(2):
            off_ap = slot_i[:, blk * 2 + k:blk * 2 + k + 1]
            sc.add("pool", lambda e, hr=hr, off_ap=off_ap: e.indirect_dma_start(
                out=xs_d[:, :], out_offset=bass.IndirectOffsetOnAxis(ap=off_ap, axis=0), in_=hr, in_offset=None),
                [("hrow", blk % 3), "slot_i"], [("xs_d", blk, k)], dma=True)
    sc.barrier()
    if stage <= 9:
        return finish(nc, es, sc, None)

    A.release(p5)
    while pre_pos[0] < len(pre_steps):
        precast_step()
    NW = 3
    wall_t = [A.alloc([6144], BF16) for _ in range(NW)]
    wg_t = [w[:, 0:2048] for w in wall_t]
    wu_t = [w[:, 2048:4096] for w in wall_t]
    wd_t = [w[:, 4096:6144] for w in wall_t]
    xs_t = [A.alloc([2, D], BF16) for _ in range(NW)]
    xsT = [A.alloc([8, SLOT_T], BF16) for _ in range(2)]
    sil = [A.alloc([2, SLOT_T], F32) for _ in range(2)]
    hidT = [A.alloc([2, SLOT_T], BF16) for _ in range(2)]
    yt = [A.alloc([D], F32) for _ in range(3)]
    ycn = 0
    breg = {}
    NTL = NTILE if stage > 10 else 4
    WXALL = [("wx", mi, e_) for mi in range(3) for e_ in range(32)]

    def prefetch6(j):
        b3 = j % NW
        wi = widx_i[:, j:j + 1]

        def wgather(e, wi=wi, b3=b3):
            if "r" not in breg:
                breg["r"] = e.alloc_register("wbound")
                e.reg_mov(breg["r"], 32 * 128 - 1)
            return e.indirect_dma_start(
                out=wall_t[b3], out_offset=None, in_=wx_d[:, :], in_offset=bass.IndirectOffsetOnAxis(ap=wi, axis=0),
                bounds_check=breg["r"], oob_is_err=False)
        sc.add("pool", wgather, (), [("wg", b3), ("wu", b3), ("wd", b3)], dma=True)
        dma("sp", xs_t[b3], xs_d[j * SLOT_T:(j + 1) * SLOT_T, :].rearrange("(s p) f -> p s f", p=128), (), [("xs_t", b3)])

    for j in range(min(2, NTL)):
        prefetch6(j)
    for j in range(NTL):
        if j + 2 < NTL:
            prefetch6(j + 2)
        b2 = j % 2
        b3 = j % NW
        for sbk in range(2):
            for half in range(2):
                pb, pk = ps_next()
                pbb = pb.bitcast(BF16)
                for q in range(4):
                    kc = half * 4 + q
                    tr(pbb[:, q * 128:(q + 1) * 128], xs_t[b3][:, sbk, kc * 128:(kc + 1) * 128], ident_b,
                       [("xs_t", b3), "ident_b"], [pk])
                evac_copy(xsT[b2][:, half * 4:(half + 1) * 4, sbk * 128:(sbk + 1) * 128],
                          pbb[:, 0:512].rearrange("p (q t) -> p q t", q=4), [pk], [("xsT", b2, sbk, half)])
        XST = [("xsT", b2, a, b) for a in range(2) for b in range(2)]
        pg, pkg = ps_next()
        for f in range(2):
            for kc in range(8):
                mm(pg[:, f * SLOT_T:(f + 1) * SLOT_T], wg_t[b3][:, kc * 256 + f * 128:kc * 256 + (f + 1) * 128],
                   xsT[b2][:, kc, :], kc == 0, kc == 7, [("wg", b3)] + XST, [pkg])
        pu, pku = ps_next()
        for f in range(2):
            for kc in range(8):
                mm(pu[:, f * SLOT_T:(f + 1) * SLOT_T], wu_t[b3][:, kc * 256 + f * 128:kc * 256 + (f + 1) * 128],
                   xsT[b2][:, kc, :], kc == 0, kc == 7, [("wu", b3)] + XST, [pku])
        act(sil[b2].rearrange("p f s -> p (f s)"), pg, AF.Silu, [pkg], [("sil", b2)])
        tt("dve", hidT[b2].rearrange("p f s -> p (f s)"), pu, sil[b2].rearrange("p f s -> p (f s)"), ALU.mult,
           [pku, ("sil", b2)], [("hidT", b2)])
        for sbk in range(2):
            y_ = yt[ycn % 3]
            yk = ("yt", ycn % 3)
            ycn += 1
            for half in range(2):
                py, pky = ps_next()
                for f in range(2):
                    mm(py, hidT[b2][:, f, sbk * 128:(sbk + 1) * 128], wd_t[b3][:, f * 1024 + half * 512:f * 1024 + (half + 1) * 512],
                       f == 0, f == 1, [("hidT", b2), ("wd", b3)], [pky])
                tt("dve", y_[:, half * 512:(half + 1) * 512], py, gt2_bc[:, half * 512:(half + 1) * 512], ALU.mult,
                   [pky], [(yk, half)])
            yoff = yidx[:, 2 * j + sbk:2 * j + sbk + 1]

            def yscatter(e, y_=y_, yoff=yoff):
                if "r2" not in breg:
                    breg["r2"] = e.alloc_register("ybound")
                    e.reg_mov(breg["r2"], NSLOT - 1)
                return e.indirect_dma_start(
                    out=ys_d[:, :], out_offset=bass.IndirectOffsetOnAxis(ap=yoff, axis=0), in_=y_, in_offset=None,
                    bounds_check=breg["r2"], oob_is_err=False)
            sc.add("pool", yscatter, [(yk, 0), (yk, 1)], [("ys_d", j, sbk)], dma=True)
    sc.barrier()
    if stage <= 11:
        return finish(nc, es, sc, None)

    A.release(p5)
    gfin = A.alloc([D], F32)
    dma("sp", gfin, gfin_d[0:1, :].to_broadcast([128, D]), (), ["gfin"])
    R7 = 4
    g1 = [A.alloc([D], F32) for _ in range(R7)]
    g2_ = [A.alloc([D], F32) for _ in range(R7)]
    x1r = [A.alloc([D], F32) for _ in range(R7)]
    ot = [A.alloc([D], F32) for _ in range(2)]
    junk7 = A.alloc([D], BF16)
    sm7 = A.alloc([8], F32)

    def load7(blk):
        i4 = blk % R7
        for k, gt_ in ((0, g1), (1, g2_)):
            off_ap = slot_i[:, blk * 2 + k:blk * 2 + k + 1]
            sc.add("pool", lambda e, gt_=gt_, off_ap=off_ap, i4=i4: e.indirect_dma_start(
                out=gt_[i4], out_offset=None, in_=ys_d[:, :], in_offset=bass.IndirectOffsetOnAxis(ap=off_ap, axis=0)),
                (), [("g", k, i4)], dma=True)
        dma("sp", x1r[i4], x1_d[blk * 128:(blk + 1) * 128, :], (), [("x1r", i4)])

    for blk in range(2):
        load7(blk)
    for blk in range(NB):
        if blk + 2 < NB:
            load7(blk + 2)
        i4 = blk % R7
        i2 = blk % 2
        stt(x1r[i4], g1[i4], cw_all[:, blk, 0:1], x1r[i4], ALU.mult, ALU.add, [("g", 0, i4), ("x1r", i4)], [("x1r", i4)])
        stt(x1r[i4], g2_[i4], cw_all[:, blk, 1:2], x1r[i4], ALU.mult, ALU.add, [("g", 1, i4), ("x1r", i4)], [("x1r", i4)])
        act(junk7, x1r[i4], AF.Square, [("x1r", i4)], [("ss7", i2)], accum_out=sm7[:, i2:i2 + 1])
        act(sm7[:, 2 + i2:3 + i2], sm7[:, i2:i2 + 1], AF.Sqrt, [("ss7", i2)], [("rs7", i2)], scale=1.0 / D, bias=eps_c[:, 0:1])
        recip(sm7[:, 2 + i2:3 + i2], sm7[:, 2 + i2:3 + i2], [("rs7", i2)], [("rs7", i2)])
        stt(ot[i2], x1r[i4], sm7[:, 2 + i2:3 + i2], gfin, ALU.mult, ALU.mult, [("x1r", i4), ("rs7", i2), "gfin"], [("ot", i2)])
        dma("sp", out_d[blk * 128:(blk + 1) * 128, :], ot[i2], [("ot", i2)], [("out", blk)])
    sc.barrier()
    return finish(nc, es, sc, None)


def finish(nc, es, sc, _):
    block = es.enter_context(nc.Block())
    sc.emit(block)
    es.close()
    return nc


def fm(v, k):
    return np.ascontiguousarray(np.asarray(v, np.float32).reshape(k, 128).T)


def kmajor(w):
    K, N = w.shape
    return np.ascontiguousarray(w.reshape(K // 128, 128, N).transpose(1, 0, 2).reshape(128, (K // 128) * N))


def host_inputs(I):
    shared = {}
    shared["wada"] = kmajor(I["w_ada"][0])
    shared["bada_row"] = np.ascontiguousarray(I["b_ada"][0].reshape(1, 6144).astype(np.float32))
    shared["gmix_row"] = np.ascontiguousarray(I["g_mix"][0].reshape(1, D).astype(np.float32))
    shared["gffn_row"] = np.ascontiguousarray(I["g_ffn"][0].reshape(1, D).astype(np.float32))
    shared["gfinal_row"] = np.ascontiguousarray(I["g_final"].reshape(1, D).astype(np.float32))
    w_in = I["w_in"][0]
    kr = w_in[:, 384:416]
    z64 = np.zeros((D, 64), np.float32)
    kr_sw = np.concatenate([kr[:, 16:32], kr[:, 0:16]], axis=1)
    win_ext = np.concatenate([w_in, z64, kr, z64, kr_sw], axis=1)
    assert win_ext.shape[1] == WC
    shared["win"] = kmajor(win_ext)
    shared["gq_fm"] = fm(I["g_q"][0], 2)
    shared["gkv_fm"] = fm(I["g_kv"][0], 1)
    wuq = I["w_uq"][0]
    wuq_sw = wuq.reshape(256, 8, 96).copy()
    wuq_sw[:, :, 64:80] = wuq.reshape(256, 8, 96)[:, :, 80:96]
    wuq_sw[:, :, 80:96] = wuq.reshape(256, 8, 96)[:, :, 64:80]
    shared["wuq"] = kmajor(wuq)
    shared["wuq_sw"] = kmajor(wuq_sw.reshape(256, 768))
    shared["wuk"] = np.ascontiguousarray(I["w_uk"][0])
    shared["wuv"] = np.ascontiguousarray(I["w_uv"][0])
    rc = np.zeros((128, 2), np.float32)
    inv = (10000.0 ** (-np.arange(16, dtype=np.float32) / 16)).astype(np.float32)
    for p in range(128):
        rc[p, 0] = inv[p % 16]
    shared["rconst"] = rc
    shared["ident"] = np.eye(128, dtype=np.float32)
    sel = np.zeros((128, 96), np.float32)
    for p in range(64, 96):
        sel[p, p] = 1.0
    shared["sel"] = sel
    kk = np.arange(128)[:, None]
    qq = np.arange(640)[None, :]
    idx = np.clip(qq - kk, -256, 256) + 256
    dq = qq // 64 - kk // 64
    valid = (dq >= 0) & (dq <= 8)
    rb = I["rel_bias"][0]
    bt = np.where(valid[None], rb[:, idx], np.float32(-1e30)).astype(np.float32)
    shared["biasT"] = np.ascontiguousarray(bt.transpose(1, 0, 2).reshape(128, 8 * 640))
    shared["woa"] = kmajor(I["w_oa"][0])
    shared["wob"] = kmajor(I["w_ob"][0])
    shared["wout"] = kmajor(I["w_out"][0])
    shared["wr"] = kmajor(np.concatenate([I["w_rg"][0], I["w_re"][0]], axis=1))
    shared["rb"] = np.ascontiguousarray(np.concatenate([I["b_rg"][0], I["b_re"][0]]).reshape(1, 36).astype(np.float32))
    shared["iota_e"] = np.ascontiguousarray(np.broadcast_to(np.arange(32, dtype=np.float32), (128, 32)))
    shared["lst"] = np.triu(np.ones((128, 128), np.float32), 1)
    shared["jv"] = np.ascontiguousarray(np.broadcast_to((np.arange(NTILE, dtype=np.float32) * SLOT_T), (128, NTILE)))
    shared["pidx"] = np.arange(128, dtype=np.float32).reshape(128, 1)
    shared["sval"] = np.ascontiguousarray(
        (np.arange(2 * NTILE, dtype=np.float32)[None, :] * 128 + np.arange(128, dtype=np.float32)[:, None]))
    shared["wgl"] = np.ascontiguousarray(I["w_gate"][0].reshape(32, 8, 128, 256).transpose(0, 2, 1, 3).reshape(32 * 128, 2048))
    shared["wul"] = np.ascontiguousarray(I["w_up"][0].reshape(32, 8, 128, 256).transpose(0, 2, 1, 3).reshape(32 * 128, 2048))
    shared["wdl"] = np.ascontiguousarray(I["w_down"][0].reshape(32, 2, 128, 1024).transpose(0, 2, 1, 3).reshape(32 * 128, 2048))
    per_core = []
    for b in range(8):
        d = dict(shared)
        d["x"] = np.ascontiguousarray(I["x"][b])
        d["cfm"] = fm(I["c"][b], 8)
        d["pos"] = np.ascontiguousarray(I["positions"][b].reshape(1, S).astype(np.int32))
        per_core.append(d)
    return per_core


_NC_CACHE = {}


def kernel(**inputs):
    I = {k: np.asarray(v) for k, v in inputs.items()}
    in_maps = host_inputs(I)
    if "nc" not in _NC_CACHE:
        _NC_CACHE["nc"] = build_nc()
    nc = _NC_CACHE["nc"]
    res = run_bass_kernel_spmd(nc, in_maps, core_ids=list(range(8)))
    return np.stack([r["out"] for r in res.results], axis=0).astype(np.float32)
```
